# Optimizing a Trainium2 kernel written in Bass

```python
import math
import jax, jax.numpy as jnp
from jax import lax
import numpy as np

D_MODEL = 1024
BATCH = 8
SEQ = 2048
DEPTH = 2

CTX_LEN = 256
GRID_W = 64
HD = 64
BLOCK = 128
WINDOW = 128
ROPE_THETA = 10000.0
EPS = 1e-6
NEG_INF = -1e30
A_HEADS = 8
A_KV = 2
B_HEADS = 8
B_KV = 2
C_HEADS = 4
D_HEADS = 8
Q_LORA = 512
KV_LORA = 256
QK_NOPE = 64
QK_ROPE = 32
V_HEAD = 64
N_BRANCH = 4
BRANCH_W = 512
Q_SIZES = (A_HEADS * HD, B_HEADS * HD, 2 * C_HEADS * HD, Q_LORA)
KV_SIZES = (A_KV * HD, A_KV * HD, B_KV * HD, B_KV * HD, 2 * C_HEADS * HD, 2 * C_HEADS * HD, KV_LORA, QK_ROPE)
Q_COLS = A_HEADS * HD + B_HEADS * HD + 2 * C_HEADS * HD + Q_LORA
KV_COLS = 2 * A_KV * HD + 2 * B_KV * HD + 4 * C_HEADS * HD + KV_LORA + QK_ROPE
GATE_COLS = N_BRANCH * D_MODEL
IN_COLS = Q_COLS + KV_COLS + GATE_COLS
D_FF = 2816
N_EXPERTS = 8
TOP_K = 2
EXPERT_FF = 3584
MOE_BLOCK = 128
N_DENSE = (DEPTH + 1) // 2
N_MOE = DEPTH // 2

kernel_name = 'hybrid_gated_branch_diffusion_block'


def _offsets(sizes):
    return [int(v) for v in np.cumsum(sizes)[:-1]]


def rms(x, g):
    xf = x.astype(jnp.float32)
    y = xf * lax.rsqrt(jnp.mean(xf * xf, axis=-1, keepdims=True) + EPS)
    return (y * g.astype(jnp.float32)).astype(x.dtype)


def adaln(x, g, shift, scale):
    return rms(x, g) * (1 + scale) + shift


def modulation(cond, w, b):
    m = jax.nn.silu(cond) @ w + b
    return jnp.split(m, 6, axis=-1)


def axial_rope(rows, rot_dim):
    t = jnp.arange(rows * GRID_W)
    n = rot_dim // 4
    inv = jnp.power(ROPE_THETA, -jnp.arange(n, dtype=jnp.float32) / n)
    ang = jnp.concatenate([(t // GRID_W).astype(jnp.float32)[:, None] * inv,
                           (t % GRID_W).astype(jnp.float32)[:, None] * inv], axis=-1)
    return jnp.cos(ang), jnp.sin(ang)


def apply_rope(x, rope):
    if rope is None:
        return x
    cos, sin = rope
    half = x.shape[-1] // 2
    c = cos[:, None, :].astype(x.dtype)
    s = sin[:, None, :].astype(x.dtype)
    x1, x2 = x[..., :half], x[..., half:]
    return jnp.concatenate([x1 * c - x2 * s, x1 * s + x2 * c], axis=-1)


def build_q(pq, P, rope_h, rope_r):
    B, L, _ = pq.shape
    qa, qb, qc, cq = jnp.split(pq, _offsets(Q_SIZES), axis=-1)
    qa = apply_rope(rms(qa.reshape(B, L, A_HEADS, HD), P['a_qn']), rope_h).reshape(B, L, A_KV, A_HEADS // A_KV, HD)
    qb = apply_rope(rms(qb.reshape(B, L, B_HEADS, HD), P['b_qn']), rope_h).reshape(B, L, B_KV, B_HEADS // B_KV, HD)
    qc = apply_rope(rms(qc.reshape(B, L, 2 * C_HEADS, HD), P['c_qn']), rope_h).reshape(B, L, C_HEADS, 2, HD)
    qd = (rms(cq, P['d_q_norm']) @ P['d_w_uq']).reshape(B, L, D_HEADS, QK_NOPE + QK_ROPE)
    qd = jnp.concatenate([rms(qd[..., :QK_NOPE], P['d_qn_nope']),
                          apply_rope(rms(qd[..., QK_NOPE:], P['d_qn_rope']), rope_r)], axis=-1)
    return {'a': qa, 'b': qb, 'c1': qc[:, :, :, 0], 'c2': qc[:, :, :, 1], 'd': qd[:, :, :, None, :]}


def build_kv(pkv, P, rope_h, rope_r):
    B, S, _ = pkv.shape
    ka, va, kb, vb, kc, vc, ckv, kr = jnp.split(pkv, _offsets(KV_SIZES), axis=-1)
    ka = apply_rope(rms(ka.reshape(B, S, A_KV, HD), P['a_kn']), rope_h)
    kb = apply_rope(rms(kb.reshape(B, S, B_KV, HD), P['b_kn']), rope_h)
    kc = apply_rope(rms(kc.reshape(B, S, 2 * C_HEADS, HD), P['c_kn']), rope_h).reshape(B, S, C_HEADS, 2, HD)
    kvd = (rms(ckv, P['d_kv_norm']) @ P['d_w_ukv']).reshape(B, S, D_HEADS, QK_NOPE + V_HEAD)
    kd_rope = apply_rope(rms(kr.reshape(B, S, 1, QK_ROPE), P['d_kn_rope']), rope_r)
    kd = jnp.concatenate([rms(kvd[..., :QK_NOPE], P['d_kn_nope']),
                          jnp.broadcast_to(kd_rope, (B, S, D_HEADS, QK_ROPE))], axis=-1)
    return {'ka': ka, 'va': va.reshape(B, S, A_KV, HD),
            'kb': kb, 'vb': vb.reshape(B, S, B_KV, HD),
            'kc1': kc[:, :, :, 0], 'kc2': kc[:, :, :, 1], 'vc': vc.reshape(B, S, C_HEADS, 2 * HD),
            'kd': kd, 'vd': kvd[..., QK_NOPE:]}


def dense_attn(q, k, v, scale, sink=None):
    B, L, Hk, G, d = q.shape
    nb = L // BLOCK
    qb = jnp.moveaxis(q.reshape(B, nb, BLOCK, Hk, G, d), 1, 0)

    def one(qblk):
        s = jnp.einsum('bqhgd,bkhd->bhgqk', qblk, k, preferred_element_type=jnp.float32) * scale
        if sink is not None:
            s_sink = jnp.broadcast_to(sink.astype(jnp.float32).reshape(1, Hk, G, 1, 1), s.shape[:-1] + (1,))
            p = jax.nn.softmax(jnp.concatenate([s, s_sink], axis=-1), axis=-1)[..., :-1]
        else:
            p = jax.nn.softmax(s, axis=-1)
        return jnp.einsum('bhgqk,bkhd->bqhgd', p.astype(v.dtype), v)

    out = lax.map(one, qb)
    return jnp.moveaxis(out, 0, 1).reshape(B, L, Hk, G, v.shape[-1])


def window_attn(q, k, v, k_ctx, v_ctx, sink, scale):
    B, L, Hk, G, d = q.shape
    nb = L // BLOCK
    pad = ((0, 0), (BLOCK, BLOCK), (0, 0), (0, 0))
    kp = jnp.pad(k, pad).reshape(B, nb + 2, BLOCK, Hk, d)
    vp = jnp.pad(v, pad).reshape(B, nb + 2, BLOCK, Hk, v.shape[-1])
    kw = jnp.concatenate([kp[:, :-2], kp[:, 1:-1], kp[:, 2:]], axis=2)
    vw = jnp.concatenate([vp[:, :-2], vp[:, 1:-1], vp[:, 2:]], axis=2)
    qb = q.reshape(B, nb, BLOCK, Hk, G, d)
    s_loc = jnp.einsum('bnqhgd,bnkhd->bnhgqk', qb, kw, preferred_element_type=jnp.float32) * scale
    qi = jnp.arange(BLOCK)[:, None]
    kj = jnp.arange(3 * BLOCK)[None, :]
    kpos = (jnp.arange(nb)[:, None, None] - 1) * BLOCK + kj
    valid = (kj >= qi + BLOCK - WINDOW) & (kj <= qi + BLOCK + WINDOW) & (kpos >= 0) & (kpos < L)
    s_loc = jnp.where(valid[None, :, None, None], s_loc, NEG_INF)
    s_ctx = jnp.einsum('bnqhgd,bkhd->bnhgqk', qb, k_ctx, preferred_element_type=jnp.float32) * scale
    s_sink = jnp.broadcast_to(sink.astype(jnp.float32).reshape(1, 1, Hk, G, 1, 1), s_loc.shape[:-1] + (1,))
    p = jax.nn.softmax(jnp.concatenate([s_loc, s_ctx, s_sink], axis=-1), axis=-1)
    nl = 3 * BLOCK
    nc = k_ctx.shape[1]
    out = (jnp.einsum('bnhgqk,bnkhd->bnqhgd', p[..., :nl].astype(v.dtype), vw)
           + jnp.einsum('bnhgqk,bkhd->bnqhgd', p[..., nl:nl + nc].astype(v.dtype), v_ctx))
    return out.reshape(B, L, Hk, G, v.shape[-1])


def diff_attn(q1, q2, k1, k2, v, lam, scale):
    B, L, H, d = q1.shape
    nb = L // BLOCK
    qs = jnp.moveaxis(jnp.stack([q1, q2], axis=2).reshape(B, nb, BLOCK, 2, H, d), 1, 0)

    def one(qblk):
        s1 = jnp.einsum('bqhd,bkhd->bhqk', qblk[:, :, 0], k1, preferred_element_type=jnp.float32) * scale
        s2 = jnp.einsum('bqhd,bkhd->bhqk', qblk[:, :, 1], k2, preferred_element_type=jnp.float32) * scale
        p = jax.nn.softmax(s1, axis=-1) - lam * jax.nn.softmax(s2, axis=-1)
        return jnp.einsum('bhqk,bkhe->bqhe', p.astype(v.dtype), v)

    out = lax.map(one, qs)
    return jnp.moveaxis(out, 0, 1).reshape(B, L, H, v.shape[-1])


def run_mixers(q, kv, kv_ctx, P, lam, lam_init):
    sc = 1.0 / math.sqrt(HD)
    if kv is None:
        keys = kv_ctx
        oa = dense_attn(q['a'], keys['ka'], keys['va'], sc, sink=P['a_sink'])
    else:
        oa = window_attn(q['a'], kv['ka'], kv['va'], kv_ctx['ka'], kv_ctx['va'], P['a_sink'], sc)
        keys = {n: jnp.concatenate([kv[n], kv_ctx[n]], axis=1)
                for n in ('kb', 'vb', 'kc1', 'kc2', 'vc', 'kd', 'vd')}
    ob = dense_attn(q['b'], keys['kb'], keys['vb'], sc)
    oc = diff_attn(q['c1'], q['c2'], keys['kc1'], keys['kc2'], keys['vc'], lam, sc)
    oc = rms(oc, P['c_subln']) * (1.0 - lam_init)
    od = dense_attn(q['d'], keys['kd'], keys['vd'], 1.0 / math.sqrt(QK_NOPE + QK_ROPE))
    B, L = oa.shape[0], oa.shape[1]
    return jnp.stack([oa.reshape(B, L, BRANCH_W), ob.reshape(B, L, BRANCH_W),
                      oc.reshape(B, L, BRANCH_W), od.reshape(B, L, BRANCH_W)], axis=2)


def merge(br, pg, P):
    y = jnp.einsum('blnw,nwd->blnd', br, P['w_br'])
    g = jax.nn.sigmoid(pg.reshape(y.shape))
    return jnp.sum(g * y, axis=2) @ P['w_out']


def swiglu(h, wg, wu, wd):
    return (jax.nn.silu(h @ wg) * (h @ wu)) @ wd


def moe_swiglu(h, w_router, w1, w3, w2):
    N, Dm = h.shape
    logits = (h @ w_router).astype(jnp.float32)
    top_v, top_i = lax.top_k(logits, TOP_K)
    wts = jax.nn.softmax(top_v, axis=-1)
    n_assign = N * TOP_K
    e_flat = top_i.reshape(-1)
    tok_flat = jnp.repeat(jnp.arange(N), TOP_K)
    order = jnp.argsort(e_flat)
    e_s, tok_s, w_s = e_flat[order], tok_flat[order], wts.reshape(-1)[order]
    counts = jnp.bincount(e_flat, length=N_EXPERTS)
    starts = jnp.cumsum(counts) - counts
    padded = (counts + MOE_BLOCK - 1) // MOE_BLOCK * MOE_BLOCK
    pstarts = jnp.cumsum(padded) - padded
    pends = pstarts + padded
    dest = pstarts[e_s] + (jnp.arange(n_assign) - starts[e_s])
    n_blocks = -(-n_assign // MOE_BLOCK) + N_EXPERTS
    xbuf = jnp.zeros((n_blocks * MOE_BLOCK, Dm), h.dtype).at[dest].set(h[tok_s])
    blk_e = jnp.minimum(jnp.searchsorted(pends, jnp.arange(n_blocks) * MOE_BLOCK, side='right'), N_EXPERTS - 1)

    def expert_block(args):
        xb, e = args
        return swiglu(xb, w1[e], w3[e], w2[e])

    ybuf = lax.map(expert_block, (xbuf.reshape(n_blocks, MOE_BLOCK, Dm), blk_e))
    y = ybuf.reshape(n_blocks * MOE_BLOCK, Dm)[dest] * w_s[:, None].astype(h.dtype)
    return jnp.zeros_like(h).at[tok_s].add(y)


def setup_inputs(seed: int = 0) -> dict:
    key = jax.random.key(seed)
    ks = jax.random.split(key, 40)
    cnt = [0]

    def nrm(shape, s):
        k = ks[cnt[0]]
        cnt[0] += 1
        return jax.random.normal(k, shape, jnp.float32) * s

    def gain(shape):
        return 1.0 + nrm(shape, 0.02)

    D = D_MODEL
    return {
        'x': nrm((BATCH, SEQ, D), 1.0),
        'c': nrm((BATCH, D), 1.0),
        'ctx': nrm((BATCH, CTX_LEN, D), 1.0),
        'c_ctx': nrm((D,), 1.0),
        'w_mod': nrm((DEPTH, D, 6 * D), 0.5 * D ** -0.5),
        'b_mod': nrm((DEPTH, 6 * D), 0.02),
        'mix_norm': gain((DEPTH, D)),
        'ffn_norm': gain((DEPTH, D)),
        'w_in': nrm((DEPTH, D, IN_COLS), D ** -0.5),
        'a_qn': gain((DEPTH, HD)),
        'a_kn': gain((DEPTH, HD)),
        'a_sink': nrm((DEPTH, A_HEADS), 0.5),
        'b_qn': gain((DEPTH, HD)),
        'b_kn': gain((DEPTH, HD)),
        'c_qn': gain((DEPTH, HD)),
        'c_kn': gain((DEPTH, HD)),
        'c_lq1': nrm((DEPTH, HD), 0.1),
        'c_lk1': nrm((DEPTH, HD), 0.1),
        'c_lq2': nrm((DEPTH, HD), 0.1),
        'c_lk2': nrm((DEPTH, HD), 0.1),
        'c_subln': gain((DEPTH, 2 * HD)),
        'd_q_norm': gain((DEPTH, Q_LORA)),
        'd_kv_norm': gain((DEPTH, KV_LORA)),
        'd_w_uq': nrm((DEPTH, Q_LORA, D_HEADS * (QK_NOPE + QK_ROPE)), Q_LORA ** -0.5),
        'd_w_ukv': nrm((DEPTH, KV_LORA, D_HEADS * (QK_NOPE + V_HEAD)), KV_LORA ** -0.5),
        'd_qn_nope': gain((DEPTH, QK_NOPE)),
        'd_kn_nope': gain((DEPTH, QK_NOPE)),
        'd_qn_rope': gain((DEPTH, QK_ROPE)),
        'd_kn_rope': gain((DEPTH, QK_ROPE)),
        'w_br': nrm((DEPTH, N_BRANCH, BRANCH_W, D), BRANCH_W ** -0.5),
        'w_out': nrm((DEPTH, D, D), D ** -0.5),
        'ff_w_gate': nrm((N_DENSE, D, D_FF), D ** -0.5),
        'ff_w_up': nrm((N_DENSE, D, D_FF), D ** -0.5),
        'ff_w_down': nrm((N_DENSE, D_FF, D), D_FF ** -0.5),
        'moe_router': nrm((N_MOE, D, N_EXPERTS), D ** -0.5),
        'moe_w1': nrm((N_MOE, N_EXPERTS, D, EXPERT_FF), D ** -0.5),
        'moe_w3': nrm((N_MOE, N_EXPERTS, D, EXPERT_FF), D ** -0.5),
        'moe_w2': nrm((N_MOE, N_EXPERTS, EXPERT_FF, D), EXPERT_FF ** -0.5),
    }


def reference(x, c, ctx, c_ctx, w_mod, b_mod, mix_norm, ffn_norm, w_in,
              a_qn, a_kn, a_sink, b_qn, b_kn, c_qn, c_kn, c_lq1, c_lk1, c_lq2, c_lk2, c_subln,
              d_q_norm, d_kv_norm, d_w_uq, d_w_ukv, d_qn_nope, d_kn_nope, d_qn_rope, d_kn_rope,
              w_br, w_out, ff_w_gate, ff_w_up, ff_w_down, moe_router, moe_w1, moe_w3, moe_w2):
    B, L, _ = x.shape
    rows = L // GRID_W
    rope_h = axial_rope(rows, HD)
    rope_r = axial_rope(rows, QK_ROPE)
    lat, cx = x, ctx
    kv_lo, kv_hi = Q_COLS, Q_COLS + KV_COLS
    for l in range(DEPTH):
        last = l == DEPTH - 1
        P = {'a_qn': a_qn[l], 'a_kn': a_kn[l], 'a_sink': a_sink[l], 'b_qn': b_qn[l], 'b_kn': b_kn[l],
             'c_qn': c_qn[l], 'c_kn': c_kn[l], 'c_subln': c_subln[l],
             'd_q_norm': d_q_norm[l], 'd_kv_norm': d_kv_norm[l], 'd_w_uq': d_w_uq[l], 'd_w_ukv': d_w_ukv[l],
             'd_qn_nope': d_qn_nope[l], 'd_kn_nope': d_kn_nope[l], 'd_qn_rope': d_qn_rope[l],
             'd_kn_rope': d_kn_rope[l], 'w_br': w_br[l], 'w_out': w_out[l]}
        lam_init = 0.8 - 0.6 * math.exp(-0.3 * l)
        lam = (jnp.exp(jnp.sum(c_lq1[l].astype(jnp.float32) * c_lk1[l].astype(jnp.float32)))
               - jnp.exp(jnp.sum(c_lq2[l].astype(jnp.float32) * c_lk2[l].astype(jnp.float32))) + lam_init)
        m_lat = [t[:, None, :] for t in modulation(c, w_mod[l], b_mod[l])]
        m_ctx = modulation(c_ctx, w_mod[l], b_mod[l])
        p_lat = adaln(lat, mix_norm[l], m_lat[0], m_lat[1]) @ w_in[l]
        h_ctx = adaln(cx, mix_norm[l], m_ctx[0], m_ctx[1])
        if last:
            p_ctx_kv = h_ctx @ w_in[l][:, kv_lo:kv_hi]
        else:
            p_ctx = h_ctx @ w_in[l]
            p_ctx_kv = p_ctx[..., kv_lo:kv_hi]
        kv_ctx = build_kv(p_ctx_kv, P, None, None)
        kv_lat = build_kv(p_lat[..., kv_lo:kv_hi], P, rope_h, rope_r)
        q_lat = build_q(p_lat[..., :kv_lo], P, rope_h, rope_r)
        br_lat = run_mixers(q_lat, kv_lat, kv_ctx, P, lam, lam_init)
        lat = lat + m_lat[2] * merge(br_lat, p_lat[..., kv_hi:], P)
        if not last:
            q_ctx = build_q(p_ctx[..., :kv_lo], P, None, None)
            br_ctx = run_mixers(q_ctx, None, kv_ctx, P, lam, lam_init)
            cx = cx + m_ctx[2] * merge(br_ctx, p_ctx[..., kv_hi:], P)
        i = l // 2
        if l % 2 == 0:
            ffn = lambda h: swiglu(h, ff_w_gate[i], ff_w_up[i], ff_w_down[i])
        else:
            ffn = lambda h: moe_swiglu(h.reshape(-1, h.shape[-1]), moe_router[i], moe_w1[i],
                                       moe_w3[i], moe_w2[i]).reshape(h.shape)
        lat = lat + m_lat[5] * ffn(adaln(lat, ffn_norm[l], m_lat[3], m_lat[4]))
        if not last:
            cx = cx + m_ctx[5] * ffn(adaln(cx, ffn_norm[l], m_ctx[3], m_ctx[4]))
    return lat
```

```python
import math
import numpy as np
import concourse.bass as bass
import concourse.mybir as mybir
from concourse.bass_utils import run_bass_kernel_spmd

F32 = mybir.dt.float32
BF16 = mybir.dt.bfloat16
ALU = mybir.AluOpType
AF = mybir.ActivationFunctionType

D = 1024
L = 2048
CTX = 256
NT = L + CTX
IN_COLS = 7968
GATE0 = 3872
D_FF = 2816
EXPERT_FF = 3584
EPS = 1e-6
TCS = [(0, 512), (512, 512), (1024, 512), (1536, 512), (2048, 256)]
NEG_BIG = -1.0e30

PV_MIXN = 0
PV_FFNN = 8
PV_BMOD = 16
PV_AQN = 64
PV_AKN = 65
PV_BQN = 66
PV_BKN = 67
PV_CQN = 68
PV_CKN = 69
PV_DQNORM = 70
PV_DKVNORM = 74
PV_DQN = 76
PV_DKN = 77
PV_SUBLN = 78
PV_SINK = 79
PV_LQK = 87
PV_INV96 = 91
NPV = 92

C_ONES = 0
C_BD64 = 128
C_BD96 = 256
C_R64 = 384
C_R96 = 512
C_MASKL = 640
C_MASKR = 768
NCB = 896


_FRONTIER = {}


class Buf:
    __slots__ = ("w", "r", "name")

    def __init__(self, name=""):
        self.w = None
        self.r = dict(_FRONTIER)
        self.name = name


class EngQ:
    def __init__(self, nc, name, h, is_pe=False):
        self.h = h
        self.sem = nc.alloc_semaphore("s_" + name)
        self.n = 0
        self.waited = {}
        self.is_pe = is_pe
        self.name = name


class DSem:
    def __init__(self, nc, name):
        self.sem = nc.alloc_semaphore(name)
        self.n = 0


class KB:
    def __init__(self, layers=(0, 1), first=True, final=True, dumps=()):
        global _FRONTIER
        _FRONTIER = {}
        self.layers = layers
        self.dumps = set(dumps)
        nc = self.nc = bass.Bass("TRN2", target_bir_lowering=False)
        self.pe = EngQ(nc, "pe", nc.tensor, True)
        self.act = EngQ(nc, "act", nc.scalar)
        self.dve = EngQ(nc, "dve", nc.vector)
        self.pool = EngQ(nc, "pool", nc.gpsimd)
        self.sp = EngQ(nc, "sp", nc.sync)
        self.engs = [self.pe, self.act, self.dve, self.pool, self.sp]
        self.dsems = []
        self.dump_outs = []
        dt = lambda n, s, k="ExternalInput": nc.dram_tensor(n, list(s), F32, kind=k).ap()
        self.xT = dt("xT", [D, L])
        self.ctxT = dt("ctxT", [D, CTX])
        self.cvec = dt("cvec", [128, 16])
        self.cstf = dt("cstf", [128, NCB])
        self.pidx_d = dt("pidx", [128, 128])
        self.tabs = dt("tabs", [4, 128, L])
        self.pv = dt("pv", [2, 128, NPV])
        self.w_mod = dt("w_mod", [2, D, 6 * D])
        self.w_in = dt("w_in", [2, D, IN_COLS])
        self.d_w_uq = dt("d_w_uq", [2, 512, 768])
        self.d_w_ukv = dt("d_w_ukv", [2, 256, 1024])
        self.w_br = dt("w_br", [2, 4, 512, D])
        self.w_out = dt("w_out", [2, D, D])
        if 0 in layers:
            self.ffg = dt("ff_w_gate", [1, D, D_FF])
            self.ffu = dt("ff_w_up", [1, D, D_FF])
            self.ffd = dt("ff_w_down", [1, D_FF, D])
        if 1 in layers:
            self.moe_r = dt("moe_router", [1, D, 8])
            self.moe_w1 = dt("moe_w1", [1, 8, D, EXPERT_FF])
            self.moe_w3 = dt("moe_w3", [1, 8, D, EXPERT_FF])
            self.moe_w2 = dt("moe_w2", [1, 8, EXPERT_FF, D])
        self.outT = dt("outT", [D, L], "ExternalOutput")
        self.ctxo = dt("ctxo", [D, CTX], "ExternalOutput")
        self.outb = [[Buf(f"out{dc}_{tc}") for tc in range(5)] for dc in range(8)]
        self.ps = [nc.alloc_psum_tensor(f"ps{i}", [128, 512], F32) for i in range(8)]
        self.psb = [Buf(f"ps{i}") for i in range(8)]
        self.ps_ctr = {}
        rem = nc.sbuf_bytes_remaining
        self.arena_size = (rem - 256) // 64 * 64
        arena = nc.alloc_sbuf_tensor("arena", [128, self.arena_size // 4], F32)
        self.arena_base = nc.lookup_mloc(arena).addr
        self.sp_off = 0
        self.uid = 0
        self.build(first, final)

    def alloc(self, shape, dtype, name="t"):
        nb = int(np.prod(shape[1:])) * (4 if dtype == F32 else 2)
        nb = (nb + 63) // 64 * 64
        off = self.sp_off
        assert off + nb <= self.arena_size, f"SBUF arena overflow: {name} {off}+{nb} > {self.arena_size}"
        self.sp_off += nb
        self.sp_max = max(getattr(self, "sp_max", 0), self.sp_off)
        self.uid += 1
        return self.nc.alloc_sbuf_tensor_at(f"{name}_{self.uid}", list(shape), dtype, offset=self.arena_base + off)

    def mark(self):
        return self.sp_off

    def release(self, m):
        self.sp_off = m

    def dsem(self, name):
        d = DSem(self.nc, name)
        self.dsems.append(d)
        return d

    def issue(self, q, fn, reads=(), writes=(), dsem=None):
        deps = {}

        def add(s, v):
            if deps.get(s, 0) < v:
                deps[s] = v

        for b in reads:
            if b.w is not None:
                add(*b.w)
        for b in writes:
            if b.w is not None:
                add(*b.w)
            for s, v in b.r.items():
                add(s, v)
        if dsem is not None and dsem.n > 0:
            add(dsem.sem, dsem.n)
        need = []
        for s, v in deps.items():
            if q.is_pe and s is q.sem:
                continue
            if q.waited.get(s, 0) >= v:
                continue
            need.append((s, v))
            q.waited[s] = v
        for s, v in need[:-1]:
            q.h.wait_ge(s, v)
        inst = fn()
        if need:
            inst._wait_ge(*need[-1])
        if dsem is None:
            q.n += 1
            inst.then_inc(q.sem, 1)
            ev = (q.sem, q.n)
        else:
            dsem.n += 16
            inst.then_inc(dsem.sem, 16)
            ev = (dsem.sem, dsem.n)
        for b in reads:
            if b.r.get(ev[0], 0) < ev[1]:
                b.r[ev[0]] = ev[1]
        for b in writes:
            b.w = ev
            b.r = {}
        return ev

    def soft_barrier(self):
        global _FRONTIER
        _FRONTIER = {s: v for s, v in ([(e.sem, e.n) for e in self.engs] + [(d.sem, d.n) for d in self.dsems]) if v > 0}

    def barrier(self):
        allsem = [(e.sem, e.n) for e in self.engs] + [(d.sem, d.n) for d in self.dsems]
        for q in self.engs:
            for s, v in allsem:
                if v > 0 and q.waited.get(s, 0) < v and not (q.is_pe and s is q.sem):
                    q.h.wait_ge(s, v)
                    q.waited[s] = v

    def mark_phase(self, name):
        if not hasattr(self, "phases"):
            self.phases = []
        self.phases.append((name, self.pe.n))

    def sel_ap(self, e):
        h, b = self.selbufs[self.sel_i % 2]
        self.sel_i += 1
        self.ts(h[:, :], self.pidx[:, :], float(e), None, ALU.is_equal, None, (self.pidxb,), (b,))
        return h, b

    def newps(self, lo=0, hi=8):
        key = (lo, hi)
        i = self.ps_ctr.get(key, 0)
        self.ps_ctr[key] = i + 1
        b = lo + i % (hi - lo)
        return self.ps[b], self.psb[b]

    def mm(self, out, lhsT, rhs, start, stop, reads, writes):
        return self.issue(self.pe, lambda: self.nc.tensor.matmul(out, lhsT=lhsT, rhs=rhs, start=start, stop=stop), reads, writes)

    def actf(self, out, in_, func, reads, writes, scale=1.0, bias=0.0):
        return self.issue(self.act, lambda: self.nc.scalar.activation(out=out, in_=in_, func=func, bias=bias, scale=scale), reads, writes)

    def tt(self, out, in0, in1, op, reads, writes):
        return self.issue(self.dve, lambda: self.nc.vector.tensor_tensor(out=out, in0=in0, in1=in1, op=op), reads, writes)

    def ts(self, out, in0, s1, s2, op0, op1, reads, writes):
        if op1 is None:
            return self.issue(self.dve, lambda: self.nc.vector.tensor_scalar(out=out, in0=in0, scalar1=s1, scalar2=None, op0=op0), reads, writes)
        return self.issue(self.dve, lambda: self.nc.vector.tensor_scalar(out=out, in0=in0, scalar1=s1, scalar2=s2, op0=op0, op1=op1), reads, writes)

    def stt(self, out, in0, scalar, in1, op0, op1, reads, writes):
        return self.issue(self.dve, lambda: self.nc.vector.scalar_tensor_tensor(out=out, in0=in0, scalar=scalar, in1=in1, op0=op0, op1=op1), reads, writes)

    def vcopy(self, out, in_, reads, writes):
        return self.issue(self.dve, lambda: self.nc.vector.tensor_copy(out=out, in_=in_), reads, writes)

    def recip(self, out, in_, reads, writes, exact=True):
        return self.issue(self.dve, lambda: self.nc.vector.reciprocal(out=out, in_=in_), reads, writes)

    def act_pow(self, out, in_, power, reads, writes, scale=1.0, bias=0.0):
        self.actf(out, in_, AF.Ln, reads, writes, scale=scale, bias=bias)
        return self.actf(out, out, AF.Exp, writes, writes, scale=power)

    def vmemset(self, ap, val, writes):
        return self.issue(self.dve, lambda: self.nc.vector.memset(ap, val), (), writes)

    def dma(self, q, out, in_, reads, writes, dsem):
        return self.issue(q, lambda: q.h.dma_start(out=out, in_=in_), reads, writes, dsem)

    def plsem(self):
        d = self.pl_sems[self.pl_i % len(self.pl_sems)]
        self.pl_i += 1
        return d

    def ldsem(self):
        d = self.ld_sems[self.ld_i % len(self.ld_sems)]
        self.ld_i += 1
        return d

    def tmp(self, kind):
        lst = self.tmps[kind]
        i = self.tmp_i.get(kind, 0)
        self.tmp_i[kind] = i + 1
        return lst[i % len(lst)]

    def wload(self, dram_ap, shape, split=1):
        slot_h, slot_bs, slot_ds = self.wslots[self.w_i % len(self.wslots)]
        self.w_i += 1
        n = int(np.prod(shape[1:]))
        assert n <= self.WSLOT
        if len(shape) == 3:
            view = slot_h[0:shape[0], 0:n].rearrange("p (a b) -> p a b", a=shape[1])
        else:
            view = slot_h[0:shape[0], 0:n]
        if split == 1:
            self.dma(self.pool, view, dram_ap, (), tuple(slot_bs), slot_ds[0])
            return view, slot_bs[0]
        h = shape[-1] // 2
        self.dma(self.pool, view[:, :, 0:h], dram_ap[:, :, 0:h], (), (slot_bs[0],), slot_ds[0])
        self.dma(self.pool, view[:, :, h:], dram_ap[:, :, h:], (), (slot_bs[1],), slot_ds[1])
        return view, list(slot_bs)

    def dump(self, name, ap, shape, bufs):
        if name not in self.dumps:
            return
        o = self.nc.dram_tensor("dbg_" + name, list(shape), F32 if ap.dtype == F32 else BF16, kind="ExternalOutput").ap()
        self.dump_outs.append("dbg_" + name)
        self.dma(self.sp, o, ap, bufs, (), self.ldsem())

    def build(self, first, final):
        nc = self.nc
        A = self.alloc
        self.cb = A([128, NCB], BF16, "cb")
        self.cbb = Buf("cb")
        self.pidx = A([128, 128], F32, "pidx")
        self.pidxb = Buf("pidx")
        self.selbufs = [(A([128, 128], F32, "selbuf"), Buf("selbuf")) for _ in range(2)]
        self.sel_i = 0
        self.pvt = A([128, 2 * NPV], F32, "pv")
        self.pvb = Buf("pv")
        self.cv = A([128, 16], F32, "cv")
        self.cvb = Buf("cv")
        self.modv = A([128, 96], F32, "modv")
        self.modb = Buf("modv")
        self.drv = A([128, 64], F32, "drv")
        self.drvb = Buf("drv")
        self.latc = A([128, 8, CTX], F32, "latc")
        self.hT = A([128, 8, NT], BF16, "hT")
        self.hTb = [[Buf(f"hT{dc}_{tc}") for tc in range(5)] for dc in range(8)]
        self.WSLOT = 4096
        self.wslots = [(A([128, self.WSLOT], BF16, f"w{i}"), [Buf(f"w{i}a"), Buf(f"w{i}b")], [self.dsem(f"wd{i}a"), self.dsem(f"wd{i}b")]) for i in range(3)]
        self.w_i = 0
        self.ld_sems = [self.dsem(f"ld{i}") for i in range(8)]
        self.ld_i = 0
        self.pl_sems = [self.dsem(f"pl{i}") for i in range(2)]
        self.pl_i = 0
        self.tmps = {
            "f32": [(A([128, 512], F32, "tf"), Buf("tf")) for _ in range(5)],
            "bf": [(A([128, 512], BF16, "tb"), Buf("tb")) for _ in range(6)],
            "pT": [(A([128, 512], BF16, "pT"), Buf("pT")) for _ in range(4)],
            "rs": [(A([128, 512], F32, "rs"), Buf("rs")) for _ in range(2)],
        }
        self.tmp_i = {}
        self.phase_base = self.mark()
        self.dma(self.pool, self.cb[:, :], self.cstf, (), (self.cbb,), self.plsem())
        self.dma(self.sp, self.pidx[:, :], self.pidx_d, (), (self.pidxb,), self.ldsem())
        self.dma(self.sp, self.pvt[:, :].rearrange("p (l n) -> p l n", l=2), self.pv.rearrange("l p n -> p l n"), (), (self.pvb,), self.ldsem())
        self.dma(self.sp, self.cv[:, :], self.cvec, (), (self.cvb,), self.ldsem())
        self.lat = A([128, 8, L], F32, "lat")
        self.latb = [[Buf(f"lat{dc}_{tc}") for tc in range(5)] for dc in range(8)]
        self.lat_end = self.mark()
        xv = self.xT.rearrange("(k p) t -> p k t", p=128)
        cxv = self.ctxT.rearrange("(k p) t -> p k t", p=128)
        for tc, (t0, n) in enumerate(TCS):
            for dc in range(8):
                if tc < 4:
                    self.dma(self.sp, self.lat[:, dc, t0:t0 + n], xv[:, dc, t0:t0 + n], (), (self.latb[dc][tc],), self.ldsem())
                else:
                    self.dma(self.sp, self.latc[:, dc, :], cxv[:, dc, :], (), (self.latb[dc][tc],), self.ldsem())
        for l in self.layers:
            last = (l == 1)
            self.mark_phase(f'mod{l}')
            self.modulation(l)
            self.mark_phase(f'adaln0_{l}')
            self.adaln(l, which=0, last=last)
            self.store_lat()
            self.soft_barrier()
            self.release(self.phase_base)
            self.mark_phase(f'mixer{l}')
            self.mixer_phase(l, last)
            self.mark_phase(f'wout{l}')
            self.soft_barrier()
            self.wout_phase(l, last)
            self.soft_barrier()
            self.mark_phase(f'adaln1_{l}')
            self.adaln(l, which=1, last=last)
            self.soft_barrier()
            self.mark_phase(f'ffn{l}')
            self.ffn_phase(l, last)
            self.mark_phase(f'end{l}')
            self.soft_barrier()
        self.store_lat(final=True)
        for d in self.dsems:
            if d.n > 0 and self.sp.waited.get(d.sem, 0) < d.n:
                self.sp.h.wait_ge(d.sem, d.n)
                self.sp.waited[d.sem] = d.n

    def lat_ap(self, dc, tc):
        t0, n = TCS[tc]
        return self.lat[:, dc, t0:t0 + n] if tc < 4 else self.latc[:, dc, :]

    def pvc(self, l, col, rows=128, n=1):
        return self.pvt[0:rows, l * NPV + col:l * NPV + col + n]

    def store_lat(self, final=False):
        ov = self.outT.rearrange("(k p) t -> p k t", p=128)
        cov = self.ctxo.rearrange("(k p) t -> p k t", p=128)
        for tc, (t0, n) in enumerate(TCS):
            for dc in range(8):
                if tc < 4:
                    self.dma(self.sp, ov[:, dc, t0:t0 + n], self.lat[:, dc, t0:t0 + n], (self.latb[dc][tc],), (self.outb[dc][tc],), self.ldsem())
                elif final:
                    self.dma(self.sp, cov[:, dc, :], self.latc[:, dc, :], (self.latb[dc][tc],), (self.outb[dc][tc],), self.ldsem())

    def modulation(self, l):
        sc32, sc32b = self.tmp("f32")
        scb, scbb = self.tmp("bf")
        self.actf(sc32[:, 0:16], self.cv[:, :], AF.Silu, (self.cvb,), (sc32b,))
        self.vcopy(scb[:, 0:16].rearrange("p (k j) -> p k j", j=2), sc32[:, 0:16].rearrange("p (j k) -> p k j", j=2), (sc32b,), (scbb,))
        wv = self.w_mod[l].rearrange("(k p) c -> p k c", p=128)
        ps, psb = self.newps()
        for cb_ in range(12):
            wt, wb = self.wload(wv[:, :, cb_ * 512:(cb_ + 1) * 512], [128, 8, 512])
            for jj in range(4):
                j = cb_ * 4 + jj
                for k in range(8):
                    self.mm(ps[:, 2 * j:2 * j + 2], wt[:, k, jj * 128:(jj + 1) * 128], scb[:, 2 * k:2 * k + 2], k == 0, k == 7, (wb, scbb), (psb,))
        bm = self.pvc(l, PV_BMOD, n=48)
        for j2 in range(2):
            self.tt(self.modv[:, :].rearrange("p (j t) -> p j t", t=2)[:, :, j2], ps[:, 0:96].rearrange("p (j t) -> p j t", t=2)[:, :, j2], bm, ALU.add, (psb, self.pvb), (self.modb,))
        mv = self.modv[:, :].rearrange("p (j t) -> p j t", t=2)
        dv = self.drv[:, 0:32].rearrange("p (a d t) -> p a d t", a=2, t=2)
        for j2 in range(2):
            for a, (mi, gcol) in enumerate(((1, PV_MIXN), (4, PV_FFNN))):
                self.stt(dv[:, a, :, j2], mv[:, mi * 8:mi * 8 + 8, j2], 1.0, self.pvc(l, gcol, n=8), ALU.add, ALU.mult, (self.modb, self.pvb), (self.drvb,))
        self.actf(self.drv[:, 32:40], self.pvc(l, PV_SINK, n=8), AF.Exp, (self.pvb,), (self.drvb,))
        self.tt(self.drv[:, 40:42], self.pvc(l, PV_LQK, n=4).rearrange("p (a b) -> p a b", b=2)[:, :, 0], self.pvc(l, PV_LQK, n=4).rearrange("p (a b) -> p a b", b=2)[:, :, 1], ALU.mult, (self.pvb,), (self.drvb,))
        ps2, ps2b = self.newps()
        self.vmemset(self.drv[:, 48:64], 0.0, (self.drvb,))
        onesf, onesfb = self.tmp("f32")
        self.vmemset(onesf[:, 0:128], 1.0, (onesfb,))
        self.mm(ps2[:, 0:2], onesf[:, 0:128], self.drv[:, 40:42], True, True, (onesfb, self.drvb), (ps2b,))
        self.actf(self.drv[:, 42:44], ps2[:, 0:2], AF.Exp, (ps2b,), (self.drvb,))
        lam_init = 0.8 - 0.6 * math.exp(-0.3 * l)
        self.tt(self.drv[:, 44:45], self.drv[:, 43:44], self.drv[:, 42:43], ALU.subtract, (self.drvb,), (self.drvb,))
        self.ts(self.drv[:, 44:45], self.drv[:, 44:45], -lam_init, None, ALU.add, None, (self.drvb,), (self.drvb,))
        self.ts(self.drv[:, 45:46], self.pvc(l, PV_SUBLN), 1.0 - lam_init, None, ALU.mult, None, (self.pvb,), (self.drvb,))
        self.dump(f"modv{l}", self.modv[:, :], [128, 96], (self.modb,))
        self.dump(f"drv{l}", self.drv[:, :], [128, 64], (self.drvb,))

    def mcol(self, i, dc, j2):
        c = (i * 8 + dc) * 2 + j2
        return self.modv[:, c:c + 1]

    def adaln(self, l, which, last):
        cb = self.cb
        moe = (which == 1 and l == 1)
        if moe:
            m = self.mark()
            self.sp_off = self.ffn_base
            self.wrpad = self.alloc([128, 8, 128], F32, "wrpad")
            self.wrpadb = Buf("wrpad")
            self.h32 = [(self.alloc([128, 512], F32, "h32"), Buf("h32")) for _ in range(3)]
            self.release(m)
            self.vmemset(self.wrpad[:, :, :], 0.0, (self.wrpadb,))
            self.dma(self.sp, self.wrpad[:, :, 0:8], self.moe_r[0].rearrange("(k p) e -> p k e", p=128), (), (self.wrpadb,), self.ldsem())
        for tc, (t0, n) in enumerate(TCS):
            if tc == 4 and (which == 1 and last):
                continue
            j2 = 1 if tc == 4 else 0
            ss, ssb = self.newps()
            for dc in range(8):
                sq, sqb = self.tmp("bf")
                self.actf(sq[:, 0:n], self.lat_ap(dc, tc), AF.Square, (self.latb[dc][tc],), (sqb,))
                self.mm(ss[:, 0:n], cb[:, C_ONES:C_ONES + 128], sq[:, 0:n], dc == 0, dc == 7, (self.cbb, sqb), (ssb,))
            rt, rtb = self.tmp("f32")
            self.actf(rt[:, 0:n], ss[:, 0:n], AF.Sqrt, (ssb,), (rtb,), scale=1.0 / D, bias=EPS)
            rs, rsb = self.tmp("rs")
            self.recip(rs[:, 0:n], rt[:, 0:n], (rtb,), (rsb,), exact=True)
            if moe:
                lp, lpb = self.newps()
            for dc in range(8):
                acol = (which * 8 + dc) * 2 + j2
                t32, t32b = self.tmp("f32")
                self.stt(t32[:, 0:n], self.lat_ap(dc, tc), self.drv[:, acol:acol + 1], rs[:, 0:n], ALU.mult, ALU.mult, (self.latb[dc][tc], self.drvb, rsb), (t32b,))
                bias = self.mcol(3 if which else 0, dc, j2)
                if not moe:
                    self.actf(self.hT[:, dc, t0:t0 + n], t32[:, 0:n], AF.Identity, (t32b, self.modb), (self.hTb[dc][tc],), bias=bias)
                else:
                    h32, h32b = self.h32[dc % 3]
                    self.actf(h32[:, 0:n], t32[:, 0:n], AF.Identity, (t32b, self.modb), (h32b,), bias=bias)
                    self.vcopy(self.hT[:, dc, t0:t0 + n], h32[:, 0:n], (h32b,), (self.hTb[dc][tc],))
                    self.mm(lp[:, 0:n], self.wrpad[:, dc, :], h32[:, 0:n], dc == 0, dc == 7, (self.wrpadb, h32b), (lpb,))
            if moe:
                self.actf(self.LT[:, t0:t0 + n], lp[:, 0:n], AF.Copy, (lpb,), (self.LTb[tc],))
        nm = "hmix" if which == 0 else "hffn"
        self.dump(f"{nm}{l}", self.hT[:, :, :], [128, 8, NT], [b for r in self.hTb for b in r])

    def headnorm(self, ps, psb, P, n, bdcol, inv, gain, rope, t0, dest, destb, extra_reads=()):
        self.headnorm_multi([dict(ps=ps, psb=psb, P=P, n=n, bdcol=bdcol, inv=inv, gain=gain, rope=rope, t0=t0, dest=dest, destb=destb)])

    def headnorm_multi(self, tiles):
        cb = self.cb
        for T in tiles:
            P, n = T["P"], T["n"]
            T["sq"] = self.tmp("bf")
            self.actf(T["sq"][0][0:P, 0:n], T["ps"][0:P, 0:n], AF.Square, (T["psb"],), (T["sq"][1],))
        for T in tiles:
            P, n = T["P"], T["n"]
            T["ss"] = self.newps()
            self.mm(T["ss"][0][0:P, 0:n], cb[0:P, T["bdcol"]:T["bdcol"] + P], T["sq"][0][0:P, 0:n], True, True, (self.cbb, T["sq"][1]), (T["ss"][1],))
        for T in tiles:
            P, n = T["P"], T["n"]
            T["rt"] = self.tmp("f32")
            self.actf(T["rt"][0][0:P, 0:n], T["ss"][0][0:P, 0:n], AF.Ln, (T["ss"][1], self.pvb), (T["rt"][1],), scale=T["inv"], bias=EPS)
        for T in tiles:
            P, n = T["P"], T["n"]
            self.actf(T["rt"][0][0:P, 0:n], T["rt"][0][0:P, 0:n], AF.Exp, (T["rt"][1],), (T["rt"][1],), scale=-0.5)
        for T in tiles:
            P, n = T["P"], T["n"]
            if T["rope"] is None:
                self.stt(T["dest"], T["ps"][0:P, 0:n], T["gain"], T["rt"][0][0:P, 0:n], ALU.mult, ALU.mult, (T["psb"], T["rt"][1], self.pvb), (T["destb"],))
            else:
                T["qn"] = self.tmp("bf")
                self.stt(T["qn"][0][0:P, 0:n], T["ps"][0:P, 0:n], T["gain"], T["rt"][0][0:P, 0:n], ALU.mult, ALU.mult, (T["psb"], T["rt"][1], self.pvb), (T["qn"][1],))
        for T in tiles:
            if T["rope"] is None:
                continue
            P, n = T["P"], T["n"]
            rcol = T["rope"][0]
            T["rq"] = self.newps()
            self.mm(T["rq"][0][0:P, 0:n], cb[0:P, rcol:rcol + P], T["qn"][0][0:P, 0:n], True, True, (self.cbb, T["qn"][1]), (T["rq"][1],))
        for T in tiles:
            if T["rope"] is None:
                continue
            P, n, t0 = T["P"], T["n"], T["t0"]
            _, cosT, sinT = T["rope"]
            self.tt(T["rt"][0][0:P, 0:n], T["qn"][0][0:P, 0:n], cosT[0:P, t0:t0 + n], ALU.mult, (T["qn"][1], self.tabb), (T["rt"][1],))
        for T in tiles:
            if T["rope"] is None:
                continue
            P, n, t0 = T["P"], T["n"], T["t0"]
            _, cosT, sinT = T["rope"]
            T["t2"] = self.tmp("f32")
            self.tt(T["t2"][0][0:P, 0:n], T["rq"][0][0:P, 0:n], sinT[0:P, t0:t0 + n], ALU.mult, (T["rq"][1], self.tabb), (T["t2"][1],))
        for T in tiles:
            P, n = T["P"], T["n"]
            if T["rope"] is not None:
                self.tt(T["dest"], T["rt"][0][0:P, 0:n], T["t2"][0][0:P, 0:n], ALU.add, (T["rt"][1], T["t2"][1]), (T["destb"],))
            if T.get("post") is not None:
                T["post"]()

    def proj_fm(self, wt, wb, col0, M, tc, kch=8, rhs=None):
        t0, n = TCS[tc]
        ps, psb = self.newps()
        for k in range(kch):
            if rhs is None:
                r, rb = self.hT[:, k, t0:t0 + n], self.hTb[k][tc]
            else:
                r, rb = rhs(k)
            self.mm(ps[0:M, 0:n], wt[:, k, col0:col0 + M], r, k == 0, k == kch - 1, (wb, rb), (psb,))
        return ps, psb

    def load_tabs(self, i0):
        for i in range(2):
            self.dma(self.pool, self.tab[i][:, :], self.tabs[i0 + i], (), (self.tabb,), self.plsem())

    def attn_stream(self, jobs, LA=3):
        seq = [(ji, si) for ji, jb in enumerate(jobs) for si in range(len(jb["kts"]))]
        pend = {}
        accs = {}
        nit = len(seq)
        for step in range(nit + LA):
            if step < nit:
                ji, si = seq[step]
                jb = jobs[ji]
                j, c0, c1 = jb["kts"][si]
                P = jb["P"]
                S, Sb = self.newps(0, 4)
                ktc = j // 4 if j < 16 else 4
                self.mm(S[:, c0:c1], jb["KT"][0:P, j * 128:(j + 1) * 128], jb["qap"][0:P, c0:c1], True, True, (jb["Kb"][ktc], jb["qb"]), (Sb,))
                pT, pTb = self.tmp("pT")
                self.actf(pT[:, c0:c1], S[:, c0:c1], AF.Exp, (Sb,), (pTb,), scale=jb["scale"])
                if jb.get("masks") is not None:
                    for (m0, m1, mcol) in jb["masks"](j, c0, c1):
                        self.tt(pT[:, m0:m1], pT[:, m0:m1], self.cb[:, mcol:mcol + 128], ALU.mult, (pTb, self.cbb), (pTb,))
                pend[step] = (pT, pTb)
            idx = step - LA
            if idx >= 0:
                ji, si = seq[idx]
                jb = jobs[ji]
                nk = len(jb["kts"])
                if si == 0:
                    accs[ji] = [self.newps(4, 8) for _ in range(jb["nacc"])]
                j, c0, c1 = jb["kts"][si]
                pT, pTb = pend.pop(idx)
                for (acc, accb), lhsT in zip(accs[ji], jb["vfun"](j)):
                    self.mm(acc[:, c0:c1], lhsT, pT[:, c0:c1], si == 0, si == nk - 1, (jb["vb_of"](j), pTb), (accb,))
                if si == nk - 1:
                    jb["fin"](accs.pop(ji))

    def norm_out(self, acc, accb, half, n, dest, destb, sinkcol=None):
        o0 = half * 64
        s0 = (1 - half) * 64
        r2, r2b = self.tmp("f32")
        if sinkcol is not None:
            self.act_pow(r2[o0:o0 + 64, 0:n], acc[s0:s0 + 64, 0:n], -1.0, (accb, self.drvb), (r2b,), bias=self.drv[s0:s0 + 64, sinkcol:sinkcol + 1])
        else:
            self.act_pow(r2[o0:o0 + 64, 0:n], acc[s0:s0 + 64, 0:n], -1.0, (accb,), (r2b,))
        self.tt(dest, acc[o0:o0 + 64, 0:n], r2[o0:o0 + 64, 0:n], ALU.mult, (accb, r2b), (destb,))

    def merge_preload(self, l, n_br, ks, hf):
        wbv = self.w_br[l, n_br].rearrange("(k p) d -> p k d", p=128)
        wgv = self.w_in[l].rearrange("(k p) c -> p k c", p=128)
        g0 = GATE0 + n_br * D
        wbt, wbb = self.wload(wbv[:, ks[0]:ks[0] + len(ks), hf * 512:(hf + 1) * 512], [128, len(ks), 512])
        wgt, wgb = self.wload(wgv[:, :, g0 + hf * 512:g0 + (hf + 1) * 512], [128, 8, 512])
        return wbt, wbb, wgt, wgb

    def merge(self, l, n_br, ks, brh, brbufs, tcs, first, pre=None):
        for hf in range(2):
            if hf == 0 and pre is not None:
                wbt, wbb, wgt, wgb = pre
            else:
                wbt, wbb, wgt, wgb = self.merge_preload(l, n_br, ks, hf)
            for tc in tcs:
                t0, n = TCS[tc]
                for d4 in range(4):
                    dc = hf * 4 + d4
                    yp, ypb = self.newps()
                    for i in range(len(ks)):
                        self.mm(yp[:, 0:n], wbt[:, i, d4 * 128:(d4 + 1) * 128], brh[i][:, t0:t0 + n], i == 0, i == len(ks) - 1, (wbb, brbufs[i][tc]), (ypb,))
                    gp, gpb = self.proj_fm(wgt, wgb, d4 * 128, 128, tc)
                    sg, sgb = self.tmp("f32")
                    self.actf(sg[:, 0:n], gp[:, 0:n], AF.Sigmoid, (gpb,), (sgb,))
                    zap = self.z[:, dc, t0:t0 + n]
                    if first:
                        self.tt(zap, yp[:, 0:n], sg[:, 0:n], ALU.mult, (ypb, sgb), (self.zb[dc][tc],))
                    else:
                        t, tb = self.tmp("f32")
                        self.tt(t[:, 0:n], yp[:, 0:n], sg[:, 0:n], ALU.mult, (ypb, sgb), (tb,))
                        self.tt(zap, zap, t[:, 0:n], ALU.add, (tb, self.zb[dc][tc]), (self.zb[dc][tc],))

    def mixer_phase(self, l, last):
        A = self.alloc
        cb = self.cb
        self.z = A([128, 8, NT], BF16, "z")
        self.zb = [[Buf(f"z{dc}_{tc}") for tc in range(5)] for dc in range(8)]
        self.mix_base = self.mark()
        self.tab = [A([128, L], BF16, f"tab{i}") for i in range(2)]
        self.tabb = Buf("tab")
        qtcs = [tc for tc in range(5) if not (last and tc == 4)]
        wv = self.w_in[l].rearrange("(k p) c -> p k c", p=128)
        inv64 = 1.0 / 64
        for mi, (qoff, koff, voff, qg, kg, windowed) in enumerate(((0, 2048, 2176, PV_AQN, PV_AKN, True), (512, 2304, 2432, PV_BQN, PV_BKN, False))):
            m0 = self.mark()
            if mi == 0:
                self.load_tabs(0)
            KT = A([128, NT], BF16, "KT")
            Kb = [Buf(f"K{tc}") for tc in range(5)]
            VA = [[A([128, 18, 128], BF16, "VA") for par in range(2)] for kv in range(2)]
            Vb = [Buf(f"V{tc}") for tc in range(5)]
            qpad = [A([128, 512], BF16, "qpad") for h in range(8)]
            qpb = [Buf(f"qp{h}") for h in range(8)]
            br = [A([128, NT], BF16, "br") for i in range(4)]
            brb = [[Buf(f"br{i}_{tc}") for tc in range(5)] for i in range(4)]
            for h in range(8):
                self.vmemset(qpad[h][:, :], 0.0, (qpb[h],))
            for kv in range(2):
                for par in range(2):
                    self.vmemset(VA[kv][par][:, :, :], 1.0, (Vb[0], Vb[1], Vb[2], Vb[3], Vb[4]))
            self.mark_phase(f'  AB{mi}_kv')
            wk, wkb = self.wload(wv[:, :, koff:koff + 128], [128, 8, 128])
            wvv, wvb = self.wload(wv[:, :, voff:voff + 128], [128, 8, 128])
            for tcg in ((0, 1), (2, 3), (4,)):
                tiles = []
                for tc in tcg:
                    t0, n = TCS[tc]
                    ps, psb = self.proj_fm(wk, wkb, 0, 128, tc)
                    rope = (C_R64, self.tab[0], self.tab[1]) if tc < 4 else None
                    tiles.append(dict(ps=ps, psb=psb, P=128, n=n, bdcol=C_BD64, inv=inv64, gain=self.pvc(l, kg), rope=rope, t0=t0, dest=KT[:, t0:t0 + n], destb=Kb[tc]))
                self.headnorm_multi(tiles)
            for tc, (t0, n) in enumerate(TCS):
                for jt in range(n // 128):
                    j = (t0 // 128) + jt
                    vp, vpb = self.newps()
                    for k in range(8):
                        self.mm(vp[:, 0:128], self.hT[:, k, j * 128:(j + 1) * 128], wvv[:, k, :], k == 0, k == 7, (self.hTb[k][tc], wvb), (vpb,))
                    for kv in range(2):
                        self.vcopy(VA[kv][0][:, j, 0:64], vp[:, kv * 64:(kv + 1) * 64], (vpb,), (Vb[tc],))
                        self.actf(VA[kv][1][:, j, 64:128], vp[:, kv * 64:(kv + 1) * 64], AF.Copy, (vpb,), (Vb[tc],))
            if mi == 0:
                self.dump(f"ka{l}", KT[:, :], [128, NT], Kb)
                self.dump(f"va{l}", VA[0][0][:, :, :], [128, 18, 128], Vb)
            nxt = self.wload(wv[:, :, qoff:qoff + 512], [128, 8, 512])
            mpre = None
            for tc in qtcs:
                t0, n = TCS[tc]
                self.mark_phase(f'  AB{mi}_q{tc}')
                wq, wqb = nxt
                rope = (C_R64, self.tab[0], self.tab[1]) if tc < 4 else None
                for ig in ((0, 1), (2, 3)):
                    tiles = []
                    for i in ig:
                        qt, qtb = self.tmp("bf")

                        def post(i=i, qt=qt, qtb=qtb, n=n):
                            for hh in range(2):
                                h = 2 * i + hh
                                kvh = h // 4
                                self.vcopy(qpad[h][kvh * 64:(kvh + 1) * 64, 0:n], qt[hh * 64:(hh + 1) * 64, 0:n], (qtb,), (qpb[h],))
                        ps, psb = self.proj_fm(wq, wqb, i * 128, 128, tc)
                        tiles.append(dict(ps=ps, psb=psb, P=128, n=n, bdcol=C_BD64, inv=inv64, gain=self.pvc(l, qg), rope=rope, t0=t0, dest=qt[:, 0:n], destb=qtb, post=post))
                    self.headnorm_multi(tiles)
                if mi == 0 and tc == 0:
                    self.dump(f"qa{l}", qpad[1][:, :], [128, 512], (qpb[1],))
                if tc != qtcs[-1]:
                    nxt = self.wload(wv[:, :, qoff:qoff + 512], [128, 8, 512])
                else:
                    mpre = self.merge_preload(l, mi, [0, 1, 2, 3], 0)
                self.mark_phase(f'  AB{mi}_att{tc}')
                jobs = []
                for h in range(8):
                    kvh = h // 4
                    par = h % 2
                    masks = None
                    if tc == 4:
                        kts = [(16, 0, n), (17, 0, n)]
                    elif not windowed:
                        kts = [(j, 0, n) for j in range(18)]
                    else:
                        kts = [(16, 0, n), (17, 0, n)]
                        b0 = t0 // 128
                        for j in range(max(0, b0 - 1), min(16, b0 + 5)):
                            qlo = max(j - 1, b0)
                            qhi = min(j + 1, b0 + 3)
                            kts.append((j, (qlo - b0) * 128, (qhi - b0 + 1) * 128))

                        def masks(j, c0, c1, b0=b0):
                            res = []
                            for qb_ in range(b0 + c0 // 128, b0 + c1 // 128):
                                if qb_ == j - 1:
                                    res.append(((qb_ - b0) * 128, (qb_ - b0 + 1) * 128, C_MASKR))
                                elif qb_ == j + 1:
                                    res.append(((qb_ - b0) * 128, (qb_ - b0 + 1) * 128, C_MASKL))
                            return res

                    def fin(accs, h=h, par=par, n=n, t0=t0, tc=tc):
                        acc, accb = accs[0]
                        self.norm_out(acc, accb, par, n, br[h // 2][par * 64:(par + 1) * 64, t0:t0 + n], brb[h // 2][tc],
                                      sinkcol=(32 + h) if windowed else None)
                    jobs.append(dict(qap=qpad[h], qb=qpb[h], KT=KT, Kb=Kb, P=128, kts=kts,
                                     vfun=(lambda j, kvh=kvh, par=par: [VA[kvh][par][:, j, :]]),
                                     vb_of=(lambda j: Vb[j // 4 if j < 16 else 4]), scale=0.125, masks=masks, nacc=1, fin=fin))
                self.attn_stream(jobs)
                if tc == 0:
                    self.dump(f"br{mi}_{l}", br[0][:, 0:512], [128, 512], (brb[0][0],))
            self.mark_phase(f'  AB{mi}_mrg0')
            self.merge(l, mi, [0, 1, 2, 3], br, brb, qtcs, first=(mi == 0), pre=mpre)
            self.soft_barrier()
            self.release(m0)
        self.mark_phase('  C_kv')
        m0 = self.mark()
        KC = [A([128, NT], BF16, "KC") for h in range(4)]
        KCb = [[Buf(f"KC{h}_{tc}") for tc in range(5)] for h in range(4)]
        VC = A([128, 18, 512], BF16, "VC")
        VCb = [Buf(f"VC{tc}") for tc in range(5)]
        qpad = [A([128, 512], BF16, "qpadc") for h in range(8)]
        qpb = [Buf(f"qpc{h}") for h in range(8)]
        br = [A([128, NT], BF16, "brc") for i in range(4)]
        brb = [[Buf(f"brc{i}_{tc}") for tc in range(5)] for i in range(4)]
        for h in range(8):
            self.vmemset(qpad[h][:, :], 0.0, (qpb[h],))
        wk, wkb = self.wload(wv[:, :, 2560:3072], [128, 8, 512])
        for tc, (t0, n) in enumerate(TCS):
            rope = (C_R64, self.tab[0], self.tab[1]) if tc < 4 else None
            for hg in ((0, 1), (2, 3)):
                tiles = []
                for h in hg:
                    ps, psb = self.proj_fm(wk, wkb, h * 128, 128, tc)
                    tiles.append(dict(ps=ps, psb=psb, P=128, n=n, bdcol=C_BD64, inv=inv64, gain=self.pvc(l, PV_CKN), rope=rope, t0=t0, dest=KC[h][:, t0:t0 + n], destb=KCb[h][tc]))
                self.headnorm_multi(tiles)
        wvv, wvb = self.wload(wv[:, :, 3072:3584], [128, 8, 512])
        for tc, (t0, n) in enumerate(TCS):
            for jt in range(n // 128):
                j = (t0 // 128) + jt
                vp, vpb = self.newps()
                for k in range(8):
                    self.mm(vp[:, 0:512], self.hT[:, k, j * 128:(j + 1) * 128], wvv[:, k, :], k == 0, k == 7, (self.hTb[k][tc], wvb), (vpb,))
                self.actf(VC[:, j, :], vp[:, 0:512], AF.Copy, (vpb,), (VCb[tc],))
        nxt = self.wload(wv[:, :, 1024:1536], [128, 8, 512])
        mpre = None
        for tc in qtcs:
            t0, n = TCS[tc]
            self.mark_phase(f'  C_q{tc}')
            wq, wqb = nxt
            rope = (C_R64, self.tab[0], self.tab[1]) if tc < 4 else None
            for hg in ((0, 1), (2, 3)):
                tiles = []
                for h in hg:
                    qt, qtb = self.tmp("bf")

                    def post(h=h, qt=qt, qtb=qtb, n=n):
                        for part in range(2):
                            self.vcopy(qpad[2 * h + part][part * 64:(part + 1) * 64, 0:n], qt[part * 64:(part + 1) * 64, 0:n], (qtb,), (qpb[2 * h + part],))
                    ps, psb = self.proj_fm(wq, wqb, h * 128, 128, tc)
                    tiles.append(dict(ps=ps, psb=psb, P=128, n=n, bdcol=C_BD64, inv=inv64, gain=self.pvc(l, PV_CQN), rope=rope, t0=t0, dest=qt[:, 0:n], destb=qtb, post=post))
                self.headnorm_multi(tiles)
            kts = [(j, 0, n) for j in range(18)] if tc < 4 else [(16, 0, n), (17, 0, n)]
            if tc != qtcs[-1]:
                nxt = self.wload(wv[:, :, 1024:1536], [128, 8, 512])
            else:
                mpre = self.merge_preload(l, 2, [0, 1, 2, 3], 0)
            self.mark_phase(f'  C_att{tc}')
            jobs = []
            held = {}
            for h in range(4):
                for part in range(2):
                    def fin(accs, h=h, part=part, n=n, t0=t0, tc=tc):
                        (o, ob), (s_, sb) = accs
                        r, rb = self.tmp("f32")
                        self.act_pow(r[:, 0:n], s_[:, 0:n], -1.0, (sb,), (rb,))
                        t, tb = self.tmp("f32")
                        self.tt(t[:, 0:n], o[:, 0:n], r[:, 0:n], ALU.mult, (ob, rb), (tb,))
                        if part == 0:
                            held[h] = (t, tb)
                            return
                        t1, t1b = held.pop(h)
                        oc, ocb = self.tmp("f32")
                        self.stt(oc[:, 0:n], t[:, 0:n], self.drv[:, 44:45], t1[:, 0:n], ALU.mult, ALU.add, (t1b, tb, self.drvb), (ocb,))
                        sq, sqb = self.tmp("bf")
                        self.actf(sq[:, 0:n], oc[:, 0:n], AF.Square, (ocb,), (sqb,))
                        ss, ssb = self.newps(0, 4)
                        self.mm(ss[:, 0:n], cb[:, C_ONES:C_ONES + 128], sq[:, 0:n], True, True, (self.cbb, sqb), (ssb,))
                        rs, rsb = self.tmp("f32")
                        self.act_pow(rs[:, 0:n], ss[:, 0:n], -0.5, (ssb,), (rsb,), scale=1.0 / 128, bias=EPS)
                        self.stt(br[h][:, t0:t0 + n], oc[:, 0:n], self.drv[:, 45:46], rs[:, 0:n], ALU.mult, ALU.mult, (ocb, rsb, self.drvb), (brb[h][tc],))
                    jobs.append(dict(qap=qpad[2 * h + part], qb=qpb[2 * h + part], KT=KC[h], Kb=KCb[h], P=128, kts=kts,
                                     vfun=(lambda j, h=h: [VC[:, j, h * 128:(h + 1) * 128], cb[:, C_ONES:C_ONES + 128]]),
                                     vb_of=(lambda j: VCb[j // 4 if j < 16 else 4]), scale=0.125, masks=None, nacc=2, fin=fin))
            self.attn_stream(jobs)
            if tc == 0:
                self.dump(f"br2_{l}", br[0][:, 0:512], [128, 512], (brb[0][0],))
        self.mark_phase('  C_mrg0')
        self.merge(l, 2, [0, 1, 2, 3], br, brb, qtcs, first=False, pre=mpre)
        self.soft_barrier()
        self.release(m0)
        self.mark_phase('  D_ckv')
        self.load_tabs(2)
        m0 = self.mark()
        ckvn = A([128, 2, NT], BF16, "ckvn")
        ckvb = [Buf(f"ckvn{tc}") for tc in range(5)]
        KR = A([128, NT], BF16, "KR")
        KRb = [Buf(f"KR{tc}") for tc in range(5)]
        KD = [A([128, NT], BF16, "KD") for h in range(4)]
        VD = [A([128, 18, 128], BF16, "VD") for h in range(4)]
        qd = [A([128, 512], BF16, "qd") for h in range(4)]
        br = [A([128, NT], BF16, "brd") for i in range(2)]
        cqn = A([128, 4, 512], BF16, "cqn")
        wck, wckb = self.wload(wv[:, :, 3584:3840], [128, 8, 256])
        wkr, wkrb = self.wload(wv[:, :, 3776:3872], [128, 8, 96])
        for tc, (t0, n) in enumerate(TCS):
            raws = []
            ss, ssb = self.newps()
            for c2 in range(2):
                ps, psb = self.proj_fm(wck, wckb, c2 * 128, 128, tc)
                raw, rawb = self.tmp("f32")
                self.actf(raw[:, 0:n], ps[:, 0:n], AF.Copy, (psb,), (rawb,))
                sq, sqb = self.tmp("bf")
                self.actf(sq[:, 0:n], ps[:, 0:n], AF.Square, (psb,), (sqb,))
                self.mm(ss[:, 0:n], cb[:, C_ONES:C_ONES + 128], sq[:, 0:n], c2 == 0, c2 == 1, (self.cbb, sqb), (ssb,))
                raws.append((raw, rawb))
            rs, rsb = self.tmp("f32")
            self.act_pow(rs[:, 0:n], ss[:, 0:n], -0.5, (ssb,), (rsb,), scale=1.0 / 256, bias=EPS)
            for c2 in range(2):
                self.stt(ckvn[:, c2, t0:t0 + n], raws[c2][0][:, 0:n], self.pvc(l, PV_DKVNORM + c2), rs[:, 0:n], ALU.mult, ALU.mult, (raws[c2][1], rsb, self.pvb), (ckvb[tc],))
            ps, psb = self.proj_fm(wkr, wkrb, 0, 96, tc)
            rope = (C_R96, self.tab[0], self.tab[1]) if tc < 4 else None
            self.headnorm(ps, psb, 96, n, C_BD96, self.pvc(l, PV_INV96, rows=96), self.pvc(l, PV_DKN, rows=96), rope, t0, KR[0:96, t0:t0 + n], KRb[tc])
        self.dump(f"ckvn{l}", ckvn[:, :, :], [128, 2, NT], ckvb)
        ukv = self.d_w_ukv[l].rearrange("(k p) c -> p k c", p=128)
        uq = self.d_w_uq[l].rearrange("(k p) c -> p k c", p=128)
        for hf in range(2):
            KDb = [[Buf(f"KD{h}_{tc}") for tc in range(5)] for h in range(4)]
            VDb = [Buf(f"VD{tc}") for tc in range(5)]
            qdb = [Buf(f"qd{h}") for h in range(4)]
            brb = [[Buf(f"brd{i}_{tc}") for tc in range(5)] for i in range(2)]
            cqnb = Buf("cqn")
            for h in range(4):
                self.vmemset(VD[h][:, :, :], 1.0, VDb)
                self.vmemset(KD[h][96:128, :], 0.0, KDb[h])
                self.vmemset(qd[h][96:128, :], 0.0, (qdb[h],))
            self.mark_phase(f'  D{hf}_kv')
            wuk, wukb = self.wload(ukv[:, :, hf * 512:(hf + 1) * 512], [128, 2, 512])
            for tc, (t0, n) in enumerate(TCS):
                for hg in ((0, 1), (2, 3)):
                    tiles = []
                    for h in hg:
                        ps, psb = self.proj_fm(wuk, wukb, h * 128, 96, tc, kch=2, rhs=lambda k: (ckvn[:, k, t0:t0 + n], ckvb[tc]))

                        def post(h=h, t0=t0, n=n, tc=tc):
                            self.vcopy(KD[h][64:96, t0:t0 + n], KR[64:96, t0:t0 + n], (KRb[tc],), (KDb[h][tc],))
                        tiles.append(dict(ps=ps, psb=psb, P=96, n=n, bdcol=C_BD96, inv=self.pvc(l, PV_INV96, rows=96), gain=self.pvc(l, PV_DKN, rows=96), rope=None, t0=t0,
                                          dest=KD[h][0:96, t0:t0 + n], destb=KDb[h][tc], post=post))
                    self.headnorm_multi(tiles)
                for jt in range(n // 128):
                    j = (t0 // 128) + jt
                    vp, vpb = self.newps()
                    for k in range(2):
                        self.mm(vp[:, 0:512], ckvn[:, k, j * 128:(j + 1) * 128], wuk[:, k, :], k == 0, k == 1, (ckvb[tc], wukb), (vpb,))
                    for h in range(4):
                        par = h % 2
                        src = vp[:, h * 128 + 64:(h + 1) * 128]
                        if par == 0:
                            self.vcopy(VD[h][:, j, 0:64], src, (vpb,), (VDb[tc],))
                        else:
                            self.actf(VD[h][:, j, 64:128], src, AF.Copy, (vpb,), (VDb[tc],))
            if hf == 0:
                self.dump(f"kd{l}", KD[1][0:96, :], [96, NT], KDb[1])
                self.dump(f"vd{l}", VD[1][:, :, :], [128, 18, 128], VDb)
            nxt = (self.wload(wv[:, :, 1536:2048], [128, 8, 512]), self.wload(uq[:, :, hf * 384:(hf + 1) * 384], [128, 4, 384]))
            mpre = None
            for tc in qtcs:
                t0, n = TCS[tc]
                self.mark_phase(f'  D{hf}_q{tc}')
                (wq, wqb), (wu, wub) = nxt
                ss, ssb = self.newps()
                raws = []
                for c4 in range(4):
                    ps, psb = self.proj_fm(wq, wqb, c4 * 128, 128, tc)
                    raw, rawb = self.tmp("f32")
                    self.actf(raw[:, 0:n], ps[:, 0:n], AF.Copy, (psb,), (rawb,))
                    sq, sqb = self.tmp("bf")
                    self.actf(sq[:, 0:n], ps[:, 0:n], AF.Square, (psb,), (sqb,))
                    self.mm(ss[:, 0:n], cb[:, C_ONES:C_ONES + 128], sq[:, 0:n], c4 == 0, c4 == 3, (self.cbb, sqb), (ssb,))
                    raws.append((raw, rawb))
                rt, rtb = self.tmp("f32")
                self.act_pow(rt[:, 0:n], ss[:, 0:n], -0.5, (ssb,), (rtb,), scale=1.0 / 512, bias=EPS)
                for c4 in range(4):
                    self.stt(cqn[:, c4, 0:n], raws[c4][0][:, 0:n], self.pvc(l, PV_DQNORM + c4), rt[:, 0:n], ALU.mult, ALU.mult, (raws[c4][1], rtb, self.pvb), (cqnb,))
                rope = (C_R96, self.tab[0], self.tab[1]) if tc < 4 else None
                for hg in ((0, 1), (2, 3)):
                    tiles = []
                    for h in hg:
                        ps, psb = self.proj_fm(wu, wub, h * 96, 96, tc, kch=4, rhs=lambda k: (cqn[:, k, 0:n], cqnb))
                        tiles.append(dict(ps=ps, psb=psb, P=96, n=n, bdcol=C_BD96, inv=self.pvc(l, PV_INV96, rows=96), gain=self.pvc(l, PV_DQN, rows=96), rope=rope, t0=t0,
                                          dest=qd[h][0:96, 0:n], destb=qdb[h]))
                    self.headnorm_multi(tiles)
                if hf == 0 and tc == 0:
                    self.dump(f"qd{l}", qd[1][0:96, :], [96, 512], (qdb[1],))
                kts = [(j, 0, n) for j in range(18)] if tc < 4 else [(16, 0, n), (17, 0, n)]
                if tc != qtcs[-1]:
                    nxt = (self.wload(wv[:, :, 1536:2048], [128, 8, 512]), self.wload(uq[:, :, hf * 384:(hf + 1) * 384], [128, 4, 384]))
                else:
                    mpre = self.merge_preload(l, 3, [2 * hf, 2 * hf + 1], 0)
                self.mark_phase(f'  D{hf}_att{tc}')
                jobs = []
                for h in range(4):
                    par = h % 2

                    def fin(accs, h=h, par=par, n=n, t0=t0, tc=tc):
                        acc, accb = accs[0]
                        self.norm_out(acc, accb, par, n, br[h // 2][par * 64:(par + 1) * 64, t0:t0 + n], brb[h // 2][tc])
                    jobs.append(dict(qap=qd[h], qb=qdb[h], KT=KD[h], Kb=KDb[h], P=128, kts=kts,
                                     vfun=(lambda j, h=h: [VD[h][:, j, :]]),
                                     vb_of=(lambda j: VDb[j // 4 if j < 16 else 4]), scale=1.0 / math.sqrt(96.0), masks=None, nacc=1, fin=fin))
                self.attn_stream(jobs)
                if tc == 0 and hf == 0:
                    self.dump(f"br3_{l}", br[0][:, 0:512], [128, 512], (brb[0][0],))
            self.mark_phase(f'  D{hf}_mrg0')
            self.merge(l, 3, [2 * hf, 2 * hf + 1], br, brb, qtcs, first=False, pre=mpre)
            self.soft_barrier()
        self.release(m0)
        self.dump(f"z{l}", self.z[:, :, :], [128, 8, NT], [b for r in self.zb for b in r])

    def wout_phase(self, l, last):
        self.sp_off = self.mix_base
        self.lat = self.alloc([128, 8, L], F32, "lat")
        self.latb = [[Buf(f"lat{dc}_{tc}") for tc in range(5)] for dc in range(8)]
        if l == 1:
            self.LT = self.alloc([128, L], F32, "LT")
            self.LTb = [Buf(f"LT{tc}") for tc in range(4)]
        self.ffn_base = self.phase_base
        ov = self.outT.rearrange("(k p) t -> p k t", p=128)
        wov = self.w_out[l].rearrange("(k p) c -> p k c", p=128)
        for hf in range(2):
            for tc, (t0, n) in enumerate(TCS[:4]):
                for d4 in range(4):
                    dc = hf * 4 + d4
                    self.dma(self.sp, self.lat[:, dc, t0:t0 + n], ov[:, dc, t0:t0 + n], (self.outb[dc][tc],), (self.latb[dc][tc],), self.ldsem())
        for hf in range(2):
            wt, wb = self.wload(wov[:, :, hf * 512:(hf + 1) * 512], [128, 8, 512])
            for tc, (t0, n) in enumerate(TCS):
                if tc == 4 and last:
                    continue
                j2 = 1 if tc == 4 else 0
                for d4 in range(4):
                    dc = hf * 4 + d4
                    ps, psb = self.newps()
                    for k in range(8):
                        self.mm(ps[:, 0:n], wt[:, k, d4 * 128:(d4 + 1) * 128], self.z[:, k, t0:t0 + n], k == 0, k == 7, (wb, self.zb[k][tc]), (psb,))
                    la = self.lat_ap(dc, tc)
                    self.stt(la, ps[:, 0:n], self.mcol(2, dc, j2), la, ALU.mult, ALU.add, (psb, self.modb, self.latb[dc][tc]), (self.latb[dc][tc],))
        self.dump(f"latmix{l}", self.lat[:, :, :], [128, 8, L], [self.latb[dc][tc] for dc in range(8) for tc in range(4)])

    def ffn_phase(self, l, last):
        moe = (l == 1)
        self.sp_off = self.ffn_base
        ntc = 4 if last else 5
        ntok = L if last else NT
        G = 4
        act = self.alloc([128, G, ntok], BF16, "act")
        actb = [[Buf(f"act{f}_{tc}") for tc in range(5)] for f in range(G)]
        if moe:
            lse = self.alloc([128, L], F32, "lse")
            m2 = self.alloc([128, L], F32, "m2")
            lseb = [Buf(f"lse{tc}") for tc in range(4)]
            m2b = [Buf(f"m2{tc}") for tc in range(4)]
            cw = [self.alloc([128, L], BF16, "cw") for _ in range(1)]
            cwb = [[Buf(f"cw{i}_{tc}") for tc in range(4)] for i in range(1)]
            for tc in range(4):
                t0, n = TCS[tc]
                for e in range(8):
                    rp, rpb = self.newps()
                    slh, slb_ = self.sel_ap(e)
                    self.mm(rp[:, 0:n], slh[:, :], self.LT[:, t0:t0 + n], True, True, (slb_, self.LTb[tc]), (rpb,))
                    if e == 0:
                        self.vcopy(lse[:, t0:t0 + n], rp[:, 0:n], (rpb,), (lseb[tc],))
                    else:
                        self.tt(lse[:, t0:t0 + n], rp[:, 0:n], lse[:, t0:t0 + n], ALU.max, (rpb, lseb[tc]), (lseb[tc],))
                for e in range(8):
                    rp, rpb = self.newps()
                    slh, slb_ = self.sel_ap(e)
                    self.mm(rp[:, 0:n], slh[:, :], self.LT[:, t0:t0 + n], True, True, (slb_, self.LTb[tc]), (rpb,))
                    ge, geb = self.tmp("f32")
                    self.tt(ge[:, 0:n], rp[:, 0:n], lse[:, t0:t0 + n], ALU.is_ge, (rpb, lseb[tc]), (geb,))
                    t2, t2b = self.tmp("f32")
                    self.stt(t2[:, 0:n], ge[:, 0:n], NEG_BIG, rp[:, 0:n], ALU.mult, ALU.add, (geb, rpb), (t2b,))
                    if e == 0:
                        self.vcopy(m2[:, t0:t0 + n], t2[:, 0:n], (t2b,), (m2b[tc],))
                    else:
                        self.tt(m2[:, t0:t0 + n], t2[:, 0:n], m2[:, t0:t0 + n], ALU.max, (t2b, m2b[tc]), (m2b[tc],))
                d, db = self.tmp("f32")
                self.tt(d[:, 0:n], m2[:, t0:t0 + n], lse[:, t0:t0 + n], ALU.subtract, (m2b[tc], lseb[tc]), (db,))
                self.actf(d[:, 0:n], d[:, 0:n], AF.Exp, (db,), (db,))
                self.actf(d[:, 0:n], d[:, 0:n], AF.Ln, (db,), (db,), bias=1.0)
                self.tt(lse[:, t0:t0 + n], lse[:, t0:t0 + n], d[:, 0:n], ALU.add, (db, lseb[tc]), (lseb[tc],))
            self.dump("m2", m2[:, :], [128, L], m2b)
            self.dump("lse", lse[:, :], [128, L], lseb)
            units = [(e, g) for e in range(8) for g in range(EXPERT_FF // 512)]
        else:
            units = [(None, g) for g in range((D_FF + 511) // 512)]
        cur_e = None
        for (e, g) in units:
            if moe:
                w1v = self.moe_w1[0, e].rearrange("(k p) c -> p k c", p=128)
                w3v = self.moe_w3[0, e].rearrange("(k p) c -> p k c", p=128)
                w2v = self.moe_w2[0, e].rearrange("(f p) d -> p f d", p=128)
                nf = 4
                if e != cur_e:
                    cur_e = e
                    cwt = cw[0]
                    cwtb = cwb[0]
                    for tc in range(4):
                        t0, n = TCS[tc]
                        rp, rpb = self.newps()
                        slh, slb_ = self.sel_ap(e)
                        self.mm(rp[:, 0:n], slh[:, :], self.LT[:, t0:t0 + n], True, True, (slb_, self.LTb[tc]), (rpb,))
                        d, db = self.tmp("f32")
                        self.tt(d[:, 0:n], rp[:, 0:n], lse[:, t0:t0 + n], ALU.subtract, (rpb, lseb[tc]), (db,))
                        self.actf(d[:, 0:n], d[:, 0:n], AF.Exp, (db,), (db,))
                        sl, slb = self.tmp("f32")
                        self.tt(sl[:, 0:n], rp[:, 0:n], m2[:, t0:t0 + n], ALU.is_ge, (rpb, m2b[tc]), (slb,))
                        self.tt(cwt[:, t0:t0 + n], d[:, 0:n], sl[:, 0:n], ALU.mult, (db, slb), (cwtb[tc],))
                    if e == 0:
                        self.dump("cw0", cwt[:, :], [128, L], cwtb)
            else:
                w1v = self.ffg[0].rearrange("(k p) c -> p k c", p=128)
                w3v = self.ffu[0].rearrange("(k p) c -> p k c", p=128)
                w2v = self.ffd[0].rearrange("(f p) d -> p f d", p=128)
                nf = min(4, (D_FF - g * 512) // 128)
            c0 = g * 512
            sp = 2 if nf == 4 else 1
            w1t, w1bs = self.wload(w1v[:, :, c0:c0 + nf * 128], [128, 8, nf * 128], split=sp)
            w3t, w3bs = self.wload(w3v[:, :, c0:c0 + nf * 128], [128, 8, nf * 128], split=sp)
            if sp == 1:
                w1bs, w3bs = [w1bs, w1bs], [w3bs, w3bs]
            for tc in range(ntc):
                t0, n = TCS[tc]
                for f in range(nf):
                    gp, gpb = self.proj_fm(w1t, w1bs[f // 2], f * 128, 128, tc)
                    up, upb = self.proj_fm(w3t, w3bs[f // 2], f * 128, 128, tc)
                    sg, sgb = self.tmp("f32")
                    self.actf(sg[:, 0:n], gp[:, 0:n], AF.Silu, (gpb,), (sgb,))
                    if moe:
                        t, tb = self.tmp("f32")
                        self.tt(t[:, 0:n], up[:, 0:n], sg[:, 0:n], ALU.mult, (upb, sgb), (tb,))
                        self.tt(act[:, f, t0:t0 + n], t[:, 0:n], cwt[:, t0:t0 + n], ALU.mult, (tb, cwtb[tc]), (actb[f][tc],))
                    else:
                        self.tt(act[:, f, t0:t0 + n], up[:, 0:n], sg[:, 0:n], ALU.mult, (upb, sgb), (actb[f][tc],))
            w2t, w2b = self.wload(w2v[:, g * 4:g * 4 + nf, :], [128, nf, 1024])
            for tc in range(ntc):
                t0, n = TCS[tc]
                j2 = 1 if tc == 4 else 0
                for dc in range(8):
                    yp, ypb = self.newps()
                    for f in range(nf):
                        self.mm(yp[:, 0:n], w2t[:, f, dc * 128:(dc + 1) * 128], act[:, f, t0:t0 + n], f == 0, f == nf - 1, (w2b, actb[f][tc]), (ypb,))
                    la = self.lat_ap(dc, tc)
                    self.stt(la, yp[:, 0:n], self.mcol(5, dc, j2), la, ALU.mult, ALU.add, (ypb, self.modb, self.latb[dc][tc]), (self.latb[dc][tc],))
        self.dump(f"lat{l}", self.lat[:, :, :], [128, 8, L], [self.latb[dc][tc] for dc in range(8) for tc in range(4)])


def _rope_tabs():
    def axial(rot_dim):
        t = np.arange(L)
        n = rot_dim // 4
        inv = np.power(np.float32(10000.0), -np.arange(n, dtype=np.float32) / np.float32(n)).astype(np.float32)
        ang = np.concatenate([(t // 64).astype(np.float32)[:, None] * inv, (t % 64).astype(np.float32)[:, None] * inv], axis=-1)
        return np.cos(ang).astype(np.float32), np.sin(ang).astype(np.float32)
    tabs = np.zeros((4, 128, L), np.float32)
    c, s = axial(64)
    for p in range(128):
        tabs[0, p] = c[:, p % 32]
        tabs[1, p] = s[:, p % 32]
    c, s = axial(32)
    tabs[2, :64] = 1.0
    for p in range(64, 96):
        tabs[2, p] = c[:, (p - 64) % 16]
        tabs[3, p] = s[:, (p - 64) % 16]
    return tabs


def _consts():
    cst = np.zeros((128, NCB), np.float32)
    cst[:, C_ONES:C_ONES + 128] = 1.0
    for p in range(128):
        for m in range(128):
            if p // 64 == m // 64:
                cst[p, C_BD64 + m] = 1.0
    for p in range(96):
        for m in range(96):
            if (p < 64) == (m < 64):
                cst[p, C_BD96 + m] = 1.0
    for m in range(128):
        if m % 64 < 32:
            cst[m + 32, C_R64 + m] = -1.0
        else:
            cst[m - 32, C_R64 + m] = 1.0
    for m in range(64, 96):
        if m < 80:
            cst[m + 16, C_R96 + m] = -1.0
        else:
            cst[m - 16, C_R96 + m] = 1.0
    k = np.arange(128)[:, None]
    i = np.arange(128)[None, :]
    cst[:, C_MASKL:C_MASKL + 128] = (k >= i)
    cst[:, C_MASKR:C_MASKR + 128] = (k <= i)
    pidx = np.broadcast_to(np.arange(128, dtype=np.float32)[:, None], (128, 128)).copy()
    return cst, pidx


def _pv(inp):
    pv = np.zeros((2, 128, NPV), np.float32)
    t2 = lambda v: np.tile(np.asarray(v, np.float32), 2)
    for l in range(2):
        pv[l, :, PV_MIXN:PV_MIXN + 8] = inp["mix_norm"][l].reshape(8, 128).T
        pv[l, :, PV_FFNN:PV_FFNN + 8] = inp["ffn_norm"][l].reshape(8, 128).T
        pv[l, :, PV_BMOD:PV_BMOD + 48] = inp["b_mod"][l].reshape(48, 128).T
        pv[l, :, PV_AQN] = t2(inp["a_qn"][l]); pv[l, :, PV_AKN] = t2(inp["a_kn"][l])
        pv[l, :, PV_BQN] = t2(inp["b_qn"][l]); pv[l, :, PV_BKN] = t2(inp["b_kn"][l])
        pv[l, :, PV_CQN] = t2(inp["c_qn"][l]); pv[l, :, PV_CKN] = t2(inp["c_kn"][l])
        pv[l, :, PV_DQNORM:PV_DQNORM + 4] = inp["d_q_norm"][l].reshape(4, 128).T
        pv[l, :, PV_DKVNORM:PV_DKVNORM + 2] = inp["d_kv_norm"][l].reshape(2, 128).T
        pv[l, 0:64, PV_DQN] = inp["d_qn_nope"][l]; pv[l, 64:96, PV_DQN] = inp["d_qn_rope"][l]
        pv[l, 0:64, PV_DKN] = inp["d_kn_nope"][l]; pv[l, 64:96, PV_DKN] = inp["d_kn_rope"][l]
        pv[l, :, PV_SUBLN] = inp["c_subln"][l]
        pv[l, :, PV_SINK:PV_SINK + 8] = np.broadcast_to(inp["a_sink"][l][None, :], (128, 8))
        pv[l, 0:64, PV_LQK + 0] = inp["c_lq1"][l]; pv[l, 0:64, PV_LQK + 1] = inp["c_lk1"][l]
        pv[l, 0:64, PV_LQK + 2] = inp["c_lq2"][l]; pv[l, 0:64, PV_LQK + 3] = inp["c_lk2"][l]
        pv[l, 0:64, PV_INV96] = 1.0 / 64; pv[l, 64:96, PV_INV96] = 1.0 / 32
    return pv


_CACHE = {}


def make_in_maps(inp, ncores=8):
    f = lambda a: np.ascontiguousarray(np.asarray(a, dtype=np.float32))
    cst, sel = _consts()
    tabs = _rope_tabs()
    pv = _pv(inp)
    shared = {k: f(inp[k]) for k in ["w_mod", "w_in", "d_w_uq", "d_w_ukv", "w_br", "w_out", "ff_w_gate", "ff_w_up", "ff_w_down",
                                      "moe_router", "moe_w1", "moe_w3", "moe_w2"]}
    shared.update(cstf=cst, pidx=sel, tabs=tabs, pv=pv)
    maps = []
    for b in range(ncores):
        m = dict(shared)
        m["xT"] = f(np.asarray(inp["x"][b]).T)
        m["ctxT"] = f(np.asarray(inp["ctx"][b]).T)
        cv = np.zeros((128, 16), np.float32)
        cv[:, 0:8] = np.asarray(inp["c"][b]).reshape(8, 128).T
        cv[:, 8:16] = np.asarray(inp["c_ctx"]).reshape(8, 128).T
        m["cvec"] = cv
        maps.append(m)
    return maps


FUSED = True


def _drop(m, layers):
    m = dict(m)
    if 0 not in layers:
        for k in ["ff_w_gate", "ff_w_up", "ff_w_down"]:
            m.pop(k)
    if 1 not in layers:
        for k in ["moe_router", "moe_w1", "moe_w3", "moe_w2"]:
            m.pop(k)
    return m


def kernel(**inputs):
    maps = make_in_maps(inputs)
    if FUSED:
        if "full" not in _CACHE:
            _CACHE["full"] = KB(layers=(0, 1))
        kb = _CACHE["full"]
        res = run_bass_kernel_spmd(kb.nc, maps, core_ids=list(range(8)))
    else:
        for key, layers in (("l0", (0,)), ("l1", (1,))):
            if key not in _CACHE:
                _CACHE[key] = KB(layers=layers)
        r0 = run_bass_kernel_spmd(_CACHE["l0"].nc, [_drop(m, (0,)) for m in maps], core_ids=list(range(8)))
        maps1 = []
        for m, r in zip(maps, r0.results):
            m1 = _drop(m, (1,))
            m1["xT"] = np.ascontiguousarray(np.asarray(r["outT"], dtype=np.float32))
            m1["ctxT"] = np.ascontiguousarray(np.asarray(r["ctxo"], dtype=np.float32))
            maps1.append(m1)
        res = run_bass_kernel_spmd(_CACHE["l1"].nc, maps1, core_ids=list(range(8)))
    out = np.stack([np.asarray(r["outT"]).T for r in res.results], axis=0)
    return np.ascontiguousarray(out.astype(np.float32))
```

```python
import math
import numpy as np
import concourse.bass as bass
import concourse.mybir as mybir
from concourse.bass_utils import run_bass_kernel_spmd

F32 = mybir.dt.float32
BF16 = mybir.dt.bfloat16
ALU = mybir.AluOpType
AF = mybir.ActivationFunctionType

D = 1024
L = 2048
CTX = 256
NT = L + CTX
IN_COLS = 7968
GATE0 = 3872
D_FF = 2816
EXPERT_FF = 3584
EPS = 1e-6
TCS = [(0, 512), (512, 512), (1024, 512), (1536, 512), (2048, 256)]
NEG_BIG = -1.0e30

PV_MIXN = 0
PV_FFNN = 8
PV_BMOD = 16
PV_AQN = 64
PV_AKN = 65
PV_BQN = 66
PV_BKN = 67
PV_CQN = 68
PV_CKN = 69
PV_DQNORM = 70
PV_DKVNORM = 74
PV_DQN = 76
PV_DKN = 77
PV_SUBLN = 78
PV_SINK = 79
PV_LQK = 87
PV_INV96 = 91
NPV = 92

C_ONES = 0
C_BD64 = 128
C_BD96 = 256
C_R64 = 384
C_R96 = 512
C_MASKL = 640
C_MASKR = 768
NCB = 896


_FRONTIER = {}


class Buf:
    __slots__ = ("w", "r", "name")

    def __init__(self, name=""):
        self.w = None
        self.r = dict(_FRONTIER)
        self.name = name


class EngQ:
    def __init__(self, nc, name, h, is_pe=False):
        self.h = h
        self.sem = nc.alloc_semaphore("s_" + name)
        self.n = 0
        self.waited = {}
        self.is_pe = is_pe
        self.name = name


class DSem:
    def __init__(self, nc, name):
        self.sem = nc.alloc_semaphore(name)
        self.n = 0


class KB:
    def __init__(self, layers=(0, 1), first=True, final=True, dumps=()):
        global _FRONTIER
        _FRONTIER = {}
        self.layers = layers
        self.dumps = set(dumps)
        nc = self.nc = bass.Bass("TRN2", target_bir_lowering=False)
        self.pe = EngQ(nc, "pe", nc.tensor, True)
        self.act = EngQ(nc, "act", nc.scalar)
        self.dve = EngQ(nc, "dve", nc.vector)
        self.pool = EngQ(nc, "pool", nc.gpsimd)
        self.sp = EngQ(nc, "sp", nc.sync)
        self.engs = [self.pe, self.act, self.dve, self.pool, self.sp]
        self.dsems = []
        self.dump_outs = []
        dt = lambda n, s, k="ExternalInput": nc.dram_tensor(n, list(s), F32, kind=k).ap()
        self.xT = dt("xT", [D, L])
        self.ctxT = dt("ctxT", [D, CTX])
        self.cvec = dt("cvec", [128, 16])
        self.cstf = dt("cstf", [128, NCB])
        self.pidx_d = dt("pidx", [128, 128])
        self.tabs = dt("tabs", [4, 128, L])
        self.pv = dt("pv", [2, 128, NPV])
        self.w_mod = dt("w_mod", [2, D, 6 * D])
        self.w_in = dt("w_in", [2, D, IN_COLS])
        self.d_w_uq = dt("d_w_uq", [2, 512, 768])
        self.d_w_ukv = dt("d_w_ukv", [2, 256, 1024])
        self.w_br = dt("w_br", [2, 4, 512, D])
        self.w_out = dt("w_out", [2, D, D])
        if 0 in layers:
            self.ffg = dt("ff_w_gate", [1, D, D_FF])
            self.ffu = dt("ff_w_up", [1, D, D_FF])
            self.ffd = dt("ff_w_down", [1, D_FF, D])
        if 1 in layers:
            self.moe_r = dt("moe_router", [1, D, 8])
            self.moe_w1 = dt("moe_w1", [1, 8, D, EXPERT_FF])
            self.moe_w3 = dt("moe_w3", [1, 8, D, EXPERT_FF])
            self.moe_w2 = dt("moe_w2", [1, 8, EXPERT_FF, D])
        self.outT = dt("outT", [D, L], "ExternalOutput")
        self.ctxo = dt("ctxo", [D, CTX], "ExternalOutput")
        self.outb = [[Buf(f"out{dc}_{tc}") for tc in range(5)] for dc in range(8)]
        self.ps = [nc.alloc_psum_tensor(f"ps{i}", [128, 512], F32) for i in range(8)]
        self.psb = [Buf(f"ps{i}") for i in range(8)]
        self.ps_ctr = {}
        rem = nc.sbuf_bytes_remaining
        self.arena_size = (rem - 256) // 64 * 64
        arena = nc.alloc_sbuf_tensor("arena", [128, self.arena_size // 4], F32)
        self.arena_base = nc.lookup_mloc(arena).addr
        self.sp_off = 0
        self.uid = 0
        self.build(first, final)

    def alloc(self, shape, dtype, name="t"):
        nb = int(np.prod(shape[1:])) * (4 if dtype == F32 else 2)
        nb = (nb + 63) // 64 * 64
        off = self.sp_off
        assert off + nb <= self.arena_size, f"SBUF arena overflow: {name} {off}+{nb} > {self.arena_size}"
        self.sp_off += nb
        self.sp_max = max(getattr(self, "sp_max", 0), self.sp_off)
        self.uid += 1
        return self.nc.alloc_sbuf_tensor_at(f"{name}_{self.uid}", list(shape), dtype, offset=self.arena_base + off)

    def mark(self):
        return self.sp_off

    def release(self, m):
        self.sp_off = m

    def dsem(self, name):
        d = DSem(self.nc, name)
        self.dsems.append(d)
        return d

    def issue(self, q, fn, reads=(), writes=(), dsem=None, guard=()):
        deps = {}

        def add(s, v):
            if deps.get(s, 0) < v:
                deps[s] = v

        for b in guard:
            if b.w is not None:
                add(*b.w)
            for s_, v_ in b.r.items():
                add(s_, v_)

        for b in reads:
            if b.w is not None:
                add(*b.w)
        for b in writes:
            if b.w is not None:
                add(*b.w)
            for s, v in b.r.items():
                add(s, v)
        if dsem is not None and dsem.n > 0:
            add(dsem.sem, dsem.n)
        need = []
        for s, v in deps.items():
            if q.is_pe and s is q.sem:
                continue
            if q.waited.get(s, 0) >= v:
                continue
            need.append((s, v))
            q.waited[s] = v
        for s, v in need[:-1]:
            q.h.wait_ge(s, v)
        inst = fn()
        if need:
            inst._wait_ge(*need[-1])
        if dsem is None:
            q.n += 1
            inst.then_inc(q.sem, 1)
            ev = (q.sem, q.n)
        else:
            dsem.n += 16
            inst.then_inc(dsem.sem, 16)
            ev = (dsem.sem, dsem.n)
        for b in reads:
            if b.r.get(ev[0], 0) < ev[1]:
                b.r[ev[0]] = ev[1]
        for b in writes:
            b.w = ev
            b.r = {}
        return ev

    def soft_barrier(self):
        global _FRONTIER
        _FRONTIER = {s: v for s, v in ([(e.sem, e.n) for e in self.engs] + [(d.sem, d.n) for d in self.dsems]) if v > 0}

    def barrier(self):
        allsem = [(e.sem, e.n) for e in self.engs] + [(d.sem, d.n) for d in self.dsems]
        for q in self.engs:
            for s, v in allsem:
                if v > 0 and q.waited.get(s, 0) < v and not (q.is_pe and s is q.sem):
                    q.h.wait_ge(s, v)
                    q.waited[s] = v

    def mark_phase(self, name):
        if not hasattr(self, "phases"):
            self.phases = []
        self.phases.append((name, self.pe.n))

    def sel_ap(self, e):
        h, b = self.selbufs[self.sel_i % 2]
        self.sel_i += 1
        self.ts(h[:, :], self.pidx[:, :], float(e), None, ALU.is_equal, None, (self.pidxb,), (b,))
        return h, b

    def newps(self, lo=0, hi=8):
        key = (lo, hi)
        i = self.ps_ctr.get(key, 0)
        self.ps_ctr[key] = i + 1
        b = lo + i % (hi - lo)
        return self.ps[b], self.psb[b]

    def mm(self, out, lhsT, rhs, start, stop, reads, writes):
        return self.issue(self.pe, lambda: self.nc.tensor.matmul(out, lhsT=lhsT, rhs=rhs, start=start, stop=stop), reads, writes)

    def actf(self, out, in_, func, reads, writes, scale=1.0, bias=0.0):
        return self.issue(self.act, lambda: self.nc.scalar.activation(out=out, in_=in_, func=func, bias=bias, scale=scale), reads, writes)

    def tt(self, out, in0, in1, op, reads, writes):
        return self.issue(self.dve, lambda: self.nc.vector.tensor_tensor(out=out, in0=in0, in1=in1, op=op), reads, writes)

    def ts(self, out, in0, s1, s2, op0, op1, reads, writes):
        if op1 is None:
            return self.issue(self.dve, lambda: self.nc.vector.tensor_scalar(out=out, in0=in0, scalar1=s1, scalar2=None, op0=op0), reads, writes)
        return self.issue(self.dve, lambda: self.nc.vector.tensor_scalar(out=out, in0=in0, scalar1=s1, scalar2=s2, op0=op0, op1=op1), reads, writes)

    def stt(self, out, in0, scalar, in1, op0, op1, reads, writes):
        return self.issue(self.dve, lambda: self.nc.vector.scalar_tensor_tensor(out=out, in0=in0, scalar=scalar, in1=in1, op0=op0, op1=op1), reads, writes)

    def vcopy(self, out, in_, reads, writes):
        return self.issue(self.dve, lambda: self.nc.vector.tensor_copy(out=out, in_=in_), reads, writes)

    def recip(self, out, in_, reads, writes, exact=True):
        return self.issue(self.dve, lambda: self.nc.vector.reciprocal(out=out, in_=in_), reads, writes)

    def act_pow(self, out, in_, power, reads, writes, scale=1.0, bias=0.0):
        self.actf(out, in_, AF.Ln, reads, writes, scale=scale, bias=bias)
        return self.actf(out, out, AF.Exp, writes, writes, scale=power)

    def pmemset(self, ap, val, writes):
        return self.issue(self.pool, lambda: self.nc.gpsimd.memset(ap, val), (), writes)

    def vmemset(self, ap, val, writes):
        return self.issue(self.dve, lambda: self.nc.vector.memset(ap, val), (), writes)

    def dma(self, q, out, in_, reads, writes, dsem, guard=()):
        return self.issue(q, lambda: q.h.dma_start(out=out, in_=in_), reads, writes, dsem, guard)

    def plsem(self):
        d = self.pl_sems[self.pl_i % len(self.pl_sems)]
        self.pl_i += 1
        return d

    def ldsem(self):
        d = self.ld_sems[self.ld_i % len(self.ld_sems)]
        self.ld_i += 1
        return d

    def tmp(self, kind):
        lst = self.tmps[kind]
        i = self.tmp_i.get(kind, 0)
        self.tmp_i[kind] = i + 1
        return lst[i % len(lst)]

    def wload(self, dram_ap, shape, split=1):
        slot_h, slot_bs, slot_ds = self.wslots[self.w_i % len(self.wslots)]
        self.w_i += 1
        n = int(np.prod(shape[1:]))
        assert n <= self.WSLOT
        if len(shape) == 3:
            view = slot_h[0:shape[0], 0:n].rearrange("p (a b) -> p a b", a=shape[1])
        else:
            view = slot_h[0:shape[0], 0:n]
        if split == 1:
            self.dma(self.pool, view, dram_ap, (), tuple(slot_bs), slot_ds[0])
            return view, slot_bs[0]
        h = shape[-1] // 2
        self.dma(self.pool, view[:, :, 0:h], dram_ap[:, :, 0:h], (), (slot_bs[0],), slot_ds[0], guard=(slot_bs[1],))
        self.dma(self.pool, view[:, :, h:], dram_ap[:, :, h:], (), (slot_bs[1],), slot_ds[1])
        return view, list(slot_bs)

    def dump(self, name, ap, shape, bufs):
        if name not in self.dumps:
            return
        o = self.nc.dram_tensor("dbg_" + name, list(shape), F32 if ap.dtype == F32 else BF16, kind="ExternalOutput").ap()
        self.dump_outs.append("dbg_" + name)
        self.dma(self.sp, o, ap, bufs, (), self.ldsem())

    def build(self, first, final):
        nc = self.nc
        A = self.alloc
        self.cb = A([128, NCB], BF16, "cb")
        self.cbb = Buf("cb")
        self.pidx = A([128, 128], F32, "pidx")
        self.pidxb = Buf("pidx")
        self.selbufs = [(A([128, 128], F32, "selbuf"), Buf("selbuf")) for _ in range(2)]
        self.sel_i = 0
        self.pvt = A([128, 2 * NPV], F32, "pv")
        self.pvb = Buf("pv")
        self.cv = A([128, 16], F32, "cv")
        self.cvb = Buf("cv")
        self.modv = A([128, 96], F32, "modv")
        self.modb = Buf("modv")
        self.drv = A([128, 64], F32, "drv")
        self.drvb = Buf("drv")
        self.latc = A([128, 8, CTX], F32, "latc")
        self.hT = A([128, 8, NT], BF16, "hT")
        self.hTb = [[Buf(f"hT{dc}_{tc}") for tc in range(5)] for dc in range(8)]
        self.WSLOT = 4096
        self.wslots = [(A([128, self.WSLOT], BF16, f"w{i}"), [Buf(f"w{i}a"), Buf(f"w{i}b")], [self.dsem(f"wd{i}a"), self.dsem(f"wd{i}b")]) for i in range(3)]
        self.w_i = 0
        self.ld_sems = [self.dsem(f"ld{i}") for i in range(8)]
        self.ld_i = 0
        self.pl_sems = [self.dsem(f"pl{i}") for i in range(2)]
        self.pl_i = 0
        self.tmps = {
            "f32": [(A([128, 512], F32, "tf"), Buf("tf")) for _ in range(5)],
            "bf": [(A([128, 512], BF16, "tb"), Buf("tb")) for _ in range(6)],
            "pT": [(A([128, 512], BF16, "pT"), Buf("pT")) for _ in range(4)],
            "rs": [(A([128, 512], F32, "rs"), Buf("rs")) for _ in range(2)],
        }
        self.tmp_i = {}
        self.phase_base = self.mark()
        self.dma(self.pool, self.cb[:, :], self.cstf, (), (self.cbb,), self.plsem())
        self.dma(self.sp, self.pidx[:, :], self.pidx_d, (), (self.pidxb,), self.ldsem())
        self.dma(self.sp, self.pvt[:, :].rearrange("p (l n) -> p l n", l=2), self.pv.rearrange("l p n -> p l n"), (), (self.pvb,), self.ldsem())
        self.dma(self.sp, self.cv[:, :], self.cvec, (), (self.cvb,), self.ldsem())
        self.lat = A([128, 8, L], F32, "lat")
        self.latb = [[Buf(f"lat{dc}_{tc}") for tc in range(5)] for dc in range(8)]
        self.lat_end = self.mark()
        xv = self.xT.rearrange("(k p) t -> p k t", p=128)
        cxv = self.ctxT.rearrange("(k p) t -> p k t", p=128)
        for tc, (t0, n) in enumerate(TCS):
            for dc in range(8):
                if tc < 4:
                    self.dma(self.sp, self.lat[:, dc, t0:t0 + n], xv[:, dc, t0:t0 + n], (), (self.latb[dc][tc],), self.ldsem())
                else:
                    self.dma(self.sp, self.latc[:, dc, :], cxv[:, dc, :], (), (self.latb[dc][tc],), self.ldsem())
        for l in self.layers:
            last = (l == 1)
            self.mark_phase(f'mod{l}')
            self.modulation(l)
            self.mark_phase(f'adaln0_{l}')
            self.adaln(l, which=0, last=last)
            self.store_lat()
            self.soft_barrier()
            self.release(self.phase_base)
            self.mark_phase(f'mixer{l}')
            self.mixer_phase(l, last)
            self.mark_phase(f'wout{l}')
            self.soft_barrier()
            self.wout_phase(l, last)
            self.soft_barrier()
            self.mark_phase(f'adaln1_{l}')
            self.adaln(l, which=1, last=last)
            self.soft_barrier()
            self.mark_phase(f'ffn{l}')
            self.ffn_phase(l, last)
            self.mark_phase(f'end{l}')
            self.soft_barrier()
        self.store_lat(final=True)
        for d in self.dsems:
            if d.n > 0 and self.sp.waited.get(d.sem, 0) < d.n:
                self.sp.h.wait_ge(d.sem, d.n)
                self.sp.waited[d.sem] = d.n

    def lat_ap(self, dc, tc):
        t0, n = TCS[tc]
        return self.lat[:, dc, t0:t0 + n] if tc < 4 else self.latc[:, dc, :]

    def pvc(self, l, col, rows=128, n=1):
        return self.pvt[0:rows, l * NPV + col:l * NPV + col + n]

    def store_lat(self, final=False):
        ov = self.outT.rearrange("(k p) t -> p k t", p=128)
        cov = self.ctxo.rearrange("(k p) t -> p k t", p=128)
        for tc, (t0, n) in enumerate(TCS):
            for dc in range(8):
                if tc < 4:
                    self.dma(self.sp, ov[:, dc, t0:t0 + n], self.lat[:, dc, t0:t0 + n], (self.latb[dc][tc],), (self.outb[dc][tc],), self.ldsem())
                elif final:
                    self.dma(self.sp, cov[:, dc, :], self.latc[:, dc, :], (self.latb[dc][tc],), (self.outb[dc][tc],), self.ldsem())

    def modulation(self, l):
        sc32, sc32b = self.tmp("f32")
        scb, scbb = self.tmp("bf")
        self.actf(sc32[:, 0:16], self.cv[:, :], AF.Silu, (self.cvb,), (sc32b,))
        self.vcopy(scb[:, 0:16].rearrange("p (k j) -> p k j", j=2), sc32[:, 0:16].rearrange("p (j k) -> p k j", j=2), (sc32b,), (scbb,))
        wv = self.w_mod[l].rearrange("(k p) c -> p k c", p=128)
        ps, psb = self.newps()
        for cb_ in range(12):
            wt, wb = self.wload(wv[:, :, cb_ * 512:(cb_ + 1) * 512], [128, 8, 512])
            for jj in range(4):
                j = cb_ * 4 + jj
                for k in range(8):
                    self.mm(ps[:, 2 * j:2 * j + 2], wt[:, k, jj * 128:(jj + 1) * 128], scb[:, 2 * k:2 * k + 2], k == 0, k == 7, (wb, scbb), (psb,))
        bm = self.pvc(l, PV_BMOD, n=48)
        for j2 in range(2):
            self.tt(self.modv[:, :].rearrange("p (j t) -> p j t", t=2)[:, :, j2], ps[:, 0:96].rearrange("p (j t) -> p j t", t=2)[:, :, j2], bm, ALU.add, (psb, self.pvb), (self.modb,))
        mv = self.modv[:, :].rearrange("p (j t) -> p j t", t=2)
        dv = self.drv[:, 0:32].rearrange("p (a d t) -> p a d t", a=2, t=2)
        for j2 in range(2):
            for a, (mi, gcol) in enumerate(((1, PV_MIXN), (4, PV_FFNN))):
                self.stt(dv[:, a, :, j2], mv[:, mi * 8:mi * 8 + 8, j2], 1.0, self.pvc(l, gcol, n=8), ALU.add, ALU.mult, (self.modb, self.pvb), (self.drvb,))
        self.actf(self.drv[:, 32:40], self.pvc(l, PV_SINK, n=8), AF.Exp, (self.pvb,), (self.drvb,))
        self.tt(self.drv[:, 40:42], self.pvc(l, PV_LQK, n=4).rearrange("p (a b) -> p a b", b=2)[:, :, 0], self.pvc(l, PV_LQK, n=4).rearrange("p (a b) -> p a b", b=2)[:, :, 1], ALU.mult, (self.pvb,), (self.drvb,))
        ps2, ps2b = self.newps()
        self.vmemset(self.drv[:, 48:64], 0.0, (self.drvb,))
        onesf, onesfb = self.tmp("f32")
        self.vmemset(onesf[:, 0:128], 1.0, (onesfb,))
        self.mm(ps2[:, 0:2], onesf[:, 0:128], self.drv[:, 40:42], True, True, (onesfb, self.drvb), (ps2b,))
        self.actf(self.drv[:, 42:44], ps2[:, 0:2], AF.Exp, (ps2b,), (self.drvb,))
        lam_init = 0.8 - 0.6 * math.exp(-0.3 * l)
        self.tt(self.drv[:, 44:45], self.drv[:, 43:44], self.drv[:, 42:43], ALU.subtract, (self.drvb,), (self.drvb,))
        self.ts(self.drv[:, 44:45], self.drv[:, 44:45], -lam_init, None, ALU.add, None, (self.drvb,), (self.drvb,))
        self.ts(self.drv[:, 45:46], self.pvc(l, PV_SUBLN), 1.0 - lam_init, None, ALU.mult, None, (self.pvb,), (self.drvb,))
        self.dump(f"modv{l}", self.modv[:, :], [128, 96], (self.modb,))
        self.dump(f"drv{l}", self.drv[:, :], [128, 64], (self.drvb,))

    def mcol(self, i, dc, j2):
        c = (i * 8 + dc) * 2 + j2
        return self.modv[:, c:c + 1]

    def adaln(self, l, which, last):
        cb = self.cb
        moe = (which == 1 and l == 1)
        if moe:
            m = self.mark()
            self.sp_off = self.ffn_base
            self.wrpad = self.alloc([128, 8, 128], F32, "wrpad")
            self.wrpadb = Buf("wrpad")
            self.h32 = [(self.alloc([128, 512], F32, "h32"), Buf("h32")) for _ in range(3)]
            self.release(m)
            self.vmemset(self.wrpad[:, :, :], 0.0, (self.wrpadb,))
            self.dma(self.sp, self.wrpad[:, :, 0:8], self.moe_r[0].rearrange("(k p) e -> p k e", p=128), (), (self.wrpadb,), self.ldsem())
        for tc, (t0, n) in enumerate(TCS):
            if tc == 4 and (which == 1 and last):
                continue
            j2 = 1 if tc == 4 else 0
            ss, ssb = self.newps()
            for dc in range(8):
                sq, sqb = self.tmp("bf")
                self.actf(sq[:, 0:n], self.lat_ap(dc, tc), AF.Square, (self.latb[dc][tc],), (sqb,))
                self.mm(ss[:, 0:n], cb[:, C_ONES:C_ONES + 128], sq[:, 0:n], dc == 0, dc == 7, (self.cbb, sqb), (ssb,))
            rt, rtb = self.tmp("f32")
            self.actf(rt[:, 0:n], ss[:, 0:n], AF.Sqrt, (ssb,), (rtb,), scale=1.0 / D, bias=EPS)
            rs, rsb = self.tmp("rs")
            self.recip(rs[:, 0:n], rt[:, 0:n], (rtb,), (rsb,), exact=True)
            if moe:
                lp, lpb = self.newps()
            for dc in range(8):
                acol = (which * 8 + dc) * 2 + j2
                t32, t32b = self.tmp("f32")
                self.stt(t32[:, 0:n], self.lat_ap(dc, tc), self.drv[:, acol:acol + 1], rs[:, 0:n], ALU.mult, ALU.mult, (self.latb[dc][tc], self.drvb, rsb), (t32b,))
                bias = self.mcol(3 if which else 0, dc, j2)
                if not moe:
                    self.actf(self.hT[:, dc, t0:t0 + n], t32[:, 0:n], AF.Identity, (t32b, self.modb), (self.hTb[dc][tc],), bias=bias)
                else:
                    h32, h32b = self.h32[dc % 3]
                    self.actf(h32[:, 0:n], t32[:, 0:n], AF.Identity, (t32b, self.modb), (h32b,), bias=bias)
                    self.vcopy(self.hT[:, dc, t0:t0 + n], h32[:, 0:n], (h32b,), (self.hTb[dc][tc],))
                    self.mm(lp[:, 0:n], self.wrpad[:, dc, :], h32[:, 0:n], dc == 0, dc == 7, (self.wrpadb, h32b), (lpb,))
            if moe:
                self.actf(self.LT[:, t0:t0 + n], lp[:, 0:n], AF.Copy, (lpb,), (self.LTb[tc],))
        nm = "hmix" if which == 0 else "hffn"
        self.dump(f"{nm}{l}", self.hT[:, :, :], [128, 8, NT], [b for r in self.hTb for b in r])

    def headnorm(self, ps, psb, P, n, bdcol, inv, gain, rope, t0, dest, destb, extra_reads=()):
        self.headnorm_multi([dict(ps=ps, psb=psb, P=P, n=n, bdcol=bdcol, inv=inv, gain=gain, rope=rope, t0=t0, dest=dest, destb=destb)])

    def headnorm_multi(self, tiles):
        cb = self.cb
        for T in tiles:
            P, n = T["P"], T["n"]
            T["sq"] = self.tmp("bf")
            self.actf(T["sq"][0][0:P, 0:n], T["ps"][0:P, 0:n], AF.Square, (T["psb"],), (T["sq"][1],))
        for T in tiles:
            P, n = T["P"], T["n"]
            T["ss"] = self.newps()
            self.mm(T["ss"][0][0:P, 0:n], cb[0:P, T["bdcol"]:T["bdcol"] + P], T["sq"][0][0:P, 0:n], True, True, (self.cbb, T["sq"][1]), (T["ss"][1],))
        for T in tiles:
            P, n = T["P"], T["n"]
            T["rt"] = self.tmp("f32")
            self.actf(T["rt"][0][0:P, 0:n], T["ss"][0][0:P, 0:n], AF.Ln, (T["ss"][1], self.pvb), (T["rt"][1],), scale=T["inv"], bias=EPS)
        for T in tiles:
            P, n = T["P"], T["n"]
            self.actf(T["rt"][0][0:P, 0:n], T["rt"][0][0:P, 0:n], AF.Exp, (T["rt"][1],), (T["rt"][1],), scale=-0.5)
        for T in tiles:
            P, n = T["P"], T["n"]
            if T["rope"] is None:
                self.stt(T["dest"], T["ps"][0:P, 0:n], T["gain"], T["rt"][0][0:P, 0:n], ALU.mult, ALU.mult, (T["psb"], T["rt"][1], self.pvb), (T["destb"],))
            else:
                T["qn"] = self.tmp("bf")
                self.stt(T["qn"][0][0:P, 0:n], T["ps"][0:P, 0:n], T["gain"], T["rt"][0][0:P, 0:n], ALU.mult, ALU.mult, (T["psb"], T["rt"][1], self.pvb), (T["qn"][1],))
        for T in tiles:
            if T["rope"] is None:
                continue
            P, n = T["P"], T["n"]
            rcol = T["rope"][0]
            T["rq"] = self.newps()
            self.mm(T["rq"][0][0:P, 0:n], cb[0:P, rcol:rcol + P], T["qn"][0][0:P, 0:n], True, True, (self.cbb, T["qn"][1]), (T["rq"][1],))
        for T in tiles:
            if T["rope"] is None:
                continue
            P, n, t0 = T["P"], T["n"], T["t0"]
            _, cosT, sinT = T["rope"]
            self.tt(T["rt"][0][0:P, 0:n], T["qn"][0][0:P, 0:n], cosT[0:P, t0:t0 + n], ALU.mult, (T["qn"][1], self.tabb), (T["rt"][1],))
        for T in tiles:
            if T["rope"] is None:
                continue
            P, n, t0 = T["P"], T["n"], T["t0"]
            _, cosT, sinT = T["rope"]
            T["t2"] = self.tmp("f32")
            self.tt(T["t2"][0][0:P, 0:n], T["rq"][0][0:P, 0:n], sinT[0:P, t0:t0 + n], ALU.mult, (T["rq"][1], self.tabb), (T["t2"][1],))
        for T in tiles:
            P, n = T["P"], T["n"]
            if T["rope"] is not None:
                self.tt(T["dest"], T["rt"][0][0:P, 0:n], T["t2"][0][0:P, 0:n], ALU.add, (T["rt"][1], T["t2"][1]), (T["destb"],))
            if T.get("post") is not None:
                T["post"]()

    def proj_fm(self, wt, wb, col0, M, tc, kch=8, rhs=None):
        t0, n = TCS[tc]
        ps, psb = self.newps()
        for k in range(kch):
            if rhs is None:
                r, rb = self.hT[:, k, t0:t0 + n], self.hTb[k][tc]
            else:
                r, rb = rhs(k)
            self.mm(ps[0:M, 0:n], wt[:, k, col0:col0 + M], r, k == 0, k == kch - 1, (wb, rb), (psb,))
        return ps, psb

    def load_tabs(self, i0):
        for i in range(2):
            self.dma(self.pool, self.tab[i][:, :], self.tabs[i0 + i], (), (self.tabb,), self.plsem())

    def attn_stream(self, jobs, LA=3):
        seq = [(ji, si) for ji, jb in enumerate(jobs) for si in range(len(jb["kts"]))]
        pend = {}
        accs = {}
        nit = len(seq)
        for step in range(nit + LA):
            if step < nit:
                ji, si = seq[step]
                jb = jobs[ji]
                j, c0, c1 = jb["kts"][si]
                P = jb["P"]
                S, Sb = self.newps(0, 4)
                ktc = j // 4 if j < 16 else 4
                self.mm(S[:, c0:c1], jb["KT"][0:P, j * 128:(j + 1) * 128], jb["qap"][0:P, c0:c1], True, True, (jb["Kb"][ktc], jb["qb"]), (Sb,))
                pT, pTb = self.tmp("pT")
                self.actf(pT[:, c0:c1], S[:, c0:c1], AF.Exp, (Sb,), (pTb,), scale=jb["scale"])
                if jb.get("masks") is not None:
                    for (m0, m1, mcol) in jb["masks"](j, c0, c1):
                        self.tt(pT[:, m0:m1], pT[:, m0:m1], self.cb[:, mcol:mcol + 128], ALU.mult, (pTb, self.cbb), (pTb,))
                pend[step] = (pT, pTb)
            idx = step - LA
            if idx >= 0:
                ji, si = seq[idx]
                jb = jobs[ji]
                nk = len(jb["kts"])
                if si == 0:
                    accs[ji] = [self.newps(4, 8) for _ in range(jb["nacc"])]
                j, c0, c1 = jb["kts"][si]
                pT, pTb = pend.pop(idx)
                for (acc, accb), lhsT in zip(accs[ji], jb["vfun"](j)):
                    self.mm(acc[:, c0:c1], lhsT, pT[:, c0:c1], si == 0, si == nk - 1, (jb["vb_of"](j), pTb), (accb,))
                if si == nk - 1:
                    jb["fin"](accs.pop(ji))

    def norm_out(self, acc, accb, half, n, dest, destb, sinkcol=None):
        o0 = half * 64
        s0 = (1 - half) * 64
        r2, r2b = self.tmp("f32")
        if sinkcol is not None:
            self.act_pow(r2[o0:o0 + 64, 0:n], acc[s0:s0 + 64, 0:n], -1.0, (accb, self.drvb), (r2b,), bias=self.drv[s0:s0 + 64, sinkcol:sinkcol + 1])
        else:
            self.act_pow(r2[o0:o0 + 64, 0:n], acc[s0:s0 + 64, 0:n], -1.0, (accb,), (r2b,))
        self.tt(dest, acc[o0:o0 + 64, 0:n], r2[o0:o0 + 64, 0:n], ALU.mult, (accb, r2b), (destb,))

    def merge_preload(self, l, n_br, ks, qb):
        wbv = self.w_br[l, n_br].rearrange("(k p) d -> p k d", p=128)
        wgv = self.w_in[l].rearrange("(k p) c -> p k c", p=128)
        g0 = GATE0 + n_br * D
        slot_h, slot_bs, slot_ds = self.wslots[self.w_i % len(self.wslots)]
        self.w_i += 1
        nk = len(ks)
        vb = slot_h[:, 0:nk * 256].rearrange("p (a b) -> p a b", a=nk)
        vg = slot_h[:, nk * 256:(nk + 8) * 256].rearrange("p (a b) -> p a b", a=8)
        self.dma(self.pool, vb, wbv[:, ks[0]:ks[0] + nk, qb * 256:(qb + 1) * 256], (), (slot_bs[0],), slot_ds[0], guard=(slot_bs[1],))
        self.dma(self.pool, vg, wgv[:, :, g0 + qb * 256:g0 + (qb + 1) * 256], (), (slot_bs[1],), slot_ds[1])
        return vb, slot_bs[0], vg, slot_bs[1]

    def merge(self, l, n_br, ks, brh, brbufs, tcs, first, pre=None):
        blocks = list(pre) if pre is not None else []
        for qb in range(4):
            while len(blocks) < min(4, qb + 3):
                blocks.append(self.merge_preload(l, n_br, ks, len(blocks)))
            wbt, wbb, wgt, wgb = blocks[qb]
            for tc in tcs:
                t0, n = TCS[tc]
                for d2 in range(2):
                    dc = qb * 2 + d2
                    yp, ypb = self.newps()
                    for i in range(len(ks)):
                        self.mm(yp[:, 0:n], wbt[:, i, d2 * 128:(d2 + 1) * 128], brh[i][:, t0:t0 + n], i == 0, i == len(ks) - 1, (wbb, brbufs[i][tc]), (ypb,))
                    gp, gpb = self.proj_fm(wgt, wgb, d2 * 128, 128, tc)
                    sg, sgb = self.tmp("f32")
                    self.actf(sg[:, 0:n], gp[:, 0:n], AF.Sigmoid, (gpb,), (sgb,))
                    zap = self.z[:, dc, t0:t0 + n]
                    if first:
                        self.tt(zap, yp[:, 0:n], sg[:, 0:n], ALU.mult, (ypb, sgb), (self.zb[dc][tc],))
                    else:
                        t, tb = self.tmp("f32")
                        self.tt(t[:, 0:n], yp[:, 0:n], sg[:, 0:n], ALU.mult, (ypb, sgb), (tb,))
                        self.tt(zap, zap, t[:, 0:n], ALU.add, (tb, self.zb[dc][tc]), (self.zb[dc][tc],))

    def mixer_phase(self, l, last):
        A = self.alloc
        cb = self.cb
        self.z = A([128, 8, NT], BF16, "z")
        self.zb = [[Buf(f"z{dc}_{tc}") for tc in range(5)] for dc in range(8)]
        self.mix_base = self.mark()
        self.tab = [A([128, L], BF16, f"tab{i}") for i in range(2)]
        self.tabb = Buf("tab")
        qtcs = [tc for tc in range(5) if not (last and tc == 4)]
        wv = self.w_in[l].rearrange("(k p) c -> p k c", p=128)
        inv64 = 1.0 / 64
        for mi, (qoff, koff, voff, qg, kg, windowed) in enumerate(((0, 2048, 2176, PV_AQN, PV_AKN, True), (512, 2304, 2432, PV_BQN, PV_BKN, False))):
            m0 = self.mark()
            if mi == 0:
                self.load_tabs(0)
            KT = A([128, NT], BF16, "KT")
            Kb = [Buf(f"K{tc}") for tc in range(5)]
            VA = [[A([128, 18, 128], BF16, "VA") for par in range(2)] for kv in range(2)]
            Vb = [Buf(f"V{tc}") for tc in range(5)]
            qpad = [A([128, 512], BF16, "qpad") for h in range(8)]
            qpb = [Buf(f"qp{h}") for h in range(8)]
            br = [A([128, NT], BF16, "br") for i in range(4)]
            brb = [[Buf(f"br{i}_{tc}") for tc in range(5)] for i in range(4)]
            for h in range(8):
                self.vmemset(qpad[h][:, :], 0.0, (qpb[h],))
            for kv in range(2):
                for par in range(2):
                    self.vmemset(VA[kv][par][:, :, :], 1.0, (Vb[0], Vb[1], Vb[2], Vb[3], Vb[4]))
            self.mark_phase(f'  AB{mi}_kv')
            wk, wkb = self.wload(wv[:, :, koff:koff + 128], [128, 8, 128])
            wvv, wvb = self.wload(wv[:, :, voff:voff + 128], [128, 8, 128])
            for tcg in ((0, 1), (2, 3), (4,)):
                tiles = []
                for tc in tcg:
                    t0, n = TCS[tc]
                    ps, psb = self.proj_fm(wk, wkb, 0, 128, tc)
                    rope = (C_R64, self.tab[0], self.tab[1]) if tc < 4 else None
                    tiles.append(dict(ps=ps, psb=psb, P=128, n=n, bdcol=C_BD64, inv=inv64, gain=self.pvc(l, kg), rope=rope, t0=t0, dest=KT[:, t0:t0 + n], destb=Kb[tc]))
                self.headnorm_multi(tiles)
            for tc, (t0, n) in enumerate(TCS):
                for jt in range(n // 128):
                    j = (t0 // 128) + jt
                    vp, vpb = self.newps()
                    for k in range(8):
                        self.mm(vp[:, 0:128], self.hT[:, k, j * 128:(j + 1) * 128], wvv[:, k, :], k == 0, k == 7, (self.hTb[k][tc], wvb), (vpb,))
                    for kv in range(2):
                        self.vcopy(VA[kv][0][:, j, 0:64], vp[:, kv * 64:(kv + 1) * 64], (vpb,), (Vb[tc],))
                        self.actf(VA[kv][1][:, j, 64:128], vp[:, kv * 64:(kv + 1) * 64], AF.Copy, (vpb,), (Vb[tc],))
            if mi == 0:
                self.dump(f"ka{l}", KT[:, :], [128, NT], Kb)
                self.dump(f"va{l}", VA[0][0][:, :, :], [128, 18, 128], Vb)
            nxt = self.wload(wv[:, :, qoff:qoff + 512], [128, 8, 512])
            mpre = None
            for tc in qtcs:
                t0, n = TCS[tc]
                self.mark_phase(f'  AB{mi}_q{tc}')
                wq, wqb = nxt
                rope = (C_R64, self.tab[0], self.tab[1]) if tc < 4 else None
                for ig in ((0, 1), (2, 3)):
                    tiles = []
                    for i in ig:
                        qt, qtb = self.tmp("bf")

                        def post(i=i, qt=qt, qtb=qtb, n=n):
                            for hh in range(2):
                                h = 2 * i + hh
                                kvh = h // 4
                                self.vcopy(qpad[h][kvh * 64:(kvh + 1) * 64, 0:n], qt[hh * 64:(hh + 1) * 64, 0:n], (qtb,), (qpb[h],))
                        ps, psb = self.proj_fm(wq, wqb, i * 128, 128, tc)
                        tiles.append(dict(ps=ps, psb=psb, P=128, n=n, bdcol=C_BD64, inv=inv64, gain=self.pvc(l, qg), rope=rope, t0=t0, dest=qt[:, 0:n], destb=qtb, post=post))
                    self.headnorm_multi(tiles)
                if mi == 0 and tc == 0:
                    self.dump(f"qa{l}", qpad[1][:, :], [128, 512], (qpb[1],))
                if tc != qtcs[-1]:
                    nxt = self.wload(wv[:, :, qoff:qoff + 512], [128, 8, 512])
                else:
                    mpre = [self.merge_preload(l, mi, [0, 1, 2, 3], qb_) for qb_ in range(2)]
                self.mark_phase(f'  AB{mi}_att{tc}')
                jobs = []
                for h in range(8):
                    kvh = h // 4
                    par = h % 2
                    masks = None
                    if tc == 4:
                        kts = [(16, 0, n), (17, 0, n)]
                    elif not windowed:
                        kts = [(j, 0, n) for j in range(18)]
                    else:
                        kts = [(16, 0, n), (17, 0, n)]
                        b0 = t0 // 128
                        for j in range(max(0, b0 - 1), min(16, b0 + 5)):
                            qlo = max(j - 1, b0)
                            qhi = min(j + 1, b0 + 3)
                            kts.append((j, (qlo - b0) * 128, (qhi - b0 + 1) * 128))

                        def masks(j, c0, c1, b0=b0):
                            res = []
                            for qb_ in range(b0 + c0 // 128, b0 + c1 // 128):
                                if qb_ == j - 1:
                                    res.append(((qb_ - b0) * 128, (qb_ - b0 + 1) * 128, C_MASKR))
                                elif qb_ == j + 1:
                                    res.append(((qb_ - b0) * 128, (qb_ - b0 + 1) * 128, C_MASKL))
                            return res

                    def fin(accs, h=h, par=par, n=n, t0=t0, tc=tc):
                        acc, accb = accs[0]
                        self.norm_out(acc, accb, par, n, br[h // 2][par * 64:(par + 1) * 64, t0:t0 + n], brb[h // 2][tc],
                                      sinkcol=(32 + h) if windowed else None)
                    jobs.append(dict(qap=qpad[h], qb=qpb[h], KT=KT, Kb=Kb, P=128, kts=kts,
                                     vfun=(lambda j, kvh=kvh, par=par: [VA[kvh][par][:, j, :]]),
                                     vb_of=(lambda j: Vb[j // 4 if j < 16 else 4]), scale=0.125, masks=masks, nacc=1, fin=fin))
                self.attn_stream(jobs)
                if tc == 0:
                    self.dump(f"br{mi}_{l}", br[0][:, 0:512], [128, 512], (brb[0][0],))
            self.mark_phase(f'  AB{mi}_mrg0')
            self.merge(l, mi, [0, 1, 2, 3], br, brb, qtcs, first=(mi == 0), pre=mpre)
            self.soft_barrier()
            self.release(m0)
        self.mark_phase('  C_kv')
        m0 = self.mark()
        KC = [A([128, NT], BF16, "KC") for h in range(4)]
        KCb = [[Buf(f"KC{h}_{tc}") for tc in range(5)] for h in range(4)]
        VC = A([128, 18, 512], BF16, "VC")
        VCb = [Buf(f"VC{tc}") for tc in range(5)]
        qpad = [A([128, 512], BF16, "qpadc") for h in range(8)]
        qpb = [Buf(f"qpc{h}") for h in range(8)]
        br = [A([128, NT], BF16, "brc") for i in range(4)]
        brb = [[Buf(f"brc{i}_{tc}") for tc in range(5)] for i in range(4)]
        for h in range(8):
            self.vmemset(qpad[h][:, :], 0.0, (qpb[h],))
        wk, wkb = self.wload(wv[:, :, 2560:3072], [128, 8, 512], split=2)
        for tc, (t0, n) in enumerate(TCS):
            rope = (C_R64, self.tab[0], self.tab[1]) if tc < 4 else None
            for hg in ((0, 1), (2, 3)):
                tiles = []
                for h in hg:
                    ps, psb = self.proj_fm(wk, wkb[h // 2], h * 128, 128, tc)
                    tiles.append(dict(ps=ps, psb=psb, P=128, n=n, bdcol=C_BD64, inv=inv64, gain=self.pvc(l, PV_CKN), rope=rope, t0=t0, dest=KC[h][:, t0:t0 + n], destb=KCb[h][tc]))
                self.headnorm_multi(tiles)
        wvv, wvb = self.wload(wv[:, :, 3072:3584], [128, 8, 512])
        for tc, (t0, n) in enumerate(TCS):
            for jt in range(n // 128):
                j = (t0 // 128) + jt
                vp, vpb = self.newps()
                for k in range(8):
                    self.mm(vp[:, 0:512], self.hT[:, k, j * 128:(j + 1) * 128], wvv[:, k, :], k == 0, k == 7, (self.hTb[k][tc], wvb), (vpb,))
                self.actf(VC[:, j, :], vp[:, 0:512], AF.Copy, (vpb,), (VCb[tc],))
        nxt = self.wload(wv[:, :, 1024:1536], [128, 8, 512])
        mpre = None
        for tc in qtcs:
            t0, n = TCS[tc]
            self.mark_phase(f'  C_q{tc}')
            wq, wqb = nxt
            rope = (C_R64, self.tab[0], self.tab[1]) if tc < 4 else None
            for hg in ((0, 1), (2, 3)):
                tiles = []
                for h in hg:
                    qt, qtb = self.tmp("bf")

                    def post(h=h, qt=qt, qtb=qtb, n=n):
                        for part in range(2):
                            self.vcopy(qpad[2 * h + part][part * 64:(part + 1) * 64, 0:n], qt[part * 64:(part + 1) * 64, 0:n], (qtb,), (qpb[2 * h + part],))
                    ps, psb = self.proj_fm(wq, wqb, h * 128, 128, tc)
                    tiles.append(dict(ps=ps, psb=psb, P=128, n=n, bdcol=C_BD64, inv=inv64, gain=self.pvc(l, PV_CQN), rope=rope, t0=t0, dest=qt[:, 0:n], destb=qtb, post=post))
                self.headnorm_multi(tiles)
            kts = [(j, 0, n) for j in range(18)] if tc < 4 else [(16, 0, n), (17, 0, n)]
            if tc != qtcs[-1]:
                nxt = self.wload(wv[:, :, 1024:1536], [128, 8, 512])
            else:
                mpre = [self.merge_preload(l, 2, [0, 1, 2, 3], qb_) for qb_ in range(2)]
            self.mark_phase(f'  C_att{tc}')
            jobs = []
            held = {}
            for h in range(4):
                for part in range(2):
                    def fin(accs, h=h, part=part, n=n, t0=t0, tc=tc):
                        (o, ob), (s_, sb) = accs
                        r, rb = self.tmp("f32")
                        self.act_pow(r[:, 0:n], s_[:, 0:n], -1.0, (sb,), (rb,))
                        t, tb = self.tmp("f32")
                        self.tt(t[:, 0:n], o[:, 0:n], r[:, 0:n], ALU.mult, (ob, rb), (tb,))
                        if part == 0:
                            held[h] = (t, tb)
                            return
                        t1, t1b = held.pop(h)
                        oc, ocb = self.tmp("f32")
                        self.stt(oc[:, 0:n], t[:, 0:n], self.drv[:, 44:45], t1[:, 0:n], ALU.mult, ALU.add, (t1b, tb, self.drvb), (ocb,))
                        sq, sqb = self.tmp("bf")
                        self.actf(sq[:, 0:n], oc[:, 0:n], AF.Square, (ocb,), (sqb,))
                        ss, ssb = self.newps(0, 4)
                        self.mm(ss[:, 0:n], cb[:, C_ONES:C_ONES + 128], sq[:, 0:n], True, True, (self.cbb, sqb), (ssb,))
                        rs, rsb = self.tmp("f32")
                        self.act_pow(rs[:, 0:n], ss[:, 0:n], -0.5, (ssb,), (rsb,), scale=1.0 / 128, bias=EPS)
                        self.stt(br[h][:, t0:t0 + n], oc[:, 0:n], self.drv[:, 45:46], rs[:, 0:n], ALU.mult, ALU.mult, (ocb, rsb, self.drvb), (brb[h][tc],))
                    jobs.append(dict(qap=qpad[2 * h + part], qb=qpb[2 * h + part], KT=KC[h], Kb=KCb[h], P=128, kts=kts,
                                     vfun=(lambda j, h=h: [VC[:, j, h * 128:(h + 1) * 128], cb[:, C_ONES:C_ONES + 128]]),
                                     vb_of=(lambda j: VCb[j // 4 if j < 16 else 4]), scale=0.125, masks=None, nacc=2, fin=fin))
            self.attn_stream(jobs)
            if tc == 0:
                self.dump(f"br2_{l}", br[0][:, 0:512], [128, 512], (brb[0][0],))
        self.mark_phase('  C_mrg0')
        self.merge(l, 2, [0, 1, 2, 3], br, brb, qtcs, first=False, pre=mpre)
        self.soft_barrier()
        self.release(m0)
        self.mark_phase('  D_ckv')
        self.load_tabs(2)
        m0 = self.mark()
        ckvn = A([128, 2, NT], BF16, "ckvn")
        ckvb = [Buf(f"ckvn{tc}") for tc in range(5)]
        KR = A([128, NT], BF16, "KR")
        KRb = [Buf(f"KR{tc}") for tc in range(5)]
        KD = [A([128, NT], BF16, "KD") for h in range(4)]
        VD = [A([128, 18, 128], BF16, "VD") for h in range(4)]
        qd = [A([128, 512], BF16, "qd") for h in range(4)]
        br = [A([128, NT], BF16, "brd") for i in range(2)]
        cqn = A([128, 4, 512], BF16, "cqn")
        wck, wckb = self.wload(wv[:, :, 3584:3840], [128, 8, 256])
        wkr, wkrb = self.wload(wv[:, :, 3776:3872], [128, 8, 96])
        for tc, (t0, n) in enumerate(TCS):
            raws = []
            ss, ssb = self.newps()
            for c2 in range(2):
                ps, psb = self.proj_fm(wck, wckb, c2 * 128, 128, tc)
                raw, rawb = self.tmp("f32")
                self.actf(raw[:, 0:n], ps[:, 0:n], AF.Copy, (psb,), (rawb,))
                sq, sqb = self.tmp("bf")
                self.actf(sq[:, 0:n], ps[:, 0:n], AF.Square, (psb,), (sqb,))
                self.mm(ss[:, 0:n], cb[:, C_ONES:C_ONES + 128], sq[:, 0:n], c2 == 0, c2 == 1, (self.cbb, sqb), (ssb,))
                raws.append((raw, rawb))
            rs, rsb = self.tmp("f32")
            self.act_pow(rs[:, 0:n], ss[:, 0:n], -0.5, (ssb,), (rsb,), scale=1.0 / 256, bias=EPS)
            for c2 in range(2):
                self.stt(ckvn[:, c2, t0:t0 + n], raws[c2][0][:, 0:n], self.pvc(l, PV_DKVNORM + c2), rs[:, 0:n], ALU.mult, ALU.mult, (raws[c2][1], rsb, self.pvb), (ckvb[tc],))
            ps, psb = self.proj_fm(wkr, wkrb, 0, 96, tc)
            rope = (C_R96, self.tab[0], self.tab[1]) if tc < 4 else None
            self.headnorm(ps, psb, 96, n, C_BD96, self.pvc(l, PV_INV96, rows=96), self.pvc(l, PV_DKN, rows=96), rope, t0, KR[0:96, t0:t0 + n], KRb[tc])
        self.dump(f"ckvn{l}", ckvn[:, :, :], [128, 2, NT], ckvb)
        ukv = self.d_w_ukv[l].rearrange("(k p) c -> p k c", p=128)
        uq = self.d_w_uq[l].rearrange("(k p) c -> p k c", p=128)
        for hf in range(2):
            KDb = [[Buf(f"KD{h}_{tc}") for tc in range(5)] for h in range(4)]
            VDb = [Buf(f"VD{tc}") for tc in range(5)]
            qdb = [Buf(f"qd{h}") for h in range(4)]
            brb = [[Buf(f"brd{i}_{tc}") for tc in range(5)] for i in range(2)]
            cqnb = Buf("cqn")
            for h in range(4):
                self.vmemset(VD[h][:, :, :], 1.0, VDb)
                self.vmemset(KD[h][96:128, :], 0.0, KDb[h])
                self.vmemset(qd[h][96:128, :], 0.0, (qdb[h],))
            self.mark_phase(f'  D{hf}_kv')
            wuk, wukb = self.wload(ukv[:, :, hf * 512:(hf + 1) * 512], [128, 2, 512])
            for tc, (t0, n) in enumerate(TCS):
                for hg in ((0, 1), (2, 3)):
                    tiles = []
                    for h in hg:
                        ps, psb = self.proj_fm(wuk, wukb, h * 128, 96, tc, kch=2, rhs=lambda k: (ckvn[:, k, t0:t0 + n], ckvb[tc]))

                        def post(h=h, t0=t0, n=n, tc=tc):
                            self.vcopy(KD[h][64:96, t0:t0 + n], KR[64:96, t0:t0 + n], (KRb[tc],), (KDb[h][tc],))
                        tiles.append(dict(ps=ps, psb=psb, P=96, n=n, bdcol=C_BD96, inv=self.pvc(l, PV_INV96, rows=96), gain=self.pvc(l, PV_DKN, rows=96), rope=None, t0=t0,
                                          dest=KD[h][0:96, t0:t0 + n], destb=KDb[h][tc], post=post))
                    self.headnorm_multi(tiles)
                for jt in range(n // 128):
                    j = (t0 // 128) + jt
                    vp, vpb = self.newps()
                    for k in range(2):
                        self.mm(vp[:, 0:512], ckvn[:, k, j * 128:(j + 1) * 128], wuk[:, k, :], k == 0, k == 1, (ckvb[tc], wukb), (vpb,))
                    for h in range(4):
                        par = h % 2
                        src = vp[:, h * 128 + 64:(h + 1) * 128]
                        if par == 0:
                            self.vcopy(VD[h][:, j, 0:64], src, (vpb,), (VDb[tc],))
                        else:
                            self.actf(VD[h][:, j, 64:128], src, AF.Copy, (vpb,), (VDb[tc],))
            if hf == 0:
                self.dump(f"kd{l}", KD[1][0:96, :], [96, NT], KDb[1])
                self.dump(f"vd{l}", VD[1][:, :, :], [128, 18, 128], VDb)
            nxt = (self.wload(wv[:, :, 1536:2048], [128, 8, 512]), self.wload(uq[:, :, hf * 384:(hf + 1) * 384], [128, 4, 384]))
            mpre = None
            for tc in qtcs:
                t0, n = TCS[tc]
                self.mark_phase(f'  D{hf}_q{tc}')
                (wq, wqb), (wu, wub) = nxt
                ss, ssb = self.newps()
                raws = []
                for c4 in range(4):
                    ps, psb = self.proj_fm(wq, wqb, c4 * 128, 128, tc)
                    raw, rawb = self.tmp("f32")
                    self.actf(raw[:, 0:n], ps[:, 0:n], AF.Copy, (psb,), (rawb,))
                    sq, sqb = self.tmp("bf")
                    self.actf(sq[:, 0:n], ps[:, 0:n], AF.Square, (psb,), (sqb,))
                    self.mm(ss[:, 0:n], cb[:, C_ONES:C_ONES + 128], sq[:, 0:n], c4 == 0, c4 == 3, (self.cbb, sqb), (ssb,))
                    raws.append((raw, rawb))
                rt, rtb = self.tmp("f32")
                self.act_pow(rt[:, 0:n], ss[:, 0:n], -0.5, (ssb,), (rtb,), scale=1.0 / 512, bias=EPS)
                for c4 in range(4):
                    self.stt(cqn[:, c4, 0:n], raws[c4][0][:, 0:n], self.pvc(l, PV_DQNORM + c4), rt[:, 0:n], ALU.mult, ALU.mult, (raws[c4][1], rtb, self.pvb), (cqnb,))
                rope = (C_R96, self.tab[0], self.tab[1]) if tc < 4 else None
                for hg in ((0, 1), (2, 3)):
                    tiles = []
                    for h in hg:
                        ps, psb = self.proj_fm(wu, wub, h * 96, 96, tc, kch=4, rhs=lambda k: (cqn[:, k, 0:n], cqnb))
                        tiles.append(dict(ps=ps, psb=psb, P=96, n=n, bdcol=C_BD96, inv=self.pvc(l, PV_INV96, rows=96), gain=self.pvc(l, PV_DQN, rows=96), rope=rope, t0=t0,
                                          dest=qd[h][0:96, 0:n], destb=qdb[h]))
                    self.headnorm_multi(tiles)
                if hf == 0 and tc == 0:
                    self.dump(f"qd{l}", qd[1][0:96, :], [96, 512], (qdb[1],))
                kts = [(j, 0, n) for j in range(18)] if tc < 4 else [(16, 0, n), (17, 0, n)]
                if tc != qtcs[-1]:
                    nxt = (self.wload(wv[:, :, 1536:2048], [128, 8, 512]), self.wload(uq[:, :, hf * 384:(hf + 1) * 384], [128, 4, 384]))
                else:
                    mpre = [self.merge_preload(l, 3, [2 * hf, 2 * hf + 1], qb_) for qb_ in range(2)]
                self.mark_phase(f'  D{hf}_att{tc}')
                jobs = []
                for h in range(4):
                    par = h % 2

                    def fin(accs, h=h, par=par, n=n, t0=t0, tc=tc):
                        acc, accb = accs[0]
                        self.norm_out(acc, accb, par, n, br[h // 2][par * 64:(par + 1) * 64, t0:t0 + n], brb[h // 2][tc])
                    jobs.append(dict(qap=qd[h], qb=qdb[h], KT=KD[h], Kb=KDb[h], P=128, kts=kts,
                                     vfun=(lambda j, h=h: [VD[h][:, j, :]]),
                                     vb_of=(lambda j: VDb[j // 4 if j < 16 else 4]), scale=1.0 / math.sqrt(96.0), masks=None, nacc=1, fin=fin))
                self.attn_stream(jobs)
                if tc == 0 and hf == 0:
                    self.dump(f"br3_{l}", br[0][:, 0:512], [128, 512], (brb[0][0],))
            self.mark_phase(f'  D{hf}_mrg0')
            self.merge(l, 3, [2 * hf, 2 * hf + 1], br, brb, qtcs, first=False, pre=mpre)
            self.soft_barrier()
        self.release(m0)
        self.dump(f"z{l}", self.z[:, :, :], [128, 8, NT], [b for r in self.zb for b in r])

    def wout_phase(self, l, last):
        self.sp_off = self.mix_base
        self.lat = self.alloc([128, 8, L], F32, "lat")
        self.latb = [[Buf(f"lat{dc}_{tc}") for tc in range(5)] for dc in range(8)]
        if l == 1:
            self.LT = self.alloc([128, L], F32, "LT")
            self.LTb = [Buf(f"LT{tc}") for tc in range(4)]
        self.ffn_base = self.phase_base
        ov = self.outT.rearrange("(k p) t -> p k t", p=128)
        wov = self.w_out[l].rearrange("(k p) c -> p k c", p=128)
        for hf in range(2):
            for tc, (t0, n) in enumerate(TCS[:4]):
                for d4 in range(4):
                    dc = hf * 4 + d4
                    self.dma(self.sp, self.lat[:, dc, t0:t0 + n], ov[:, dc, t0:t0 + n], (self.outb[dc][tc],), (self.latb[dc][tc],), self.ldsem())
        wblocks = []
        for qb in range(4):
            while len(wblocks) < min(4, qb + 3):
                c0 = len(wblocks) * 256
                wblocks.append(self.wload(wov[:, :, c0:c0 + 256], [128, 8, 256]))
            wt, wb = wblocks[qb]
            for tc, (t0, n) in enumerate(TCS):
                if tc == 4 and last:
                    continue
                j2 = 1 if tc == 4 else 0
                for d2 in range(2):
                    dc = qb * 2 + d2
                    ps, psb = self.newps()
                    for k in range(8):
                        self.mm(ps[:, 0:n], wt[:, k, d2 * 128:(d2 + 1) * 128], self.z[:, k, t0:t0 + n], k == 0, k == 7, (wb, self.zb[k][tc]), (psb,))
                    la = self.lat_ap(dc, tc)
                    self.stt(la, ps[:, 0:n], self.mcol(2, dc, j2), la, ALU.mult, ALU.add, (psb, self.modb, self.latb[dc][tc]), (self.latb[dc][tc],))
        self.dump(f"latmix{l}", self.lat[:, :, :], [128, 8, L], [self.latb[dc][tc] for dc in range(8) for tc in range(4)])

    def ffn_phase(self, l, last):
        moe = (l == 1)
        self.sp_off = self.ffn_base
        ntc = 4 if last else 5
        ntok = L if last else NT
        G = 4
        act = self.alloc([128, G, ntok], BF16, "act")
        actb = [[Buf(f"act{f}_{tc}") for tc in range(5)] for f in range(G)]
        if moe:
            lse = self.alloc([128, L], F32, "lse")
            m2 = self.alloc([128, L], F32, "m2")
            lseb = [Buf(f"lse{tc}") for tc in range(4)]
            m2b = [Buf(f"m2{tc}") for tc in range(4)]
            cw = [self.alloc([128, L], BF16, "cw") for _ in range(1)]
            cwb = [[Buf(f"cw{i}_{tc}") for tc in range(4)] for i in range(1)]
            for tc in range(4):
                t0, n = TCS[tc]
                for e in range(8):
                    rp, rpb = self.newps()
                    slh, slb_ = self.sel_ap(e)
                    self.mm(rp[:, 0:n], slh[:, :], self.LT[:, t0:t0 + n], True, True, (slb_, self.LTb[tc]), (rpb,))
                    if e == 0:
                        self.vcopy(lse[:, t0:t0 + n], rp[:, 0:n], (rpb,), (lseb[tc],))
                    else:
                        self.tt(lse[:, t0:t0 + n], rp[:, 0:n], lse[:, t0:t0 + n], ALU.max, (rpb, lseb[tc]), (lseb[tc],))
                for e in range(8):
                    rp, rpb = self.newps()
                    slh, slb_ = self.sel_ap(e)
                    self.mm(rp[:, 0:n], slh[:, :], self.LT[:, t0:t0 + n], True, True, (slb_, self.LTb[tc]), (rpb,))
                    ge, geb = self.tmp("f32")
                    self.tt(ge[:, 0:n], rp[:, 0:n], lse[:, t0:t0 + n], ALU.is_ge, (rpb, lseb[tc]), (geb,))
                    t2, t2b = self.tmp("f32")
                    self.stt(t2[:, 0:n], ge[:, 0:n], NEG_BIG, rp[:, 0:n], ALU.mult, ALU.add, (geb, rpb), (t2b,))
                    if e == 0:
                        self.vcopy(m2[:, t0:t0 + n], t2[:, 0:n], (t2b,), (m2b[tc],))
                    else:
                        self.tt(m2[:, t0:t0 + n], t2[:, 0:n], m2[:, t0:t0 + n], ALU.max, (t2b, m2b[tc]), (m2b[tc],))
                d, db = self.tmp("f32")
                self.tt(d[:, 0:n], m2[:, t0:t0 + n], lse[:, t0:t0 + n], ALU.subtract, (m2b[tc], lseb[tc]), (db,))
                self.actf(d[:, 0:n], d[:, 0:n], AF.Exp, (db,), (db,))
                self.actf(d[:, 0:n], d[:, 0:n], AF.Ln, (db,), (db,), bias=1.0)
                self.tt(lse[:, t0:t0 + n], lse[:, t0:t0 + n], d[:, 0:n], ALU.add, (db, lseb[tc]), (lseb[tc],))
            self.dump("m2", m2[:, :], [128, L], m2b)
            self.dump("lse", lse[:, :], [128, L], lseb)
            units = [(e, g) for e in range(8) for g in range(EXPERT_FF // 512)]
        else:
            units = [(None, g) for g in range((D_FF + 511) // 512)]
        cur_e = None
        for (e, g) in units:
            if moe:
                w1v = self.moe_w1[0, e].rearrange("(k p) c -> p k c", p=128)
                w3v = self.moe_w3[0, e].rearrange("(k p) c -> p k c", p=128)
                w2v = self.moe_w2[0, e].rearrange("(f p) d -> p f d", p=128)
                nf = 4
                if e != cur_e:
                    cur_e = e
                    cwt = cw[0]
                    cwtb = cwb[0]
                    for tc in range(4):
                        t0, n = TCS[tc]
                        rp, rpb = self.newps()
                        slh, slb_ = self.sel_ap(e)
                        self.mm(rp[:, 0:n], slh[:, :], self.LT[:, t0:t0 + n], True, True, (slb_, self.LTb[tc]), (rpb,))
                        d, db = self.tmp("f32")
                        self.tt(d[:, 0:n], rp[:, 0:n], lse[:, t0:t0 + n], ALU.subtract, (rpb, lseb[tc]), (db,))
                        self.actf(d[:, 0:n], d[:, 0:n], AF.Exp, (db,), (db,))
                        sl, slb = self.tmp("f32")
                        self.tt(sl[:, 0:n], rp[:, 0:n], m2[:, t0:t0 + n], ALU.is_ge, (rpb, m2b[tc]), (slb,))
                        self.tt(cwt[:, t0:t0 + n], d[:, 0:n], sl[:, 0:n], ALU.mult, (db, slb), (cwtb[tc],))
                    if e == 0:
                        self.dump("cw0", cwt[:, :], [128, L], cwtb)
            else:
                w1v = self.ffg[0].rearrange("(k p) c -> p k c", p=128)
                w3v = self.ffu[0].rearrange("(k p) c -> p k c", p=128)
                w2v = self.ffd[0].rearrange("(f p) d -> p f d", p=128)
                nf = min(4, (D_FF - g * 512) // 128)
            c0 = g * 512
            sp = 2 if nf == 4 else 1
            w1t, w1bs = self.wload(w1v[:, :, c0:c0 + nf * 128], [128, 8, nf * 128], split=sp)
            w3t, w3bs = self.wload(w3v[:, :, c0:c0 + nf * 128], [128, 8, nf * 128], split=sp)
            if sp == 1:
                w1bs, w3bs = [w1bs, w1bs], [w3bs, w3bs]
            for tc in range(ntc):
                t0, n = TCS[tc]
                for f in range(nf):
                    gp, gpb = self.proj_fm(w1t, w1bs[f // 2], f * 128, 128, tc)
                    up, upb = self.proj_fm(w3t, w3bs[f // 2], f * 128, 128, tc)
                    sg, sgb = self.tmp("f32")
                    self.actf(sg[:, 0:n], gp[:, 0:n], AF.Silu, (gpb,), (sgb,))
                    if moe:
                        t, tb = self.tmp("f32")
                        self.tt(t[:, 0:n], up[:, 0:n], sg[:, 0:n], ALU.mult, (upb, sgb), (tb,))
                        self.tt(act[:, f, t0:t0 + n], t[:, 0:n], cwt[:, t0:t0 + n], ALU.mult, (tb, cwtb[tc]), (actb[f][tc],))
                    else:
                        self.tt(act[:, f, t0:t0 + n], up[:, 0:n], sg[:, 0:n], ALU.mult, (upb, sgb), (actb[f][tc],))
            w2t, w2b = self.wload(w2v[:, g * 4:g * 4 + nf, :], [128, nf, 1024])
            for tc in range(ntc):
                t0, n = TCS[tc]
                j2 = 1 if tc == 4 else 0
                for dc in range(8):
                    yp, ypb = self.newps()
                    for f in range(nf):
                        self.mm(yp[:, 0:n], w2t[:, f, dc * 128:(dc + 1) * 128], act[:, f, t0:t0 + n], f == 0, f == nf - 1, (w2b, actb[f][tc]), (ypb,))
                    la = self.lat_ap(dc, tc)
                    self.stt(la, yp[:, 0:n], self.mcol(5, dc, j2), la, ALU.mult, ALU.add, (ypb, self.modb, self.latb[dc][tc]), (self.latb[dc][tc],))
        self.dump(f"lat{l}", self.lat[:, :, :], [128, 8, L], [self.latb[dc][tc] for dc in range(8) for tc in range(4)])


def _rope_tabs():
    def axial(rot_dim):
        t = np.arange(L)
        n = rot_dim // 4
        inv = np.power(np.float32(10000.0), -np.arange(n, dtype=np.float32) / np.float32(n)).astype(np.float32)
        ang = np.concatenate([(t // 64).astype(np.float32)[:, None] * inv, (t % 64).astype(np.float32)[:, None] * inv], axis=-1)
        return np.cos(ang).astype(np.float32), np.sin(ang).astype(np.float32)
    tabs = np.zeros((4, 128, L), np.float32)
    c, s = axial(64)
    for p in range(128):
        tabs[0, p] = c[:, p % 32]
        tabs[1, p] = s[:, p % 32]
    c, s = axial(32)
    tabs[2, :64] = 1.0
    for p in range(64, 96):
        tabs[2, p] = c[:, (p - 64) % 16]
        tabs[3, p] = s[:, (p - 64) % 16]
    return tabs


def _consts():
    cst = np.zeros((128, NCB), np.float32)
    cst[:, C_ONES:C_ONES + 128] = 1.0
    for p in range(128):
        for m in range(128):
            if p // 64 == m // 64:
                cst[p, C_BD64 + m] = 1.0
    for p in range(96):
        for m in range(96):
            if (p < 64) == (m < 64):
                cst[p, C_BD96 + m] = 1.0
    for m in range(128):
        if m % 64 < 32:
            cst[m + 32, C_R64 + m] = -1.0
        else:
            cst[m - 32, C_R64 + m] = 1.0
    for m in range(64, 96):
        if m < 80:
            cst[m + 16, C_R96 + m] = -1.0
        else:
            cst[m - 16, C_R96 + m] = 1.0
    k = np.arange(128)[:, None]
    i = np.arange(128)[None, :]
    cst[:, C_MASKL:C_MASKL + 128] = (k >= i)
    cst[:, C_MASKR:C_MASKR + 128] = (k <= i)
    pidx = np.broadcast_to(np.arange(128, dtype=np.float32)[:, None], (128, 128)).copy()
    return cst, pidx


def _pv(inp):
    pv = np.zeros((2, 128, NPV), np.float32)
    t2 = lambda v: np.tile(np.asarray(v, np.float32), 2)
    for l in range(2):
        pv[l, :, PV_MIXN:PV_MIXN + 8] = inp["mix_norm"][l].reshape(8, 128).T
        pv[l, :, PV_FFNN:PV_FFNN + 8] = inp["ffn_norm"][l].reshape(8, 128).T
        pv[l, :, PV_BMOD:PV_BMOD + 48] = inp["b_mod"][l].reshape(48, 128).T
        pv[l, :, PV_AQN] = t2(inp["a_qn"][l]); pv[l, :, PV_AKN] = t2(inp["a_kn"][l])
        pv[l, :, PV_BQN] = t2(inp["b_qn"][l]); pv[l, :, PV_BKN] = t2(inp["b_kn"][l])
        pv[l, :, PV_CQN] = t2(inp["c_qn"][l]); pv[l, :, PV_CKN] = t2(inp["c_kn"][l])
        pv[l, :, PV_DQNORM:PV_DQNORM + 4] = inp["d_q_norm"][l].reshape(4, 128).T
        pv[l, :, PV_DKVNORM:PV_DKVNORM + 2] = inp["d_kv_norm"][l].reshape(2, 128).T
        pv[l, 0:64, PV_DQN] = inp["d_qn_nope"][l]; pv[l, 64:96, PV_DQN] = inp["d_qn_rope"][l]
        pv[l, 0:64, PV_DKN] = inp["d_kn_nope"][l]; pv[l, 64:96, PV_DKN] = inp["d_kn_rope"][l]
        pv[l, :, PV_SUBLN] = inp["c_subln"][l]
        pv[l, :, PV_SINK:PV_SINK + 8] = np.broadcast_to(inp["a_sink"][l][None, :], (128, 8))
        pv[l, 0:64, PV_LQK + 0] = inp["c_lq1"][l]; pv[l, 0:64, PV_LQK + 1] = inp["c_lk1"][l]
        pv[l, 0:64, PV_LQK + 2] = inp["c_lq2"][l]; pv[l, 0:64, PV_LQK + 3] = inp["c_lk2"][l]
        pv[l, 0:64, PV_INV96] = 1.0 / 64; pv[l, 64:96, PV_INV96] = 1.0 / 32
    return pv


_CACHE = {}


def make_in_maps(inp, ncores=8):
    f = lambda a: np.ascontiguousarray(np.asarray(a, dtype=np.float32))
    cst, sel = _consts()
    tabs = _rope_tabs()
    pv = _pv(inp)
    shared = {k: f(inp[k]) for k in ["w_mod", "w_in", "d_w_uq", "d_w_ukv", "w_br", "w_out", "ff_w_gate", "ff_w_up", "ff_w_down",
                                      "moe_router", "moe_w1", "moe_w3", "moe_w2"]}
    shared.update(cstf=cst, pidx=sel, tabs=tabs, pv=pv)
    maps = []
    for b in range(ncores):
        m = dict(shared)
        m["xT"] = f(np.asarray(inp["x"][b]).T)
        m["ctxT"] = f(np.asarray(inp["ctx"][b]).T)
        cv = np.zeros((128, 16), np.float32)
        cv[:, 0:8] = np.asarray(inp["c"][b]).reshape(8, 128).T
        cv[:, 8:16] = np.asarray(inp["c_ctx"]).reshape(8, 128).T
        m["cvec"] = cv
        maps.append(m)
    return maps


FUSED = True


def _drop(m, layers):
    m = dict(m)
    if 0 not in layers:
        for k in ["ff_w_gate", "ff_w_up", "ff_w_down"]:
            m.pop(k)
    if 1 not in layers:
        for k in ["moe_router", "moe_w1", "moe_w3", "moe_w2"]:
            m.pop(k)
    return m


def kernel(**inputs):
    maps = make_in_maps(inputs)
    if FUSED:
        if "full" not in _CACHE:
            _CACHE["full"] = KB(layers=(0, 1))
        kb = _CACHE["full"]
        res = run_bass_kernel_spmd(kb.nc, maps, core_ids=list(range(8)))
    else:
        for key, layers in (("l0", (0,)), ("l1", (1,))):
            if key not in _CACHE:
                _CACHE[key] = KB(layers=layers)
        r0 = run_bass_kernel_spmd(_CACHE["l0"].nc, [_drop(m, (0,)) for m in maps], core_ids=list(range(8)))
        maps1 = []
        for m, r in zip(maps, r0.results):
            m1 = _drop(m, (1,))
            m1["xT"] = np.ascontiguousarray(np.asarray(r["outT"], dtype=np.float32))
            m1["ctxT"] = np.ascontiguousarray(np.asarray(r["ctxo"], dtype=np.float32))
            maps1.append(m1)
        res = run_bass_kernel_spmd(_CACHE["l1"].nc, maps1, core_ids=list(range(8)))
    out = np.stack([np.asarray(r["outT"]).T for r in res.results], axis=0)
    return np.ascontiguousarray(out.astype(np.float32))
```

```python
import math
import numpy as np
import concourse.bass as bass
import concourse.mybir as mybir
from concourse.bass_utils import run_bass_kernel_spmd

F32 = mybir.dt.float32
BF16 = mybir.dt.bfloat16
ALU = mybir.AluOpType
AF = mybir.ActivationFunctionType

D = 1024
L = 2048
CTX = 256
NT = L + CTX
IN_COLS = 7968
GATE0 = 3872
D_FF = 2816
EXPERT_FF = 3584
EPS = 1e-6
TCS = [(0, 512), (512, 512), (1024, 512), (1536, 512), (2048, 256)]
NEG_BIG = -1.0e30

PV_MIXN = 0
PV_FFNN = 8
PV_BMOD = 16
PV_AQN = 64
PV_AKN = 65
PV_BQN = 66
PV_BKN = 67
PV_CQN = 68
PV_CKN = 69
PV_DQNORM = 70
PV_DKVNORM = 74
PV_DQN = 76
PV_DKN = 77
PV_SUBLN = 78
PV_SINK = 79
PV_LQK = 87
PV_INV96 = 91
NPV = 92

C_ONES = 0
C_BD64 = 128
C_BD96 = 256
C_R64 = 384
C_R96 = 512
C_MASKL = 640
C_MASKR = 768
NCB = 896


_FRONTIER = {}


class Buf:
    __slots__ = ("w", "r", "name")

    def __init__(self, name=""):
        self.w = None
        self.r = dict(_FRONTIER)
        self.name = name


class EngQ:
    def __init__(self, nc, name, h, is_pe=False):
        self.h = h
        self.sem = nc.alloc_semaphore("s_" + name)
        self.n = 0
        self.waited = {}
        self.is_pe = is_pe
        self.name = name


class DSem:
    def __init__(self, nc, name):
        self.sem = nc.alloc_semaphore(name)
        self.n = 0


class KB:
    def __init__(self, layers=(0, 1), first=True, final=True, dumps=()):
        global _FRONTIER
        _FRONTIER = {}
        self.layers = layers
        self.dumps = set(dumps)
        nc = self.nc = bass.Bass("TRN2", target_bir_lowering=False)
        self.pe = EngQ(nc, "pe", nc.tensor, True)
        self.act = EngQ(nc, "act", nc.scalar)
        self.dve = EngQ(nc, "dve", nc.vector)
        self.pool = EngQ(nc, "pool", nc.gpsimd)
        self.sp = EngQ(nc, "sp", nc.sync)
        self.engs = [self.pe, self.act, self.dve, self.pool, self.sp]
        self.dsems = []
        self.dump_outs = []
        dt = lambda n, s, k="ExternalInput": nc.dram_tensor(n, list(s), F32, kind=k).ap()
        self.xT = dt("xT", [D, L])
        self.ctxT = dt("ctxT", [D, CTX])
        self.cvec = dt("cvec", [128, 16])
        self.cstf = dt("cstf", [128, NCB])
        self.pidx_d = dt("pidx", [128, 128])
        self.tabs = dt("tabs", [4, 128, L])
        self.pv = dt("pv", [2, 128, NPV])
        self.w_mod = dt("w_mod", [2, D, 6 * D])
        self.w_in = dt("w_in", [2, D, IN_COLS])
        self.d_w_uq = dt("d_w_uq", [2, 512, 768])
        self.d_w_ukv = dt("d_w_ukv", [2, 256, 1024])
        self.w_br = dt("w_br", [2, 4, 512, D])
        self.w_out = dt("w_out", [2, D, D])
        if 0 in layers:
            self.ffg = dt("ff_w_gate", [1, D, D_FF])
            self.ffu = dt("ff_w_up", [1, D, D_FF])
            self.ffd = dt("ff_w_down", [1, D_FF, D])
        if 1 in layers:
            self.moe_r = dt("moe_router", [1, D, 8])
            self.moe_w1 = dt("moe_w1", [1, 8, D, EXPERT_FF])
            self.moe_w3 = dt("moe_w3", [1, 8, D, EXPERT_FF])
            self.moe_w2 = dt("moe_w2", [1, 8, EXPERT_FF, D])
        self.outT = dt("outT", [D, L], "ExternalOutput")
        self.ctxo = dt("ctxo", [D, CTX], "ExternalOutput")
        self.outb = [[Buf(f"out{dc}_{tc}") for tc in range(5)] for dc in range(8)]
        self.ps = [nc.alloc_psum_tensor(f"ps{i}", [128, 512], F32) for i in range(8)]
        self.psb = [Buf(f"ps{i}") for i in range(8)]
        self.ps_ctr = {}
        rem = nc.sbuf_bytes_remaining
        self.arena_size = (rem - 256) // 64 * 64
        arena = nc.alloc_sbuf_tensor("arena", [128, self.arena_size // 4], F32)
        self.arena_base = nc.lookup_mloc(arena).addr
        self.sp_off = 0
        self.uid = 0
        self.build(first, final)

    def alloc(self, shape, dtype, name="t"):
        nb = int(np.prod(shape[1:])) * (4 if dtype == F32 else 2)
        nb = (nb + 63) // 64 * 64
        off = self.sp_off
        assert off + nb <= self.arena_size, f"SBUF arena overflow: {name} {off}+{nb} > {self.arena_size}"
        self.sp_off += nb
        self.sp_max = max(getattr(self, "sp_max", 0), self.sp_off)
        self.uid += 1
        return self.nc.alloc_sbuf_tensor_at(f"{name}_{self.uid}", list(shape), dtype, offset=self.arena_base + off)

    def mark(self):
        return self.sp_off

    def release(self, m):
        self.sp_off = m

    def dsem(self, name):
        d = DSem(self.nc, name)
        self.dsems.append(d)
        return d

    def issue(self, q, fn, reads=(), writes=(), dsem=None, guard=()):
        deps = {}

        def add(s, v):
            if deps.get(s, 0) < v:
                deps[s] = v

        for b in guard:
            if b.w is not None:
                add(*b.w)
            for s_, v_ in b.r.items():
                add(s_, v_)

        for b in reads:
            if b.w is not None:
                add(*b.w)
        for b in writes:
            if b.w is not None:
                add(*b.w)
            for s, v in b.r.items():
                add(s, v)
        if dsem is not None and dsem.n > 0:
            add(dsem.sem, dsem.n)
        need = []
        for s, v in deps.items():
            if q.is_pe and s is q.sem:
                continue
            if q.waited.get(s, 0) >= v:
                continue
            need.append((s, v))
            q.waited[s] = v
        for s, v in need[:-1]:
            q.h.wait_ge(s, v)
        inst = fn()
        if need:
            inst._wait_ge(*need[-1])
        if dsem is None:
            q.n += 1
            inst.then_inc(q.sem, 1)
            ev = (q.sem, q.n)
        else:
            dsem.n += 16
            inst.then_inc(dsem.sem, 16)
            ev = (dsem.sem, dsem.n)
        for b in reads:
            if b.r.get(ev[0], 0) < ev[1]:
                b.r[ev[0]] = ev[1]
        for b in writes:
            b.w = ev
            b.r = {}
        return ev

    def soft_barrier(self):
        global _FRONTIER
        _FRONTIER = {s: v for s, v in ([(e.sem, e.n) for e in self.engs] + [(d.sem, d.n) for d in self.dsems]) if v > 0}

    def barrier(self):
        allsem = [(e.sem, e.n) for e in self.engs] + [(d.sem, d.n) for d in self.dsems]
        for q in self.engs:
            for s, v in allsem:
                if v > 0 and q.waited.get(s, 0) < v and not (q.is_pe and s is q.sem):
                    q.h.wait_ge(s, v)
                    q.waited[s] = v

    def mark_phase(self, name):
        if not hasattr(self, "phases"):
            self.phases = []
        self.phases.append((name, self.pe.n))

    def sel_ap(self, e):
        h, b = self.selbufs[self.sel_i % 2]
        self.sel_i += 1
        self.ts(h[:, :], self.pidx[:, :], float(e), None, ALU.is_equal, None, (self.pidxb,), (b,))
        return h, b

    def newps(self, lo=0, hi=8):
        key = (lo, hi)
        i = self.ps_ctr.get(key, 0)
        self.ps_ctr[key] = i + 1
        b = lo + i % (hi - lo)
        return self.ps[b], self.psb[b]

    def mm(self, out, lhsT, rhs, start, stop, reads, writes):
        return self.issue(self.pe, lambda: self.nc.tensor.matmul(out, lhsT=lhsT, rhs=rhs, start=start, stop=stop), reads, writes)

    def actf(self, out, in_, func, reads, writes, scale=1.0, bias=0.0):
        return self.issue(self.act, lambda: self.nc.scalar.activation(out=out, in_=in_, func=func, bias=bias, scale=scale), reads, writes)

    def tt(self, out, in0, in1, op, reads, writes):
        return self.issue(self.dve, lambda: self.nc.vector.tensor_tensor(out=out, in0=in0, in1=in1, op=op), reads, writes)

    def ts(self, out, in0, s1, s2, op0, op1, reads, writes):
        if op1 is None:
            return self.issue(self.dve, lambda: self.nc.vector.tensor_scalar(out=out, in0=in0, scalar1=s1, scalar2=None, op0=op0), reads, writes)
        return self.issue(self.dve, lambda: self.nc.vector.tensor_scalar(out=out, in0=in0, scalar1=s1, scalar2=s2, op0=op0, op1=op1), reads, writes)

    def stt(self, out, in0, scalar, in1, op0, op1, reads, writes):
        return self.issue(self.dve, lambda: self.nc.vector.scalar_tensor_tensor(out=out, in0=in0, scalar=scalar, in1=in1, op0=op0, op1=op1), reads, writes)

    def vcopy(self, out, in_, reads, writes):
        return self.issue(self.dve, lambda: self.nc.vector.tensor_copy(out=out, in_=in_), reads, writes)

    def recip(self, out, in_, reads, writes, exact=True):
        return self.issue(self.dve, lambda: self.nc.vector.reciprocal(out=out, in_=in_), reads, writes)

    def act_pow(self, out, in_, power, reads, writes, scale=1.0, bias=0.0):
        self.actf(out, in_, AF.Ln, reads, writes, scale=scale, bias=bias)
        return self.actf(out, out, AF.Exp, writes, writes, scale=power)

    def pmemset(self, ap, val, writes):
        return self.issue(self.pool, lambda: self.nc.gpsimd.memset(ap, val), (), writes)

    def vmemset(self, ap, val, writes):
        return self.issue(self.dve, lambda: self.nc.vector.memset(ap, val), (), writes)

    def dma(self, q, out, in_, reads, writes, dsem, guard=()):
        return self.issue(q, lambda: q.h.dma_start(out=out, in_=in_), reads, writes, dsem, guard)

    def plsem(self):
        d = self.pl_sems[self.pl_i % len(self.pl_sems)]
        self.pl_i += 1
        return d

    def ldsem(self):
        d = self.ld_sems[self.ld_i % len(self.ld_sems)]
        self.ld_i += 1
        return d

    def tmp(self, kind):
        lst = self.tmps[kind]
        i = self.tmp_i.get(kind, 0)
        self.tmp_i[kind] = i + 1
        return lst[i % len(lst)]

    def wload(self, dram_ap, shape, split=1):
        slot_h, slot_bs, slot_ds = self.wslots[self.w_i % len(self.wslots)]
        self.w_i += 1
        n = int(np.prod(shape[1:]))
        assert n <= self.WSLOT
        if len(shape) == 3:
            view = slot_h[0:shape[0], 0:n].rearrange("p (a b) -> p a b", a=shape[1])
        else:
            view = slot_h[0:shape[0], 0:n]
        if split == 1:
            self.dma(self.pool, view, dram_ap, (), tuple(slot_bs), slot_ds[0])
            return view, slot_bs[0]
        h = shape[-1] // 2
        self.dma(self.pool, view[:, :, 0:h], dram_ap[:, :, 0:h], (), (slot_bs[0],), slot_ds[0], guard=(slot_bs[1],))
        self.dma(self.pool, view[:, :, h:], dram_ap[:, :, h:], (), (slot_bs[1],), slot_ds[1])
        return view, list(slot_bs)

    def dump(self, name, ap, shape, bufs):
        if name not in self.dumps:
            return
        o = self.nc.dram_tensor("dbg_" + name, list(shape), F32 if ap.dtype == F32 else BF16, kind="ExternalOutput").ap()
        self.dump_outs.append("dbg_" + name)
        self.dma(self.sp, o, ap, bufs, (), self.ldsem())

    def build(self, first, final):
        nc = self.nc
        A = self.alloc
        self.cb = A([128, NCB], BF16, "cb")
        self.cbb = Buf("cb")
        self.pidx = A([128, 128], F32, "pidx")
        self.pidxb = Buf("pidx")
        self.selbufs = [(A([128, 128], F32, "selbuf"), Buf("selbuf")) for _ in range(2)]
        self.sel_i = 0
        self.pvt = A([128, 2 * NPV], F32, "pv")
        self.pvb = Buf("pv")
        self.cv = A([128, 16], F32, "cv")
        self.cvb = Buf("cv")
        self.modv = A([128, 96], F32, "modv")
        self.modb = Buf("modv")
        self.drv = A([128, 64], F32, "drv")
        self.drvb = Buf("drv")
        self.latc = A([128, 8, CTX], F32, "latc")
        self.hT = A([128, 8, NT], BF16, "hT")
        self.hTb = [[Buf(f"hT{dc}_{tc}") for tc in range(5)] for dc in range(8)]
        self.WSLOT = 4096
        self.wslots = [(A([128, self.WSLOT], BF16, f"w{i}"), [Buf(f"w{i}a"), Buf(f"w{i}b")], [self.dsem(f"wd{i}a"), self.dsem(f"wd{i}b")]) for i in range(3)]
        self.w_i = 0
        self.ld_sems = [self.dsem(f"ld{i}") for i in range(8)]
        self.ld_i = 0
        self.pl_sems = [self.dsem(f"pl{i}") for i in range(2)]
        self.pl_i = 0
        self.tmps = {
            "f32": [(A([128, 512], F32, "tf"), Buf("tf")) for _ in range(5)],
            "bf": [(A([128, 512], BF16, "tb"), Buf("tb")) for _ in range(6)],
            "pT": [(A([128, 512], BF16, "pT"), Buf("pT")) for _ in range(4)],
            "rs": [(A([128, 512], F32, "rs"), Buf("rs")) for _ in range(2)],
        }
        self.tmp_i = {}
        self.phase_base = self.mark()
        self.dma(self.pool, self.cb[:, :], self.cstf, (), (self.cbb,), self.plsem())
        self.dma(self.sp, self.pidx[:, :], self.pidx_d, (), (self.pidxb,), self.ldsem())
        self.dma(self.sp, self.pvt[:, :].rearrange("p (l n) -> p l n", l=2), self.pv.rearrange("l p n -> p l n"), (), (self.pvb,), self.ldsem())
        self.dma(self.sp, self.cv[:, :], self.cvec, (), (self.cvb,), self.ldsem())
        self.lat = A([128, 8, L], F32, "lat")
        self.latb = [[Buf(f"lat{dc}_{tc}") for tc in range(5)] for dc in range(8)]
        self.lat_end = self.mark()
        self.lat_at_base = True
        xv = self.xT.rearrange("(k p) t -> p k t", p=128)
        cxv = self.ctxT.rearrange("(k p) t -> p k t", p=128)
        for tc, (t0, n) in enumerate(TCS):
            for dc in range(8):
                if tc < 4:
                    self.dma(self.sp, self.lat[:, dc, t0:t0 + n], xv[:, dc, t0:t0 + n], (), (self.latb[dc][tc],), self.ldsem())
                else:
                    self.dma(self.sp, self.latc[:, dc, :], cxv[:, dc, :], (), (self.latb[dc][tc],), self.ldsem())
        for l in self.layers:
            last = (l == 1)
            self.mark_phase(f'mod{l}')
            rs5 = self.adaln_stats(l)
            self.modulation(l)
            self.mark_phase(f'adaln0_{l}')
            self.adaln(l, which=0, last=last, rs_pre=rs5)
            self.store_lat()
            self.soft_barrier()
            self.release(self.phase_base)
            self.mark_phase(f'mixer{l}')
            self.mixer_phase(l, last)
            self.mark_phase(f'wout{l}')
            self.soft_barrier()
            self.wout_phase(l, last)
            self.soft_barrier()
            self.mark_phase(f'adaln1_{l}')
            self.adaln(l, which=1, last=last)
            self.soft_barrier()
            self.mark_phase(f'ffn{l}')
            self.ffn_phase(l, last)
            self.mark_phase(f'end{l}')
            self.soft_barrier()
        self.store_lat(final=True)
        for d in self.dsems:
            if d.n > 0 and self.sp.waited.get(d.sem, 0) < d.n:
                self.sp.h.wait_ge(d.sem, d.n)
                self.sp.waited[d.sem] = d.n

    def lat_ap(self, dc, tc):
        t0, n = TCS[tc]
        return self.lat[:, dc, t0:t0 + n] if tc < 4 else self.latc[:, dc, :]

    def pvc(self, l, col, rows=128, n=1):
        return self.pvt[0:rows, l * NPV + col:l * NPV + col + n]

    def store_lat(self, final=False):
        ov = self.outT.rearrange("(k p) t -> p k t", p=128)
        cov = self.ctxo.rearrange("(k p) t -> p k t", p=128)
        for tc, (t0, n) in enumerate(TCS):
            for dc in range(8):
                if tc < 4:
                    self.dma(self.sp, ov[:, dc, t0:t0 + n], self.lat[:, dc, t0:t0 + n], (self.latb[dc][tc],), (self.outb[dc][tc],), self.ldsem())
                elif final:
                    self.dma(self.sp, cov[:, dc, :], self.latc[:, dc, :], (self.latb[dc][tc],), (self.outb[dc][tc],), self.ldsem())

    def modulation(self, l):
        sc32, sc32b = self.tmp("f32")
        scb, scbb = self.tmp("bf")
        self.actf(sc32[:, 0:16], self.cv[:, :], AF.Silu, (self.cvb,), (sc32b,))
        self.vcopy(scb[:, 0:16].rearrange("p (k j) -> p k j", j=2), sc32[:, 0:16].rearrange("p (j k) -> p k j", j=2), (sc32b,), (scbb,))
        wv = self.w_mod[l].rearrange("(k p) c -> p k c", p=128)
        ps, psb = self.newps()
        for cb_ in range(12):
            wt, wb = self.wload(wv[:, :, cb_ * 512:(cb_ + 1) * 512], [128, 8, 512])
            for jj in range(4):
                j = cb_ * 4 + jj
                for k in range(8):
                    self.mm(ps[:, 2 * j:2 * j + 2], wt[:, k, jj * 128:(jj + 1) * 128], scb[:, 2 * k:2 * k + 2], k == 0, k == 7, (wb, scbb), (psb,))
        bm = self.pvc(l, PV_BMOD, n=48)
        for j2 in range(2):
            self.tt(self.modv[:, :].rearrange("p (j t) -> p j t", t=2)[:, :, j2], ps[:, 0:96].rearrange("p (j t) -> p j t", t=2)[:, :, j2], bm, ALU.add, (psb, self.pvb), (self.modb,))
        mv = self.modv[:, :].rearrange("p (j t) -> p j t", t=2)
        dv = self.drv[:, 0:32].rearrange("p (a d t) -> p a d t", a=2, t=2)
        for j2 in range(2):
            for a, (mi, gcol) in enumerate(((1, PV_MIXN), (4, PV_FFNN))):
                self.stt(dv[:, a, :, j2], mv[:, mi * 8:mi * 8 + 8, j2], 1.0, self.pvc(l, gcol, n=8), ALU.add, ALU.mult, (self.modb, self.pvb), (self.drvb,))
        self.actf(self.drv[:, 32:40], self.pvc(l, PV_SINK, n=8), AF.Exp, (self.pvb,), (self.drvb,))
        self.tt(self.drv[:, 40:42], self.pvc(l, PV_LQK, n=4).rearrange("p (a b) -> p a b", b=2)[:, :, 0], self.pvc(l, PV_LQK, n=4).rearrange("p (a b) -> p a b", b=2)[:, :, 1], ALU.mult, (self.pvb,), (self.drvb,))
        ps2, ps2b = self.newps()
        self.vmemset(self.drv[:, 48:64], 0.0, (self.drvb,))
        onesf, onesfb = self.tmp("f32")
        self.vmemset(onesf[:, 0:128], 1.0, (onesfb,))
        self.mm(ps2[:, 0:2], onesf[:, 0:128], self.drv[:, 40:42], True, True, (onesfb, self.drvb), (ps2b,))
        self.actf(self.drv[:, 42:44], ps2[:, 0:2], AF.Exp, (ps2b,), (self.drvb,))
        lam_init = 0.8 - 0.6 * math.exp(-0.3 * l)
        self.tt(self.drv[:, 44:45], self.drv[:, 43:44], self.drv[:, 42:43], ALU.subtract, (self.drvb,), (self.drvb,))
        self.ts(self.drv[:, 44:45], self.drv[:, 44:45], -lam_init, None, ALU.add, None, (self.drvb,), (self.drvb,))
        self.ts(self.drv[:, 45:46], self.pvc(l, PV_SUBLN), 1.0 - lam_init, None, ALU.mult, None, (self.pvb,), (self.drvb,))
        self.dump(f"modv{l}", self.modv[:, :], [128, 96], (self.modb,))
        self.dump(f"drv{l}", self.drv[:, :], [128, 64], (self.drvb,))

    def mcol(self, i, dc, j2):
        c = (i * 8 + dc) * 2 + j2
        return self.modv[:, c:c + 1]

    def adaln_stats(self, l):
        cb = self.cb
        m = self.mark()
        self.sp_off = self.lat_end if self.lat_at_base else self.phase_base
        out = []
        for tc in range(5):
            out.append((self.alloc([128, 512], F32, "rs5"), Buf(f"rs5_{tc}")))
        self.release(m)
        for tc, (t0, n) in enumerate(TCS):
            ss, ssb = self.newps()
            for dc in range(8):
                sq, sqb = self.tmp("bf")
                self.actf(sq[:, 0:n], self.lat_ap(dc, tc), AF.Square, (self.latb[dc][tc],), (sqb,))
                self.mm(ss[:, 0:n], cb[:, C_ONES:C_ONES + 128], sq[:, 0:n], dc == 0, dc == 7, (self.cbb, sqb), (ssb,))
            rt, rtb = self.tmp("f32")
            self.actf(rt[:, 0:n], ss[:, 0:n], AF.Sqrt, (ssb,), (rtb,), scale=1.0 / D, bias=EPS)
            rs, rsb = out[tc]
            self.recip(rs[:, 0:n], rt[:, 0:n], (rtb,), (rsb,), exact=True)
        return out

    def adaln(self, l, which, last, rs_pre=None):
        cb = self.cb
        moe = (which == 1 and l == 1)
        if moe:
            m = self.mark()
            self.sp_off = self.ffn_base
            self.wrpad = self.alloc([128, 8, 128], F32, "wrpad")
            self.wrpadb = Buf("wrpad")
            self.h32 = [(self.alloc([128, 512], F32, "h32"), Buf("h32")) for _ in range(3)]
            self.release(m)
            self.vmemset(self.wrpad[:, :, :], 0.0, (self.wrpadb,))
            self.dma(self.sp, self.wrpad[:, :, 0:8], self.moe_r[0].rearrange("(k p) e -> p k e", p=128), (), (self.wrpadb,), self.ldsem())
        for tc, (t0, n) in enumerate(TCS):
            if tc == 4 and (which == 1 and last):
                continue
            j2 = 1 if tc == 4 else 0
            if rs_pre is not None:
                rs, rsb = rs_pre[tc]
            else:
                ss, ssb = self.newps()
                for dc in range(8):
                    sq, sqb = self.tmp("bf")
                    self.actf(sq[:, 0:n], self.lat_ap(dc, tc), AF.Square, (self.latb[dc][tc],), (sqb,))
                    self.mm(ss[:, 0:n], cb[:, C_ONES:C_ONES + 128], sq[:, 0:n], dc == 0, dc == 7, (self.cbb, sqb), (ssb,))
                rt, rtb = self.tmp("f32")
                self.actf(rt[:, 0:n], ss[:, 0:n], AF.Sqrt, (ssb,), (rtb,), scale=1.0 / D, bias=EPS)
                rs, rsb = self.tmp("rs")
                self.recip(rs[:, 0:n], rt[:, 0:n], (rtb,), (rsb,), exact=True)
            if moe:
                lp, lpb = self.newps()
            for dc in range(8):
                acol = (which * 8 + dc) * 2 + j2
                t32, t32b = self.tmp("f32")
                self.stt(t32[:, 0:n], self.lat_ap(dc, tc), self.drv[:, acol:acol + 1], rs[:, 0:n], ALU.mult, ALU.mult, (self.latb[dc][tc], self.drvb, rsb), (t32b,))
                bias = self.mcol(3 if which else 0, dc, j2)
                if not moe:
                    self.actf(self.hT[:, dc, t0:t0 + n], t32[:, 0:n], AF.Identity, (t32b, self.modb), (self.hTb[dc][tc],), bias=bias)
                else:
                    h32, h32b = self.h32[dc % 3]
                    self.actf(h32[:, 0:n], t32[:, 0:n], AF.Identity, (t32b, self.modb), (h32b,), bias=bias)
                    self.vcopy(self.hT[:, dc, t0:t0 + n], h32[:, 0:n], (h32b,), (self.hTb[dc][tc],))
                    self.mm(lp[:, 0:n], self.wrpad[:, dc, :], h32[:, 0:n], dc == 0, dc == 7, (self.wrpadb, h32b), (lpb,))
            if moe:
                self.actf(self.LT[:, t0:t0 + n], lp[:, 0:n], AF.Copy, (lpb,), (self.LTb[tc],))
        nm = "hmix" if which == 0 else "hffn"
        self.dump(f"{nm}{l}", self.hT[:, :, :], [128, 8, NT], [b for r in self.hTb for b in r])

    def headnorm(self, ps, psb, P, n, bdcol, inv, gain, rope, t0, dest, destb, extra_reads=()):
        self.headnorm_multi([dict(ps=ps, psb=psb, P=P, n=n, bdcol=bdcol, inv=inv, gain=gain, rope=rope, t0=t0, dest=dest, destb=destb)])

    def headnorm_multi(self, tiles):
        cb = self.cb
        for T in tiles:
            P, n = T["P"], T["n"]
            T["sq"] = self.tmp("bf")
            self.actf(T["sq"][0][0:P, 0:n], T["ps"][0:P, 0:n], AF.Square, (T["psb"],), (T["sq"][1],))
        for T in tiles:
            P, n = T["P"], T["n"]
            T["ss"] = self.newps()
            self.mm(T["ss"][0][0:P, 0:n], cb[0:P, T["bdcol"]:T["bdcol"] + P], T["sq"][0][0:P, 0:n], True, True, (self.cbb, T["sq"][1]), (T["ss"][1],))
        for T in tiles:
            P, n = T["P"], T["n"]
            T["rt"] = self.tmp("f32")
            self.actf(T["rt"][0][0:P, 0:n], T["ss"][0][0:P, 0:n], AF.Ln, (T["ss"][1], self.pvb), (T["rt"][1],), scale=T["inv"], bias=EPS)
        for T in tiles:
            P, n = T["P"], T["n"]
            self.actf(T["rt"][0][0:P, 0:n], T["rt"][0][0:P, 0:n], AF.Exp, (T["rt"][1],), (T["rt"][1],), scale=-0.5)
        for T in tiles:
            P, n = T["P"], T["n"]
            if T["rope"] is None:
                self.stt(T["dest"], T["ps"][0:P, 0:n], T["gain"], T["rt"][0][0:P, 0:n], ALU.mult, ALU.mult, (T["psb"], T["rt"][1], self.pvb), (T["destb"],))
            else:
                T["qn"] = self.tmp("bf")
                self.stt(T["qn"][0][0:P, 0:n], T["ps"][0:P, 0:n], T["gain"], T["rt"][0][0:P, 0:n], ALU.mult, ALU.mult, (T["psb"], T["rt"][1], self.pvb), (T["qn"][1],))
        for T in tiles:
            if T["rope"] is None:
                continue
            P, n = T["P"], T["n"]
            rcol = T["rope"][0]
            T["rq"] = self.newps()
            self.mm(T["rq"][0][0:P, 0:n], cb[0:P, rcol:rcol + P], T["qn"][0][0:P, 0:n], True, True, (self.cbb, T["qn"][1]), (T["rq"][1],))
        for T in tiles:
            if T["rope"] is None:
                continue
            P, n, t0 = T["P"], T["n"], T["t0"]
            _, cosT, sinT = T["rope"]
            self.tt(T["rt"][0][0:P, 0:n], T["qn"][0][0:P, 0:n], cosT[0:P, t0:t0 + n], ALU.mult, (T["qn"][1], self.tabb), (T["rt"][1],))
        for T in tiles:
            if T["rope"] is None:
                continue
            P, n, t0 = T["P"], T["n"], T["t0"]
            _, cosT, sinT = T["rope"]
            T["t2"] = self.tmp("f32")
            self.tt(T["t2"][0][0:P, 0:n], T["rq"][0][0:P, 0:n], sinT[0:P, t0:t0 + n], ALU.mult, (T["rq"][1], self.tabb), (T["t2"][1],))
        for T in tiles:
            P, n = T["P"], T["n"]
            if T["rope"] is not None:
                self.tt(T["dest"], T["rt"][0][0:P, 0:n], T["t2"][0][0:P, 0:n], ALU.add, (T["rt"][1], T["t2"][1]), (T["destb"],))
            if T.get("post") is not None:
                T["post"]()

    def proj_fm(self, wt, wb, col0, M, tc, kch=8, rhs=None):
        t0, n = TCS[tc]
        ps, psb = self.newps()
        for k in range(kch):
            if rhs is None:
                r, rb = self.hT[:, k, t0:t0 + n], self.hTb[k][tc]
            else:
                r, rb = rhs(k)
            self.mm(ps[0:M, 0:n], wt[:, k, col0:col0 + M], r, k == 0, k == kch - 1, (wb, rb), (psb,))
        return ps, psb

    def load_tabs(self, i0):
        for i in range(2):
            self.dma(self.pool, self.tab[i][:, :], self.tabs[i0 + i], (), (self.tabb,), self.plsem())

    def attn_stream(self, jobs, LA=3):
        seq = [(ji, si) for ji, jb in enumerate(jobs) for si in range(len(jb["kts"]))]
        pend = {}
        accs = {}
        nit = len(seq)
        for step in range(nit + LA):
            if step < nit:
                ji, si = seq[step]
                jb = jobs[ji]
                j, c0, c1 = jb["kts"][si]
                P = jb["P"]
                S, Sb = self.newps(0, 4)
                ktc = j // 4 if j < 16 else 4
                self.mm(S[:, c0:c1], jb["KT"][0:P, j * 128:(j + 1) * 128], jb["qap"][0:P, c0:c1], True, True, (jb["Kb"][ktc], jb["qb"]), (Sb,))
                pT, pTb = self.tmp("pT")
                self.actf(pT[:, c0:c1], S[:, c0:c1], AF.Exp, (Sb,), (pTb,), scale=jb["scale"])
                if jb.get("masks") is not None:
                    for (m0, m1, mcol) in jb["masks"](j, c0, c1):
                        self.tt(pT[:, m0:m1], pT[:, m0:m1], self.cb[:, mcol:mcol + 128], ALU.mult, (pTb, self.cbb), (pTb,))
                pend[step] = (pT, pTb)
            idx = step - LA
            if idx >= 0:
                ji, si = seq[idx]
                jb = jobs[ji]
                nk = len(jb["kts"])
                if si == 0:
                    accs[ji] = [self.newps(4, 8) for _ in range(jb["nacc"])]
                j, c0, c1 = jb["kts"][si]
                pT, pTb = pend.pop(idx)
                for (acc, accb), lhsT in zip(accs[ji], jb["vfun"](j)):
                    self.mm(acc[:, c0:c1], lhsT, pT[:, c0:c1], si == 0, si == nk - 1, (jb["vb_of"](j), pTb), (accb,))
                if si == nk - 1:
                    jb["fin"](accs.pop(ji))

    def norm_out(self, acc, accb, half, n, dest, destb, sinkcol=None):
        o0 = half * 64
        s0 = (1 - half) * 64
        r2, r2b = self.tmp("f32")
        if sinkcol is not None:
            self.act_pow(r2[o0:o0 + 64, 0:n], acc[s0:s0 + 64, 0:n], -1.0, (accb, self.drvb), (r2b,), bias=self.drv[s0:s0 + 64, sinkcol:sinkcol + 1])
        else:
            self.act_pow(r2[o0:o0 + 64, 0:n], acc[s0:s0 + 64, 0:n], -1.0, (accb,), (r2b,))
        self.tt(dest, acc[o0:o0 + 64, 0:n], r2[o0:o0 + 64, 0:n], ALU.mult, (accb, r2b), (destb,))

    def merge_preload(self, l, n_br, ks, qb):
        wbv = self.w_br[l, n_br].rearrange("(k p) d -> p k d", p=128)
        wgv = self.w_in[l].rearrange("(k p) c -> p k c", p=128)
        g0 = GATE0 + n_br * D
        slot_h, slot_bs, slot_ds = self.wslots[self.w_i % len(self.wslots)]
        self.w_i += 1
        nk = len(ks)
        vb = slot_h[:, 0:nk * 256].rearrange("p (a b) -> p a b", a=nk)
        vg = slot_h[:, nk * 256:(nk + 8) * 256].rearrange("p (a b) -> p a b", a=8)
        self.dma(self.pool, vb, wbv[:, ks[0]:ks[0] + nk, qb * 256:(qb + 1) * 256], (), (slot_bs[0],), slot_ds[0], guard=(slot_bs[1],))
        self.dma(self.pool, vg, wgv[:, :, g0 + qb * 256:g0 + (qb + 1) * 256], (), (slot_bs[1],), slot_ds[1])
        return vb, slot_bs[0], vg, slot_bs[1]

    def merge(self, l, n_br, ks, brh, brbufs, tcs, first, pre=None):
        blocks = list(pre) if pre is not None else []
        for qb in range(4):
            while len(blocks) < min(4, qb + 3):
                blocks.append(self.merge_preload(l, n_br, ks, len(blocks)))
            wbt, wbb, wgt, wgb = blocks[qb]
            for tc in tcs:
                t0, n = TCS[tc]
                for d2 in range(2):
                    dc = qb * 2 + d2
                    yp, ypb = self.newps()
                    for i in range(len(ks)):
                        self.mm(yp[:, 0:n], wbt[:, i, d2 * 128:(d2 + 1) * 128], brh[i][:, t0:t0 + n], i == 0, i == len(ks) - 1, (wbb, brbufs[i][tc]), (ypb,))
                    gp, gpb = self.proj_fm(wgt, wgb, d2 * 128, 128, tc)
                    sg, sgb = self.tmp("f32")
                    self.actf(sg[:, 0:n], gp[:, 0:n], AF.Sigmoid, (gpb,), (sgb,))
                    zap = self.z[:, dc, t0:t0 + n]
                    if first:
                        self.tt(zap, yp[:, 0:n], sg[:, 0:n], ALU.mult, (ypb, sgb), (self.zb[dc][tc],))
                    else:
                        t, tb = self.tmp("f32")
                        self.tt(t[:, 0:n], yp[:, 0:n], sg[:, 0:n], ALU.mult, (ypb, sgb), (tb,))
                        self.tt(zap, zap, t[:, 0:n], ALU.add, (tb, self.zb[dc][tc]), (self.zb[dc][tc],))

    def mixer_phase(self, l, last):
        A = self.alloc
        cb = self.cb
        self.z = A([128, 8, NT], BF16, "z")
        self.zb = [[Buf(f"z{dc}_{tc}") for tc in range(5)] for dc in range(8)]
        self.mix_base = self.mark()
        self.tab = [A([128, L], BF16, f"tab{i}") for i in range(2)]
        self.tabb = Buf("tab")
        qtcs = [tc for tc in range(5) if not (last and tc == 4)]
        wv = self.w_in[l].rearrange("(k p) c -> p k c", p=128)
        inv64 = 1.0 / 64
        for mi, (qoff, koff, voff, qg, kg, windowed) in enumerate(((0, 2048, 2176, PV_AQN, PV_AKN, True), (512, 2304, 2432, PV_BQN, PV_BKN, False))):
            m0 = self.mark()
            if mi == 0:
                self.load_tabs(0)
            KT = A([128, NT], BF16, "KT")
            Kb = [Buf(f"K{tc}") for tc in range(5)]
            VA = [[A([128, 18, 128], BF16, "VA") for par in range(2)] for kv in range(2)]
            Vb = [Buf(f"V{tc}") for tc in range(5)]
            qpad = [A([128, 512], BF16, "qpad") for h in range(8)]
            qpb = [Buf(f"qp{h}") for h in range(8)]
            br = [A([128, NT], BF16, "br") for i in range(4)]
            brb = [[Buf(f"br{i}_{tc}") for tc in range(5)] for i in range(4)]
            for h in range(8):
                self.vmemset(qpad[h][:, :], 0.0, (qpb[h],))
            for kv in range(2):
                for par in range(2):
                    self.vmemset(VA[kv][par][:, :, :], 1.0, (Vb[0], Vb[1], Vb[2], Vb[3], Vb[4]))
            self.mark_phase(f'  AB{mi}_kv')
            wk, wkb = self.wload(wv[:, :, koff:koff + 128], [128, 8, 128])
            wvv, wvb = self.wload(wv[:, :, voff:voff + 128], [128, 8, 128])
            for tcg in ((0, 1), (2, 3), (4,)):
                tiles = []
                for tc in tcg:
                    t0, n = TCS[tc]
                    ps, psb = self.proj_fm(wk, wkb, 0, 128, tc)
                    rope = (C_R64, self.tab[0], self.tab[1]) if tc < 4 else None
                    tiles.append(dict(ps=ps, psb=psb, P=128, n=n, bdcol=C_BD64, inv=inv64, gain=self.pvc(l, kg), rope=rope, t0=t0, dest=KT[:, t0:t0 + n], destb=Kb[tc]))
                self.headnorm_multi(tiles)
            for tc, (t0, n) in enumerate(TCS):
                for jt in range(n // 128):
                    j = (t0 // 128) + jt
                    vp, vpb = self.newps()
                    for k in range(8):
                        self.mm(vp[:, 0:128], self.hT[:, k, j * 128:(j + 1) * 128], wvv[:, k, :], k == 0, k == 7, (self.hTb[k][tc], wvb), (vpb,))
                    for kv in range(2):
                        self.vcopy(VA[kv][0][:, j, 0:64], vp[:, kv * 64:(kv + 1) * 64], (vpb,), (Vb[tc],))
                        self.actf(VA[kv][1][:, j, 64:128], vp[:, kv * 64:(kv + 1) * 64], AF.Copy, (vpb,), (Vb[tc],))
            if mi == 0:
                self.dump(f"ka{l}", KT[:, :], [128, NT], Kb)
                self.dump(f"va{l}", VA[0][0][:, :, :], [128, 18, 128], Vb)
            nxt = self.wload(wv[:, :, qoff:qoff + 512], [128, 8, 512])
            mpre = None
            for tc in qtcs:
                t0, n = TCS[tc]
                self.mark_phase(f'  AB{mi}_q{tc}')
                wq, wqb = nxt
                rope = (C_R64, self.tab[0], self.tab[1]) if tc < 4 else None
                for ig in ((0, 1), (2, 3)):
                    tiles = []
                    for i in ig:
                        qt, qtb = self.tmp("bf")

                        def post(i=i, qt=qt, qtb=qtb, n=n):
                            for hh in range(2):
                                h = 2 * i + hh
                                kvh = h // 4
                                self.vcopy(qpad[h][kvh * 64:(kvh + 1) * 64, 0:n], qt[hh * 64:(hh + 1) * 64, 0:n], (qtb,), (qpb[h],))
                        ps, psb = self.proj_fm(wq, wqb, i * 128, 128, tc)
                        tiles.append(dict(ps=ps, psb=psb, P=128, n=n, bdcol=C_BD64, inv=inv64, gain=self.pvc(l, qg), rope=rope, t0=t0, dest=qt[:, 0:n], destb=qtb, post=post))
                    self.headnorm_multi(tiles)
                if mi == 0 and tc == 0:
                    self.dump(f"qa{l}", qpad[1][:, :], [128, 512], (qpb[1],))
                if tc != qtcs[-1]:
                    nxt = self.wload(wv[:, :, qoff:qoff + 512], [128, 8, 512])
                else:
                    mpre = [self.merge_preload(l, mi, [0, 1, 2, 3], qb_) for qb_ in range(2)]
                self.mark_phase(f'  AB{mi}_att{tc}')
                jobs = []
                for h in range(8):
                    kvh = h // 4
                    par = h % 2
                    masks = None
                    if tc == 4:
                        kts = [(16, 0, n), (17, 0, n)]
                    elif not windowed:
                        kts = [(j, 0, n) for j in range(18)]
                    else:
                        kts = [(16, 0, n), (17, 0, n)]
                        b0 = t0 // 128
                        for j in range(max(0, b0 - 1), min(16, b0 + 5)):
                            qlo = max(j - 1, b0)
                            qhi = min(j + 1, b0 + 3)
                            kts.append((j, (qlo - b0) * 128, (qhi - b0 + 1) * 128))

                        def masks(j, c0, c1, b0=b0):
                            res = []
                            for qb_ in range(b0 + c0 // 128, b0 + c1 // 128):
                                if qb_ == j - 1:
                                    res.append(((qb_ - b0) * 128, (qb_ - b0 + 1) * 128, C_MASKR))
                                elif qb_ == j + 1:
                                    res.append(((qb_ - b0) * 128, (qb_ - b0 + 1) * 128, C_MASKL))
                            return res

                    def fin(accs, h=h, par=par, n=n, t0=t0, tc=tc):
                        acc, accb = accs[0]
                        self.norm_out(acc, accb, par, n, br[h // 2][par * 64:(par + 1) * 64, t0:t0 + n], brb[h // 2][tc],
                                      sinkcol=(32 + h) if windowed else None)
                    jobs.append(dict(qap=qpad[h], qb=qpb[h], KT=KT, Kb=Kb, P=128, kts=kts,
                                     vfun=(lambda j, kvh=kvh, par=par: [VA[kvh][par][:, j, :]]),
                                     vb_of=(lambda j: Vb[j // 4 if j < 16 else 4]), scale=0.125, masks=masks, nacc=1, fin=fin))
                self.attn_stream(jobs)
                if tc == 0:
                    self.dump(f"br{mi}_{l}", br[0][:, 0:512], [128, 512], (brb[0][0],))
            self.mark_phase(f'  AB{mi}_mrg0')
            self.merge(l, mi, [0, 1, 2, 3], br, brb, qtcs, first=(mi == 0), pre=mpre)
            self.soft_barrier()
            self.release(m0)
        self.mark_phase('  C_kv')
        m0 = self.mark()
        KC = [A([128, NT], BF16, "KC") for h in range(4)]
        KCb = [[Buf(f"KC{h}_{tc}") for tc in range(5)] for h in range(4)]
        VC = A([128, 18, 512], BF16, "VC")
        VCb = [Buf(f"VC{tc}") for tc in range(5)]
        qpad = [A([128, 512], BF16, "qpadc") for h in range(8)]
        qpb = [Buf(f"qpc{h}") for h in range(8)]
        br = [A([128, NT], BF16, "brc") for i in range(4)]
        brb = [[Buf(f"brc{i}_{tc}") for tc in range(5)] for i in range(4)]
        for h in range(8):
            self.vmemset(qpad[h][:, :], 0.0, (qpb[h],))
        wk, wkb = self.wload(wv[:, :, 2560:3072], [128, 8, 512], split=2)
        for tc, (t0, n) in enumerate(TCS):
            rope = (C_R64, self.tab[0], self.tab[1]) if tc < 4 else None
            for hg in ((0, 1), (2, 3)):
                tiles = []
                for h in hg:
                    ps, psb = self.proj_fm(wk, wkb[h // 2], h * 128, 128, tc)
                    tiles.append(dict(ps=ps, psb=psb, P=128, n=n, bdcol=C_BD64, inv=inv64, gain=self.pvc(l, PV_CKN), rope=rope, t0=t0, dest=KC[h][:, t0:t0 + n], destb=KCb[h][tc]))
                self.headnorm_multi(tiles)
        wvv, wvb = self.wload(wv[:, :, 3072:3584], [128, 8, 512])
        for tc, (t0, n) in enumerate(TCS):
            for jt in range(n // 128):
                j = (t0 // 128) + jt
                vp, vpb = self.newps()
                for k in range(8):
                    self.mm(vp[:, 0:512], self.hT[:, k, j * 128:(j + 1) * 128], wvv[:, k, :], k == 0, k == 7, (self.hTb[k][tc], wvb), (vpb,))
                self.actf(VC[:, j, :], vp[:, 0:512], AF.Copy, (vpb,), (VCb[tc],))
        nxt = self.wload(wv[:, :, 1024:1536], [128, 8, 512])
        mpre = None
        for tc in qtcs:
            t0, n = TCS[tc]
            self.mark_phase(f'  C_q{tc}')
            wq, wqb = nxt
            rope = (C_R64, self.tab[0], self.tab[1]) if tc < 4 else None
            for hg in ((0, 1), (2, 3)):
                tiles = []
                for h in hg:
                    qt, qtb = self.tmp("bf")

                    def post(h=h, qt=qt, qtb=qtb, n=n):
                        for part in range(2):
                            self.vcopy(qpad[2 * h + part][part * 64:(part + 1) * 64, 0:n], qt[part * 64:(part + 1) * 64, 0:n], (qtb,), (qpb[2 * h + part],))
                    ps, psb = self.proj_fm(wq, wqb, h * 128, 128, tc)
                    tiles.append(dict(ps=ps, psb=psb, P=128, n=n, bdcol=C_BD64, inv=inv64, gain=self.pvc(l, PV_CQN), rope=rope, t0=t0, dest=qt[:, 0:n], destb=qtb, post=post))
                self.headnorm_multi(tiles)
            kts = [(j, 0, n) for j in range(18)] if tc < 4 else [(16, 0, n), (17, 0, n)]
            if tc != qtcs[-1]:
                nxt = self.wload(wv[:, :, 1024:1536], [128, 8, 512])
            else:
                mpre = [self.merge_preload(l, 2, [0, 1, 2, 3], qb_) for qb_ in range(2)]
            self.mark_phase(f'  C_att{tc}')
            jobs = []
            held = {}
            for h in range(4):
                for part in range(2):
                    def fin(accs, h=h, part=part, n=n, t0=t0, tc=tc):
                        (o, ob), (s_, sb) = accs
                        r, rb = self.tmp("f32")
                        self.act_pow(r[:, 0:n], s_[:, 0:n], -1.0, (sb,), (rb,))
                        t, tb = self.tmp("f32")
                        self.tt(t[:, 0:n], o[:, 0:n], r[:, 0:n], ALU.mult, (ob, rb), (tb,))
                        if part == 0:
                            held[h] = (t, tb)
                            return
                        t1, t1b = held.pop(h)
                        oc, ocb = self.tmp("f32")
                        self.stt(oc[:, 0:n], t[:, 0:n], self.drv[:, 44:45], t1[:, 0:n], ALU.mult, ALU.add, (t1b, tb, self.drvb), (ocb,))
                        sq, sqb = self.tmp("bf")
                        self.actf(sq[:, 0:n], oc[:, 0:n], AF.Square, (ocb,), (sqb,))
                        ss, ssb = self.newps(0, 4)
                        self.mm(ss[:, 0:n], cb[:, C_ONES:C_ONES + 128], sq[:, 0:n], True, True, (self.cbb, sqb), (ssb,))
                        rs, rsb = self.tmp("f32")
                        self.act_pow(rs[:, 0:n], ss[:, 0:n], -0.5, (ssb,), (rsb,), scale=1.0 / 128, bias=EPS)
                        self.stt(br[h][:, t0:t0 + n], oc[:, 0:n], self.drv[:, 45:46], rs[:, 0:n], ALU.mult, ALU.mult, (ocb, rsb, self.drvb), (brb[h][tc],))
                    jobs.append(dict(qap=qpad[2 * h + part], qb=qpb[2 * h + part], KT=KC[h], Kb=KCb[h], P=128, kts=kts,
                                     vfun=(lambda j, h=h: [VC[:, j, h * 128:(h + 1) * 128], cb[:, C_ONES:C_ONES + 128]]),
                                     vb_of=(lambda j: VCb[j // 4 if j < 16 else 4]), scale=0.125, masks=None, nacc=2, fin=fin))
            self.attn_stream(jobs)
            if tc == 0:
                self.dump(f"br2_{l}", br[0][:, 0:512], [128, 512], (brb[0][0],))
        self.mark_phase('  C_mrg0')
        self.merge(l, 2, [0, 1, 2, 3], br, brb, qtcs, first=False, pre=mpre)
        self.soft_barrier()
        self.release(m0)
        self.mark_phase('  D_ckv')
        self.load_tabs(2)
        m0 = self.mark()
        ckvn = A([128, 2, NT], BF16, "ckvn")
        ckvb = [Buf(f"ckvn{tc}") for tc in range(5)]
        KR = A([128, NT], BF16, "KR")
        KRb = [Buf(f"KR{tc}") for tc in range(5)]
        KD = [A([128, NT], BF16, "KD") for h in range(4)]
        VD = [A([128, 18, 128], BF16, "VD") for h in range(4)]
        qd = [A([128, 512], BF16, "qd") for h in range(4)]
        br = [A([128, NT], BF16, "brd") for i in range(2)]
        cqn = A([128, 4, 512], BF16, "cqn")
        wck, wckb = self.wload(wv[:, :, 3584:3840], [128, 8, 256])
        wkr, wkrb = self.wload(wv[:, :, 3776:3872], [128, 8, 96])
        for tc, (t0, n) in enumerate(TCS):
            raws = []
            ss, ssb = self.newps()
            for c2 in range(2):
                ps, psb = self.proj_fm(wck, wckb, c2 * 128, 128, tc)
                raw, rawb = self.tmp("f32")
                self.actf(raw[:, 0:n], ps[:, 0:n], AF.Copy, (psb,), (rawb,))
                sq, sqb = self.tmp("bf")
                self.actf(sq[:, 0:n], ps[:, 0:n], AF.Square, (psb,), (sqb,))
                self.mm(ss[:, 0:n], cb[:, C_ONES:C_ONES + 128], sq[:, 0:n], c2 == 0, c2 == 1, (self.cbb, sqb), (ssb,))
                raws.append((raw, rawb))
            rs, rsb = self.tmp("f32")
            self.act_pow(rs[:, 0:n], ss[:, 0:n], -0.5, (ssb,), (rsb,), scale=1.0 / 256, bias=EPS)
            for c2 in range(2):
                self.stt(ckvn[:, c2, t0:t0 + n], raws[c2][0][:, 0:n], self.pvc(l, PV_DKVNORM + c2), rs[:, 0:n], ALU.mult, ALU.mult, (raws[c2][1], rsb, self.pvb), (ckvb[tc],))
            ps, psb = self.proj_fm(wkr, wkrb, 0, 96, tc)
            rope = (C_R96, self.tab[0], self.tab[1]) if tc < 4 else None
            self.headnorm(ps, psb, 96, n, C_BD96, self.pvc(l, PV_INV96, rows=96), self.pvc(l, PV_DKN, rows=96), rope, t0, KR[0:96, t0:t0 + n], KRb[tc])
        self.dump(f"ckvn{l}", ckvn[:, :, :], [128, 2, NT], ckvb)
        ukv = self.d_w_ukv[l].rearrange("(k p) c -> p k c", p=128)
        uq = self.d_w_uq[l].rearrange("(k p) c -> p k c", p=128)
        for hf in range(2):
            KDb = [[Buf(f"KD{h}_{tc}") for tc in range(5)] for h in range(4)]
            VDb = [Buf(f"VD{tc}") for tc in range(5)]
            qdb = [Buf(f"qd{h}") for h in range(4)]
            brb = [[Buf(f"brd{i}_{tc}") for tc in range(5)] for i in range(2)]
            cqnb = Buf("cqn")
            for h in range(4):
                self.vmemset(VD[h][:, :, :], 1.0, VDb)
                self.vmemset(KD[h][96:128, :], 0.0, KDb[h])
                self.vmemset(qd[h][96:128, :], 0.0, (qdb[h],))
            self.mark_phase(f'  D{hf}_kv')
            wuk, wukb = self.wload(ukv[:, :, hf * 512:(hf + 1) * 512], [128, 2, 512])
            for tc, (t0, n) in enumerate(TCS):
                for hg in ((0, 1), (2, 3)):
                    tiles = []
                    for h in hg:
                        ps, psb = self.proj_fm(wuk, wukb, h * 128, 96, tc, kch=2, rhs=lambda k: (ckvn[:, k, t0:t0 + n], ckvb[tc]))

                        def post(h=h, t0=t0, n=n, tc=tc):
                            self.vcopy(KD[h][64:96, t0:t0 + n], KR[64:96, t0:t0 + n], (KRb[tc],), (KDb[h][tc],))
                        tiles.append(dict(ps=ps, psb=psb, P=96, n=n, bdcol=C_BD96, inv=self.pvc(l, PV_INV96, rows=96), gain=self.pvc(l, PV_DKN, rows=96), rope=None, t0=t0,
                                          dest=KD[h][0:96, t0:t0 + n], destb=KDb[h][tc], post=post))
                    self.headnorm_multi(tiles)
                for jt in range(n // 128):
                    j = (t0 // 128) + jt
                    vp, vpb = self.newps()
                    for k in range(2):
                        self.mm(vp[:, 0:512], ckvn[:, k, j * 128:(j + 1) * 128], wuk[:, k, :], k == 0, k == 1, (ckvb[tc], wukb), (vpb,))
                    for h in range(4):
                        par = h % 2
                        src = vp[:, h * 128 + 64:(h + 1) * 128]
                        if par == 0:
                            self.vcopy(VD[h][:, j, 0:64], src, (vpb,), (VDb[tc],))
                        else:
                            self.actf(VD[h][:, j, 64:128], src, AF.Copy, (vpb,), (VDb[tc],))
            if hf == 0:
                self.dump(f"kd{l}", KD[1][0:96, :], [96, NT], KDb[1])
                self.dump(f"vd{l}", VD[1][:, :, :], [128, 18, 128], VDb)
            nxt = (self.wload(wv[:, :, 1536:2048], [128, 8, 512]), self.wload(uq[:, :, hf * 384:(hf + 1) * 384], [128, 4, 384]))
            mpre = None
            for tc in qtcs:
                t0, n = TCS[tc]
                self.mark_phase(f'  D{hf}_q{tc}')
                (wq, wqb), (wu, wub) = nxt
                ss, ssb = self.newps()
                raws = []
                for c4 in range(4):
                    ps, psb = self.proj_fm(wq, wqb, c4 * 128, 128, tc)
                    raw, rawb = self.tmp("f32")
                    self.actf(raw[:, 0:n], ps[:, 0:n], AF.Copy, (psb,), (rawb,))
                    sq, sqb = self.tmp("bf")
                    self.actf(sq[:, 0:n], ps[:, 0:n], AF.Square, (psb,), (sqb,))
                    self.mm(ss[:, 0:n], cb[:, C_ONES:C_ONES + 128], sq[:, 0:n], c4 == 0, c4 == 3, (self.cbb, sqb), (ssb,))
                    raws.append((raw, rawb))
                rt, rtb = self.tmp("f32")
                self.act_pow(rt[:, 0:n], ss[:, 0:n], -0.5, (ssb,), (rtb,), scale=1.0 / 512, bias=EPS)
                for c4 in range(4):
                    self.stt(cqn[:, c4, 0:n], raws[c4][0][:, 0:n], self.pvc(l, PV_DQNORM + c4), rt[:, 0:n], ALU.mult, ALU.mult, (raws[c4][1], rtb, self.pvb), (cqnb,))
                rope = (C_R96, self.tab[0], self.tab[1]) if tc < 4 else None
                for hg in ((0, 1), (2, 3)):
                    tiles = []
                    for h in hg:
                        ps, psb = self.proj_fm(wu, wub, h * 96, 96, tc, kch=4, rhs=lambda k: (cqn[:, k, 0:n], cqnb))
                        tiles.append(dict(ps=ps, psb=psb, P=96, n=n, bdcol=C_BD96, inv=self.pvc(l, PV_INV96, rows=96), gain=self.pvc(l, PV_DQN, rows=96), rope=rope, t0=t0,
                                          dest=qd[h][0:96, 0:n], destb=qdb[h]))
                    self.headnorm_multi(tiles)
                if hf == 0 and tc == 0:
                    self.dump(f"qd{l}", qd[1][0:96, :], [96, 512], (qdb[1],))
                kts = [(j, 0, n) for j in range(18)] if tc < 4 else [(16, 0, n), (17, 0, n)]
                if tc != qtcs[-1]:
                    nxt = (self.wload(wv[:, :, 1536:2048], [128, 8, 512]), self.wload(uq[:, :, hf * 384:(hf + 1) * 384], [128, 4, 384]))
                else:
                    mpre = [self.merge_preload(l, 3, [2 * hf, 2 * hf + 1], qb_) for qb_ in range(2)]
                self.mark_phase(f'  D{hf}_att{tc}')
                jobs = []
                for h in range(4):
                    par = h % 2

                    def fin(accs, h=h, par=par, n=n, t0=t0, tc=tc):
                        acc, accb = accs[0]
                        self.norm_out(acc, accb, par, n, br[h // 2][par * 64:(par + 1) * 64, t0:t0 + n], brb[h // 2][tc])
                    jobs.append(dict(qap=qd[h], qb=qdb[h], KT=KD[h], Kb=KDb[h], P=128, kts=kts,
                                     vfun=(lambda j, h=h: [VD[h][:, j, :]]),
                                     vb_of=(lambda j: VDb[j // 4 if j < 16 else 4]), scale=1.0 / math.sqrt(96.0), masks=None, nacc=1, fin=fin))
                self.attn_stream(jobs)
                if tc == 0 and hf == 0:
                    self.dump(f"br3_{l}", br[0][:, 0:512], [128, 512], (brb[0][0],))
            self.mark_phase(f'  D{hf}_mrg0')
            self.merge(l, 3, [2 * hf, 2 * hf + 1], br, brb, qtcs, first=False, pre=mpre)
            self.soft_barrier()
        self.release(m0)
        self.dump(f"z{l}", self.z[:, :, :], [128, 8, NT], [b for r in self.zb for b in r])

    def wout_phase(self, l, last):
        self.sp_off = self.mix_base
        self.lat_at_base = False
        self.lat = self.alloc([128, 8, L], F32, "lat")
        self.latb = [[Buf(f"lat{dc}_{tc}") for tc in range(5)] for dc in range(8)]
        if l == 1:
            self.LT = self.alloc([128, L], F32, "LT")
            self.LTb = [Buf(f"LT{tc}") for tc in range(4)]
        self.ffn_base = self.phase_base
        ov = self.outT.rearrange("(k p) t -> p k t", p=128)
        wov = self.w_out[l].rearrange("(k p) c -> p k c", p=128)
        for hf in range(2):
            for tc, (t0, n) in enumerate(TCS[:4]):
                for d4 in range(4):
                    dc = hf * 4 + d4
                    self.dma(self.sp, self.lat[:, dc, t0:t0 + n], ov[:, dc, t0:t0 + n], (self.outb[dc][tc],), (self.latb[dc][tc],), self.ldsem())
        wblocks = []
        for qb in range(4):
            while len(wblocks) < min(4, qb + 3):
                c0 = len(wblocks) * 256
                wblocks.append(self.wload(wov[:, :, c0:c0 + 256], [128, 8, 256]))
            wt, wb = wblocks[qb]
            for tc, (t0, n) in enumerate(TCS):
                if tc == 4 and last:
                    continue
                j2 = 1 if tc == 4 else 0
                for d2 in range(2):
                    dc = qb * 2 + d2
                    ps, psb = self.newps()
                    for k in range(8):
                        self.mm(ps[:, 0:n], wt[:, k, d2 * 128:(d2 + 1) * 128], self.z[:, k, t0:t0 + n], k == 0, k == 7, (wb, self.zb[k][tc]), (psb,))
                    la = self.lat_ap(dc, tc)
                    self.stt(la, ps[:, 0:n], self.mcol(2, dc, j2), la, ALU.mult, ALU.add, (psb, self.modb, self.latb[dc][tc]), (self.latb[dc][tc],))
        self.dump(f"latmix{l}", self.lat[:, :, :], [128, 8, L], [self.latb[dc][tc] for dc in range(8) for tc in range(4)])

    def ffn_phase(self, l, last):
        moe = (l == 1)
        self.sp_off = self.ffn_base
        ntc = 4 if last else 5
        ntok = L if last else NT
        G = 4
        act = self.alloc([128, G, ntok], BF16, "act")
        actb = [[Buf(f"act{f}_{tc}") for tc in range(5)] for f in range(G)]
        if moe:
            lse = self.alloc([128, L], F32, "lse")
            m2 = self.alloc([128, L], F32, "m2")
            lseb = [Buf(f"lse{tc}") for tc in range(4)]
            m2b = [Buf(f"m2{tc}") for tc in range(4)]
            cw = [self.alloc([128, L], BF16, "cw") for _ in range(1)]
            cwb = [[Buf(f"cw{i}_{tc}") for tc in range(4)] for i in range(1)]
            for tc in range(4):
                t0, n = TCS[tc]
                for e in range(8):
                    rp, rpb = self.newps()
                    slh, slb_ = self.sel_ap(e)
                    self.mm(rp[:, 0:n], slh[:, :], self.LT[:, t0:t0 + n], True, True, (slb_, self.LTb[tc]), (rpb,))
                    if e == 0:
                        self.vcopy(lse[:, t0:t0 + n], rp[:, 0:n], (rpb,), (lseb[tc],))
                    else:
                        self.tt(lse[:, t0:t0 + n], rp[:, 0:n], lse[:, t0:t0 + n], ALU.max, (rpb, lseb[tc]), (lseb[tc],))
                for e in range(8):
                    rp, rpb = self.newps()
                    slh, slb_ = self.sel_ap(e)
                    self.mm(rp[:, 0:n], slh[:, :], self.LT[:, t0:t0 + n], True, True, (slb_, self.LTb[tc]), (rpb,))
                    ge, geb = self.tmp("f32")
                    self.tt(ge[:, 0:n], rp[:, 0:n], lse[:, t0:t0 + n], ALU.is_ge, (rpb, lseb[tc]), (geb,))
                    t2, t2b = self.tmp("f32")
                    self.stt(t2[:, 0:n], ge[:, 0:n], NEG_BIG, rp[:, 0:n], ALU.mult, ALU.add, (geb, rpb), (t2b,))
                    if e == 0:
                        self.vcopy(m2[:, t0:t0 + n], t2[:, 0:n], (t2b,), (m2b[tc],))
                    else:
                        self.tt(m2[:, t0:t0 + n], t2[:, 0:n], m2[:, t0:t0 + n], ALU.max, (t2b, m2b[tc]), (m2b[tc],))
                d, db = self.tmp("f32")
                self.tt(d[:, 0:n], m2[:, t0:t0 + n], lse[:, t0:t0 + n], ALU.subtract, (m2b[tc], lseb[tc]), (db,))
                self.actf(d[:, 0:n], d[:, 0:n], AF.Exp, (db,), (db,))
                self.actf(d[:, 0:n], d[:, 0:n], AF.Ln, (db,), (db,), bias=1.0)
                self.tt(lse[:, t0:t0 + n], lse[:, t0:t0 + n], d[:, 0:n], ALU.add, (db, lseb[tc]), (lseb[tc],))
            self.dump("m2", m2[:, :], [128, L], m2b)
            self.dump("lse", lse[:, :], [128, L], lseb)
            units = [(e, g) for e in range(8) for g in range(EXPERT_FF // 512)]
        else:
            units = [(None, g) for g in range((D_FF + 511) // 512)]
        cur_e = None
        for (e, g) in units:
            if moe:
                w1v = self.moe_w1[0, e].rearrange("(k p) c -> p k c", p=128)
                w3v = self.moe_w3[0, e].rearrange("(k p) c -> p k c", p=128)
                w2v = self.moe_w2[0, e].rearrange("(f p) d -> p f d", p=128)
                nf = 4
                if e != cur_e:
                    cur_e = e
                    cwt = cw[0]
                    cwtb = cwb[0]
                    for tc in range(4):
                        t0, n = TCS[tc]
                        rp, rpb = self.newps()
                        slh, slb_ = self.sel_ap(e)
                        self.mm(rp[:, 0:n], slh[:, :], self.LT[:, t0:t0 + n], True, True, (slb_, self.LTb[tc]), (rpb,))
                        d, db = self.tmp("f32")
                        self.tt(d[:, 0:n], rp[:, 0:n], lse[:, t0:t0 + n], ALU.subtract, (rpb, lseb[tc]), (db,))
                        self.actf(d[:, 0:n], d[:, 0:n], AF.Exp, (db,), (db,))
                        sl, slb = self.tmp("f32")
                        self.tt(sl[:, 0:n], rp[:, 0:n], m2[:, t0:t0 + n], ALU.is_ge, (rpb, m2b[tc]), (slb,))
                        self.tt(cwt[:, t0:t0 + n], d[:, 0:n], sl[:, 0:n], ALU.mult, (db, slb), (cwtb[tc],))
                    if e == 0:
                        self.dump("cw0", cwt[:, :], [128, L], cwtb)
            else:
                w1v = self.ffg[0].rearrange("(k p) c -> p k c", p=128)
                w3v = self.ffu[0].rearrange("(k p) c -> p k c", p=128)
                w2v = self.ffd[0].rearrange("(f p) d -> p f d", p=128)
                nf = min(4, (D_FF - g * 512) // 128)
            c0 = g * 512
            sp = 2 if nf == 4 else 1
            w1t, w1bs = self.wload(w1v[:, :, c0:c0 + nf * 128], [128, 8, nf * 128], split=sp)
            w3t, w3bs = self.wload(w3v[:, :, c0:c0 + nf * 128], [128, 8, nf * 128], split=sp)
            if sp == 1:
                w1bs, w3bs = [w1bs, w1bs], [w3bs, w3bs]
            for tc in range(ntc):
                t0, n = TCS[tc]
                for f in range(nf):
                    gp, gpb = self.proj_fm(w1t, w1bs[f // 2], f * 128, 128, tc)
                    up, upb = self.proj_fm(w3t, w3bs[f // 2], f * 128, 128, tc)
                    sg, sgb = self.tmp("f32")
                    self.actf(sg[:, 0:n], gp[:, 0:n], AF.Silu, (gpb,), (sgb,))
                    if moe:
                        t, tb = self.tmp("f32")
                        self.tt(t[:, 0:n], up[:, 0:n], sg[:, 0:n], ALU.mult, (upb, sgb), (tb,))
                        self.tt(act[:, f, t0:t0 + n], t[:, 0:n], cwt[:, t0:t0 + n], ALU.mult, (tb, cwtb[tc]), (actb[f][tc],))
                    else:
                        self.tt(act[:, f, t0:t0 + n], up[:, 0:n], sg[:, 0:n], ALU.mult, (upb, sgb), (actb[f][tc],))
            w2t, w2b = self.wload(w2v[:, g * 4:g * 4 + nf, :], [128, nf, 1024])
            for tc in range(ntc):
                t0, n = TCS[tc]
                j2 = 1 if tc == 4 else 0
                for dc in range(8):
                    yp, ypb = self.newps()
                    for f in range(nf):
                        self.mm(yp[:, 0:n], w2t[:, f, dc * 128:(dc + 1) * 128], act[:, f, t0:t0 + n], f == 0, f == nf - 1, (w2b, actb[f][tc]), (ypb,))
                    la = self.lat_ap(dc, tc)
                    self.stt(la, yp[:, 0:n], self.mcol(5, dc, j2), la, ALU.mult, ALU.add, (ypb, self.modb, self.latb[dc][tc]), (self.latb[dc][tc],))
        self.dump(f"lat{l}", self.lat[:, :, :], [128, 8, L], [self.latb[dc][tc] for dc in range(8) for tc in range(4)])


def _rope_tabs():
    def axial(rot_dim):
        t = np.arange(L)
        n = rot_dim // 4
        inv = np.power(np.float32(10000.0), -np.arange(n, dtype=np.float32) / np.float32(n)).astype(np.float32)
        ang = np.concatenate([(t // 64).astype(np.float32)[:, None] * inv, (t % 64).astype(np.float32)[:, None] * inv], axis=-1)
        return np.cos(ang).astype(np.float32), np.sin(ang).astype(np.float32)
    tabs = np.zeros((4, 128, L), np.float32)
    c, s = axial(64)
    for p in range(128):
        tabs[0, p] = c[:, p % 32]
        tabs[1, p] = s[:, p % 32]
    c, s = axial(32)
    tabs[2, :64] = 1.0
    for p in range(64, 96):
        tabs[2, p] = c[:, (p - 64) % 16]
        tabs[3, p] = s[:, (p - 64) % 16]
    return tabs


def _consts():
    cst = np.zeros((128, NCB), np.float32)
    cst[:, C_ONES:C_ONES + 128] = 1.0
    for p in range(128):
        for m in range(128):
            if p // 64 == m // 64:
                cst[p, C_BD64 + m] = 1.0
    for p in range(96):
        for m in range(96):
            if (p < 64) == (m < 64):
                cst[p, C_BD96 + m] = 1.0
    for m in range(128):
        if m % 64 < 32:
            cst[m + 32, C_R64 + m] = -1.0
        else:
            cst[m - 32, C_R64 + m] = 1.0
    for m in range(64, 96):
        if m < 80:
            cst[m + 16, C_R96 + m] = -1.0
        else:
            cst[m - 16, C_R96 + m] = 1.0
    k = np.arange(128)[:, None]
    i = np.arange(128)[None, :]
    cst[:, C_MASKL:C_MASKL + 128] = (k >= i)
    cst[:, C_MASKR:C_MASKR + 128] = (k <= i)
    pidx = np.broadcast_to(np.arange(128, dtype=np.float32)[:, None], (128, 128)).copy()
    return cst, pidx


def _pv(inp):
    pv = np.zeros((2, 128, NPV), np.float32)
    t2 = lambda v: np.tile(np.asarray(v, np.float32), 2)
    for l in range(2):
        pv[l, :, PV_MIXN:PV_MIXN + 8] = inp["mix_norm"][l].reshape(8, 128).T
        pv[l, :, PV_FFNN:PV_FFNN + 8] = inp["ffn_norm"][l].reshape(8, 128).T
        pv[l, :, PV_BMOD:PV_BMOD + 48] = inp["b_mod"][l].reshape(48, 128).T
        pv[l, :, PV_AQN] = t2(inp["a_qn"][l]); pv[l, :, PV_AKN] = t2(inp["a_kn"][l])
        pv[l, :, PV_BQN] = t2(inp["b_qn"][l]); pv[l, :, PV_BKN] = t2(inp["b_kn"][l])
        pv[l, :, PV_CQN] = t2(inp["c_qn"][l]); pv[l, :, PV_CKN] = t2(inp["c_kn"][l])
        pv[l, :, PV_DQNORM:PV_DQNORM + 4] = inp["d_q_norm"][l].reshape(4, 128).T
        pv[l, :, PV_DKVNORM:PV_DKVNORM + 2] = inp["d_kv_norm"][l].reshape(2, 128).T
        pv[l, 0:64, PV_DQN] = inp["d_qn_nope"][l]; pv[l, 64:96, PV_DQN] = inp["d_qn_rope"][l]
        pv[l, 0:64, PV_DKN] = inp["d_kn_nope"][l]; pv[l, 64:96, PV_DKN] = inp["d_kn_rope"][l]
        pv[l, :, PV_SUBLN] = inp["c_subln"][l]
        pv[l, :, PV_SINK:PV_SINK + 8] = np.broadcast_to(inp["a_sink"][l][None, :], (128, 8))
        pv[l, 0:64, PV_LQK + 0] = inp["c_lq1"][l]; pv[l, 0:64, PV_LQK + 1] = inp["c_lk1"][l]
        pv[l, 0:64, PV_LQK + 2] = inp["c_lq2"][l]; pv[l, 0:64, PV_LQK + 3] = inp["c_lk2"][l]
        pv[l, 0:64, PV_INV96] = 1.0 / 64; pv[l, 64:96, PV_INV96] = 1.0 / 32
    return pv


_CACHE = {}


def make_in_maps(inp, ncores=8):
    f = lambda a: np.ascontiguousarray(np.asarray(a, dtype=np.float32))
    cst, sel = _consts()
    tabs = _rope_tabs()
    pv = _pv(inp)
    shared = {k: f(inp[k]) for k in ["w_mod", "w_in", "d_w_uq", "d_w_ukv", "w_br", "w_out", "ff_w_gate", "ff_w_up", "ff_w_down",
                                      "moe_router", "moe_w1", "moe_w3", "moe_w2"]}
    shared.update(cstf=cst, pidx=sel, tabs=tabs, pv=pv)
    maps = []
    for b in range(ncores):
        m = dict(shared)
        m["xT"] = f(np.asarray(inp["x"][b]).T)
        m["ctxT"] = f(np.asarray(inp["ctx"][b]).T)
        cv = np.zeros((128, 16), np.float32)
        cv[:, 0:8] = np.asarray(inp["c"][b]).reshape(8, 128).T
        cv[:, 8:16] = np.asarray(inp["c_ctx"]).reshape(8, 128).T
        m["cvec"] = cv
        maps.append(m)
    return maps


FUSED = True


def _drop(m, layers):
    m = dict(m)
    if 0 not in layers:
        for k in ["ff_w_gate", "ff_w_up", "ff_w_down"]:
            m.pop(k)
    if 1 not in layers:
        for k in ["moe_router", "moe_w1", "moe_w3", "moe_w2"]:
            m.pop(k)
    return m


def kernel(**inputs):
    maps = make_in_maps(inputs)
    if FUSED:
        if "full" not in _CACHE:
            _CACHE["full"] = KB(layers=(0, 1))
        kb = _CACHE["full"]
        res = run_bass_kernel_spmd(kb.nc, maps, core_ids=list(range(8)))
    else:
        for key, layers in (("l0", (0,)), ("l1", (1,))):
            if key not in _CACHE:
                _CACHE[key] = KB(layers=layers)
        r0 = run_bass_kernel_spmd(_CACHE["l0"].nc, [_drop(m, (0,)) for m in maps], core_ids=list(range(8)))
        maps1 = []
        for m, r in zip(maps, r0.results):
            m1 = _drop(m, (1,))
            m1["xT"] = np.ascontiguousarray(np.asarray(r["outT"], dtype=np.float32))
            m1["ctxT"] = np.ascontiguousarray(np.asarray(r["ctxo"], dtype=np.float32))
            maps1.append(m1)
        res = run_bass_kernel_spmd(_CACHE["l1"].nc, maps1, core_ids=list(range(8)))
    out = np.stack([np.asarray(r["outT"]).T for r in res.results], axis=0)
    return np.ascontiguousarray(out.astype(np.float32))
```
